# Optimizing a Trainium2 kernel written in Bass

```python
import math
import jax
import jax.numpy as jnp
from jax import lax
import numpy as np

D_MODEL = 2048
BATCH = 16
SEQ = 2048
DEPTH = 2

GRID_W = 64
CTX_LEN = 256
EPS = 1e-6
N_EVEN = (DEPTH + 1) // 2
N_ODD = DEPTH // 2

CONV_WIDTH = D_MODEL // 2
CONV_K = 3
LRU_WIDTH = D_MODEL // 2
LRU_HEADS = 8
LRU_BLOCK = LRU_WIDTH // LRU_HEADS
LRU_CONV_K = 4
LRU_C = 8.0
EVEN_IN_COLS = 3 * CONV_WIDTH + 2 * LRU_WIDTH
DIFF_HEADS = 8
DIFF_QK_DIM = 128
DIFF_V_DIM = 2 * DIFF_QK_DIM
DIFF_QK_COLS = DIFF_HEADS * 2 * DIFF_QK_DIM
DIFF_V_COLS = DIFF_HEADS * DIFF_V_DIM
ODD_IN_COLS = 2 * DIFF_QK_COLS + DIFF_V_COLS
ROPE_AXIS_DIM = DIFF_QK_DIM // 2
ROPE_BASE = 10000.0
Q_BLOCK = 128
N_EXPERTS = 16
N_GROUPS = 4
EXPERTS_PER_GROUP = N_EXPERTS // N_GROUPS
TOP_K = 2
EXPERT_FF = 1024

kernel_name = 'hybrid_conv_rglru_diffattn_moe_dit'


def _rmsnorm(x, g):
    xf = x.astype(jnp.float32)
    xf = xf * lax.rsqrt(jnp.mean(xf * xf, axis=-1, keepdims=True) + EPS)
    return (xf * g.astype(jnp.float32)).astype(x.dtype)


def _dwconv(u, w, pad):
    return lax.conv_general_dilated(
        u, w[:, None, :].astype(u.dtype), window_strides=(1,), padding=[pad],
        dimension_numbers=('NWC', 'WIO', 'NWC'), feature_group_count=u.shape[-1])


def _linear_scan(a, b, h0=None):
    if h0 is not None:
        b = b.at[:, 0].add(a[:, 0] * h0)

    def combine(left, right):
        a_l, b_l = left
        a_r, b_r = right
        return a_l * a_r, a_r * b_l + b_r

    return lax.associative_scan(combine, (a, b), axis=1)[1]


def _rglru_coeffs(u, wa, ba, wi, bi, lam):
    bsz, n, w = u.shape
    ub = u.reshape(bsz, n, LRU_HEADS, LRU_BLOCK)
    r = jax.nn.sigmoid(jnp.einsum('bshi,hij->bshj', ub, wa).reshape(bsz, n, w) + ba)
    i = jax.nn.sigmoid(jnp.einsum('bshi,hij->bshj', ub, wi).reshape(bsz, n, w) + bi)
    log_a = -LRU_C * r * jax.nn.softplus(-lam)
    a = jnp.exp(log_a)
    b = jnp.sqrt(-jnp.expm1(2.0 * log_a)) * (i * u)
    return a, b


def _bi_rglru(u_ctx, u_lat, wa, ba, wi, bi, lam):
    out_c, out_l = 0.0, 0.0
    for d in range(2):
        uc = u_ctx if d == 0 else u_ctx[:, ::-1]
        ul = u_lat if d == 0 else u_lat[:, ::-1]
        a_c, b_c = _rglru_coeffs(uc, wa[d], ba[d], wi[d], bi[d], lam[d])
        h_c = _linear_scan(a_c, b_c)
        a_l, b_l = _rglru_coeffs(ul, wa[d], ba[d], wi[d], bi[d], lam[d])
        h_l = _linear_scan(a_l, b_l, h_c[:, -1])
        if d == 1:
            h_c, h_l = h_c[:, ::-1], h_l[:, ::-1]
        out_c = out_c + h_c
        out_l = out_l + h_l
    return out_c, out_l


def _conv_lru_mixer(h_ctx, h_lat, w_in, conv_a_w, conv_b_w, conv_b_b,
                    lru_wa, lru_ba, lru_wi, lru_bi, lru_lam, w_out):
    cuts = [CONV_WIDTH, 2 * CONV_WIDTH, 3 * CONV_WIDTH, 3 * CONV_WIDTH + LRU_WIDTH]

    def branches(h):
        gb, gc, xa, yb, xb = jnp.split(h @ w_in, cuts, axis=-1)
        y_a = gb * _dwconv(gc * xa, conv_a_w, (1, 1))
        u = _dwconv(xb, conv_b_w, (2, 1)) + conv_b_b
        return y_a, jax.nn.gelu(yb), u

    ya_c, gate_c, u_c = branches(h_ctx)
    ya_l, gate_l, u_l = branches(h_lat)
    hr_c, hr_l = _bi_rglru(u_c, u_l, lru_wa, lru_ba, lru_wi, lru_bi, lru_lam)
    y_ctx = jnp.concatenate([ya_c, gate_c * hr_c], axis=-1) @ w_out
    y_lat = jnp.concatenate([ya_l, gate_l * hr_l], axis=-1) @ w_out
    return y_ctx, y_lat


def _axial_rope_tables(n_tok, dtype):
    rows = n_tok // GRID_W
    row = jnp.repeat(jnp.arange(rows, dtype=jnp.float32), GRID_W)
    col = jnp.tile(jnp.arange(GRID_W, dtype=jnp.float32), rows)
    inv = ROPE_BASE ** (-jnp.arange(0, ROPE_AXIS_DIM, 2, dtype=jnp.float32) / ROPE_AXIS_DIM)
    ang_r = row[:, None] * inv
    ang_c = col[:, None] * inv
    ang = jnp.concatenate([ang_r, ang_r, ang_c, ang_c], axis=-1)
    return jnp.cos(ang).astype(dtype), jnp.sin(ang).astype(dtype)


def _apply_axial_rope(t, cos, sin):
    half = ROPE_AXIS_DIM // 2
    tr = t.reshape(t.shape[:-1] + (2, 2, half))
    rot = jnp.stack([-tr[..., 1, :], tr[..., 0, :]], axis=-2).reshape(t.shape)
    return t * cos[:, None, None, :] + rot * sin[:, None, None, :]


def _diff_softmax_mix(q, k, v, lam):
    s = jnp.einsum('bqhmd,bkhmd->bhmqk', q, k).astype(jnp.float32) * (DIFF_QK_DIM ** -0.5)
    p = jax.nn.softmax(s, axis=-1)
    w = p[:, :, 0] - lam * p[:, :, 1]
    return jnp.einsum('bhqk,bkhe->bqhe', w.astype(v.dtype), v)


def _diff_head_out(o, g, lam_init, w_out):
    bsz, n = o.shape[0], o.shape[1]
    of = o.astype(jnp.float32)
    of = of * lax.rsqrt(jnp.mean(of * of, axis=-1, keepdims=True) + EPS)
    of = of * g.astype(jnp.float32) * (1.0 - lam_init)
    return of.astype(o.dtype).reshape(bsz, n, DIFF_V_COLS) @ w_out


def _diff_attn_mixer(h_ctx, h_lat, w_in, lq1, lk1, lq2, lk2, subln_g, w_out, lam_init, ctx_out):
    bsz, n, _ = h_lat.shape
    n_ctx = h_ctx.shape[1]
    q, k, v = jnp.split(h_lat @ w_in, [DIFF_QK_COLS, 2 * DIFF_QK_COLS], axis=-1)
    q = q.reshape(bsz, n, DIFF_HEADS, 2, DIFF_QK_DIM)
    k = k.reshape(bsz, n, DIFF_HEADS, 2, DIFF_QK_DIM)
    v = v.reshape(bsz, n, DIFF_HEADS, DIFF_V_DIM)
    cos, sin = _axial_rope_tables(n, h_lat.dtype)
    q = _apply_axial_rope(q, cos, sin)
    k = _apply_axial_rope(k, cos, sin)
    kc, vc = jnp.split(h_ctx @ w_in[:, DIFF_QK_COLS:], [DIFF_QK_COLS], axis=-1)
    kc = kc.reshape(bsz, n_ctx, DIFF_HEADS, 2, DIFF_QK_DIM)
    vc = vc.reshape(bsz, n_ctx, DIFF_HEADS, DIFF_V_DIM)
    k_all = jnp.concatenate([kc, k], axis=1)
    v_all = jnp.concatenate([vc, v], axis=1)
    lam = (jnp.exp(jnp.sum(lq1.astype(jnp.float32) * lk1.astype(jnp.float32)))
           - jnp.exp(jnp.sum(lq2.astype(jnp.float32) * lk2.astype(jnp.float32))) + lam_init)
    n_blk = n // Q_BLOCK
    qb = q.reshape(bsz, n_blk, Q_BLOCK, DIFF_HEADS, 2, DIFF_QK_DIM).swapaxes(0, 1)
    o = lax.map(lambda qi: _diff_softmax_mix(qi, k_all, v_all, lam), qb)
    o = o.swapaxes(0, 1).reshape(bsz, n, DIFF_HEADS, DIFF_V_DIM)
    y_lat = _diff_head_out(o, subln_g, lam_init, w_out)
    y_ctx = None
    if ctx_out:
        qc = (h_ctx @ w_in[:, :DIFF_QK_COLS]).reshape(bsz, n_ctx, DIFF_HEADS, 2, DIFF_QK_DIM)
        y_ctx = _diff_head_out(_diff_softmax_mix(qc, kc, vc, lam), subln_g, lam_init, w_out)
    return y_ctx, y_lat


def _moe(h, router_w, router_b, w_gate, w_up, w_down):
    n_tok = h.shape[0]
    scores = jax.nn.sigmoid((h @ router_w).astype(jnp.float32))
    sel = (scores + router_b.astype(jnp.float32)).reshape(n_tok, N_GROUPS, EXPERTS_PER_GROUP)
    grp_score = jnp.sum(lax.top_k(sel, 2)[0], axis=-1)
    g_idx = jnp.argmax(grp_score, axis=-1)
    in_grp = (jnp.arange(N_GROUPS)[None, :] == g_idx[:, None])[:, :, None]
    masked = jnp.where(in_grp, sel, -jnp.inf).reshape(n_tok, N_EXPERTS)
    _, top_idx = lax.top_k(masked, TOP_K)
    top_s = jnp.take_along_axis(scores, top_idx, axis=-1)
    gates = top_s / jnp.sum(top_s, axis=-1, keepdims=True)
    comb = jnp.sum(jax.nn.one_hot(top_idx, N_EXPERTS, dtype=jnp.float32) * gates[..., None], axis=1)
    comb = comb.astype(h.dtype)
    out = jnp.zeros_like(h)
    for e in range(N_EXPERTS):
        hid = jax.nn.silu(h @ w_gate[e]) * (h @ w_up[e])
        out = out + comb[:, e:e + 1] * (hid @ w_down[e])
    return out


def setup_inputs(seed: int = 0) -> dict:
    key = jax.random.key(seed)
    ks = iter(jax.random.split(key, 40))

    def nrm(shape, scale):
        return jax.random.normal(next(ks), shape, jnp.float32) * scale

    d = D_MODEL
    u = jax.random.uniform(next(ks), (N_EVEN, 2, LRU_WIDTH), jnp.float32, minval=0.9, maxval=0.999)
    a0 = u ** (1.0 / LRU_C)
    return {
        'x': nrm((BATCH, SEQ, d), 1.0),
        'c': nrm((BATCH, d), 1.0),
        'ctx': nrm((BATCH, CTX_LEN, d), 1.0),
        'c_ctx': nrm((d,), 1.0),
        'ada_w': nrm((DEPTH, d, 6 * d), 0.5 * d ** -0.5),
        'ada_b': nrm((DEPTH, 6 * d), 0.01),
        'norm_mix_g': 1.0 + nrm((DEPTH, d), 0.02),
        'norm_ffn_g': 1.0 + nrm((DEPTH, d), 0.02),
        'final_g': 1.0 + nrm((d,), 0.02),
        'w_in_e': nrm((N_EVEN, d, EVEN_IN_COLS), d ** -0.5),
        'conv_a_w': nrm((N_EVEN, CONV_K, CONV_WIDTH), CONV_K ** -0.5),
        'conv_b_w': nrm((N_EVEN, LRU_CONV_K, LRU_WIDTH), LRU_CONV_K ** -0.5),
        'conv_b_b': nrm((N_EVEN, LRU_WIDTH), 0.01),
        'lru_wa': nrm((N_EVEN, 2, LRU_HEADS, LRU_BLOCK, LRU_BLOCK), LRU_BLOCK ** -0.5),
        'lru_ba': nrm((N_EVEN, 2, LRU_WIDTH), 0.01),
        'lru_wi': nrm((N_EVEN, 2, LRU_HEADS, LRU_BLOCK, LRU_BLOCK), LRU_BLOCK ** -0.5),
        'lru_bi': nrm((N_EVEN, 2, LRU_WIDTH), 0.01),
        'lru_lam': jnp.log(a0) - jnp.log1p(-a0),
        'w_out_e': nrm((N_EVEN, CONV_WIDTH + LRU_WIDTH, d), (CONV_WIDTH + LRU_WIDTH) ** -0.5),
        'w_in_o': nrm((N_ODD, d, ODD_IN_COLS), d ** -0.5),
        'lam_q1': nrm((N_ODD, DIFF_QK_DIM), 0.1),
        'lam_k1': nrm((N_ODD, DIFF_QK_DIM), 0.1),
        'lam_q2': nrm((N_ODD, DIFF_QK_DIM), 0.1),
        'lam_k2': nrm((N_ODD, DIFF_QK_DIM), 0.1),
        'subln_g': 1.0 + nrm((N_ODD, DIFF_V_DIM), 0.02),
        'w_out_o': nrm((N_ODD, DIFF_V_COLS, d), DIFF_V_COLS ** -0.5),
        'router_w': nrm((d, N_EXPERTS), d ** -0.5),
        'router_b': nrm((N_EXPERTS,), 0.01),
        'exp_w_gate': nrm((DEPTH, N_EXPERTS, d, EXPERT_FF), d ** -0.5),
        'exp_w_up': nrm((DEPTH, N_EXPERTS, d, EXPERT_FF), d ** -0.5),
        'exp_w_down': nrm((DEPTH, N_EXPERTS, EXPERT_FF, d), EXPERT_FF ** -0.5),
    }


def reference(x, c, ctx, c_ctx, ada_w, ada_b, norm_mix_g, norm_ffn_g, final_g,
              w_in_e, conv_a_w, conv_b_w, conv_b_b, lru_wa, lru_ba, lru_wi, lru_bi, lru_lam, w_out_e,
              w_in_o, lam_q1, lam_k1, lam_q2, lam_k2, subln_g, w_out_o,
              router_w, router_b, exp_w_gate, exp_w_up, exp_w_down):
    d = x.shape[-1]
    silu_c = jax.nn.silu(c)
    silu_cc = jax.nn.silu(c_ctx)
    x_lat, x_ctx = x, ctx
    for l in range(DEPTH):
        last = l == DEPTH - 1
        i = l // 2
        sh1, sc1, g1, sh2, sc2, g2 = jnp.split((silu_c @ ada_w[l] + ada_b[l])[:, None, :], 6, axis=-1)
        sh1c, sc1c, g1c, sh2c, sc2c, g2c = jnp.split(silu_cc @ ada_w[l] + ada_b[l], 6, axis=-1)
        h_lat = _rmsnorm(x_lat, norm_mix_g[l]) * (1.0 + sc1) + sh1
        h_ctx = _rmsnorm(x_ctx, norm_mix_g[l]) * (1.0 + sc1c) + sh1c
        if l % 2 == 0:
            y_ctx, y_lat = _conv_lru_mixer(h_ctx, h_lat, w_in_e[i], conv_a_w[i], conv_b_w[i], conv_b_b[i],
                                           lru_wa[i], lru_ba[i], lru_wi[i], lru_bi[i], lru_lam[i], w_out_e[i])
        else:
            lam_init = 0.8 - 0.6 * math.exp(-0.3 * l)
            y_ctx, y_lat = _diff_attn_mixer(h_ctx, h_lat, w_in_o[i], lam_q1[i], lam_k1[i], lam_q2[i],
                                            lam_k2[i], subln_g[i], w_out_o[i], lam_init, not last)
        x_lat = x_lat + g1 * y_lat
        f_lat = _rmsnorm(x_lat, norm_ffn_g[l]) * (1.0 + sc2) + sh2
        if last:
            y = _moe(f_lat.reshape(-1, d), router_w, router_b, exp_w_gate[l], exp_w_up[l], exp_w_down[l])
            x_lat = x_lat + g2 * y.reshape(x_lat.shape)
        else:
            x_ctx = x_ctx + g1c * y_ctx
            f_ctx = _rmsnorm(x_ctx, norm_ffn_g[l]) * (1.0 + sc2c) + sh2c
            n_ctx_tok = f_ctx.shape[0] * f_ctx.shape[1]
            y = _moe(jnp.concatenate([f_ctx.reshape(-1, d), f_lat.reshape(-1, d)], axis=0),
                     router_w, router_b, exp_w_gate[l], exp_w_up[l], exp_w_down[l])
            x_ctx = x_ctx + g2c * y[:n_ctx_tok].reshape(x_ctx.shape)
            x_lat = x_lat + g2 * y[n_ctx_tok:].reshape(x_lat.shape)
    return _rmsnorm(x_lat, final_g)
```

```python
import math
import os
import contextlib
import numpy as np
import concourse.bass as bass
import concourse.mybir as mybir
from concourse.bass_utils import run_bass_kernel_spmd

F32 = mybir.dt.float32
BF16 = mybir.dt.bfloat16
ALU = mybir.AluOpType
AF = mybir.ActivationFunctionType
AX = mybir.AxisListType

D = 2048
SEQ = 2048
CTX = 256
TOK = SEQ + CTX
NT = TOK // 128
EPS = 1e-6
NE = 16
FF = 1024


def bc(ap, n):
    return bass.AP(ap.tensor, ap.offset, [list(x) for x in ap.ap] + [[0, n]])


def bc_mid(ap, n):
    a = [list(x) for x in ap.ap]
    return bass.AP(ap.tensor, ap.offset, [a[0], [0, n]] + a[1:])


def rev(ap):
    a = [list(x) for x in ap.ap]
    st, n = a[-1]
    a[-1] = [-st, n]
    return bass.AP(ap.tensor, ap.offset + st * (n - 1), a)


class Prog:
    def __init__(self, nc, es):
        self.nc = nc
        self.es = es
        self.E = {'pe': nc.tensor, 'act': nc.scalar, 'dve': nc.vector, 'pool': nc.gpsimd, 'sp': nc.sync}
        self.sems = {}
        self.cnt = {}
        self.waited = {e: {} for e in self.E}
        self.lastw = {}
        self.readers = {}
        self.q = {e: [] for e in self.E}

    def sem(self, name):
        if name not in self.sems:
            self.sems[name] = self.es.enter_context(self.nc.semaphore(name))
            self.cnt[name] = 0
        return self.sems[name]

    def _wait(self, eng, s, v):
        if self.waited[eng].get(s, 0) >= v:
            return
        self.waited[eng][s] = v
        self.q[eng].append(('wait', s, v))

    def _deps(self, eng, reads, writes):
        need = {}

        def add(tok):
            if tok is None:
                return
            s, v = tok
            if eng == 'pe' and s == 'c_pe':
                return
            if need.get(s, 0) < v:
                need[s] = v
        for k in reads:
            add(self.lastw.get(k))
        for k in writes:
            add(self.lastw.get(k))
            for s, v in self.readers.get(k, {}).items():
                add((s, v))
        for s, v in need.items():
            self._wait(eng, s, v)

    def _commit(self, tok, reads, writes):
        s, v = tok
        for k in writes:
            self.lastw[k] = tok
            self.readers[k] = {}
        for k in reads:
            r = self.readers.setdefault(k, {})
            if r.get(s, 0) < v:
                r[s] = v

    def op(self, eng, fn, reads=(), writes=()):
        self._deps(eng, reads, writes)
        s = 'c_' + eng
        self.sem(s)
        self.cnt[s] += 1
        self.q[eng].append(('op', fn, s, 1))
        self._commit((s, self.cnt[s]), reads, writes)

    def dma(self, eng, pairs, reads, writes, sem, **kw):
        self._deps(eng, reads, writes)
        self.sem(sem)
        self._wait(eng, sem, self.cnt[sem])
        for (o, i) in pairs:
            self.cnt[sem] += 16
            self.q[eng].append(('op', (lambda e, o=o, i=i: e.dma_start(out=o, in_=i, **kw)), sem, 16))
        self._commit((sem, self.cnt[sem]), reads, writes)

    def barrier(self):
        for eng in self.E:
            for s, v in self.cnt.items():
                if v > 0:
                    self._wait(eng, s, v)
        self.lastw.clear()
        self.readers.clear()

    def flush(self):
        sems = self.sems
        with self.nc.Block() as block:
            for eng, deco in (('pe', block.tensor), ('act', block.scalar), ('dve', block.vector),
                              ('pool', block.gpsimd), ('sp', block.sync)):
                items = self.q[eng]
                self.q[eng] = []
                if not items:
                    continue

                def body(e, items=items):
                    for it in items:
                        if it[0] == 'wait':
                            e.wait_ge(sems[it[1]], it[2])
                        else:
                            ins = it[1](e)
                            ins.then_inc(sems[it[2]], it[3])
                deco(body)


def build(stop_after=None, debug=False, start_layer=0):
    nc = bass.Bass("TRN2", target_bir_lowering=False)
    es = contextlib.ExitStack()
    P = Prog(nc, es)

    def din(name, shape, dt=F32):
        return nc.dram_tensor(name, list(shape), dt, kind="ExternalInput").ap()

    def dscr(name, shape, dt=F32):
        return nc.dram_tensor(name, list(shape), dt, kind=("ExternalOutput" if debug else "Internal")).ap()

    xs = din("xs", [2 * SEQ, D])
    ctxs = din("ctxs", [2 * CTX, D])
    c3T = din("c3T", [128, 48])
    adaw = din("adaw", [2 * D, 6 * D])
    adabT = din("adabT", [128, 192])
    ngT = din("ngT", [128, 64])
    fgrow = din("fgrow", [1, D])
    wine = din("wine", [D, 5120])
    cawT = din("cawT", [128, 24])
    cbwT = din("cbwT", [128, 32])
    cbbT = din("cbbT", [128, 8])
    lruw = din("lruw", [4096, 128])
    lrubT = din("lrubT", [128, 32])
    lamT = din("lamT", [128, 16])
    woute = din("woute", [D, D])
    wino = din("wino", [D, 6144])
    lamv = din("lamv", [1, 512])
    sublnT = din("sublnT", [128, 2])
    wouto = din("wouto", [D, D])
    rw = din("rw", [D, NE])
    rb = din("rb", [1, NE])
    ewg = din("ewg", [2 * NE * D, FF])
    ewu = din("ewu", [2 * NE * D, FF])
    ewd = din("ewd", [2 * NE * FF, D])
    identd = din("identd", [128, 128])
    ropec = din("ropec", [128, SEQ])
    ropes = din("ropes", [128, SEQ])
    rpermd = din("rpermd", [128, 128])

    outd = nc.dram_tensor("out", [2 * SEQ, D], F32, kind="ExternalOutput").ap()

    grow = dscr("grow", [2 * 2 * 3, D])
    mixT_d = dscr("mixT_d", [2 * 16 * 128, TOK], BF16)
    fT_d = dscr("fT_d", [2 * 128, 16 * TOK], BF16)
    x1_d = dscr("x1_d", [2 * TOK, D])
    x2_d = din("x2_d", [2 * TOK, D]) if start_layer == 1 else dscr("x2_d", [2 * TOK, D])
    x3_d = dscr("x3_d", [2 * SEQ, D])

    uid = [0]

    def sb(name, shape, dt=F32, stack=es):
        uid[0] += 1
        return stack.enter_context(nc.sbuf_tensor(f"{name}_{uid[0]}", list(shape), dt))

    def ps(name, shape, dt=F32, stack=es):
        uid[0] += 1
        return stack.enter_context(nc.psum_tensor(f"{name}_{uid[0]}", list(shape), dt))

    identf = sb("identf", [128, 128])
    identb = sb("identb", [128, 128], BF16)
    onesf = sb("onesf", [128, 128])
    onesb = sb("onesb", [128, 128], BF16)
    rpermb = sb("rpermb", [128, 128], BF16)
    rpermf = sb("rpermf", [128, 128])
    scT = sb("scT", [128, 48])
    scTb = sb("scTb", [128, 48], BF16)
    mhalf = sb("mhalf", [128, 1])
    adab = sb("adab", [128, 192])
    ng = sb("ng", [128, 64])
    modc = sb("modc", [128, 2, 4, 48])
    comb = sb("comb", [128, 2, NT, 16])
    caw = sb("caw", [128, 24])
    cbw = sb("cbw", [128, 32])
    cbb = sb("cbb", [128, 8])
    lrub = sb("lrub", [128, 32])
    cneg = sb("cneg", [128, 16])
    lamt = sb("lamt", [128, 16])
    rbb = sb("rbb", [128, 16])
    rwt = sb("rwt", [128, 16, 16])
    subg = sb("subg", [128, 2])
    neglam = sb("neglam", [128, 1])
    lamrow = sb("lamrow", [1, 512])
    lamtmp = sb("lamtmp", [1, 8])

    LAM_INIT = 0.8 - 0.6 * math.exp(-0.3 * 1)
    EMBED_ADA1 = True

    def phase_setup():
        with contextlib.ExitStack() as st:
            pl = ps("pl", [128, 512], stack=st)
            P.dma('sp', [(identf[:, :], identd), (scT[:, :], c3T), (adab[:, :], adabT), (ng[:, :], ngT),
                         (caw[:, :], cawT), (cbw[:, :], cbwT), (cbb[:, :], cbbT), (lrub[:, :], lrubT),
                         (lamt[:, :], lamT), (rbb[:, :], rb.partition_broadcast(128)),
                         (rwt[:, :, :], rw.rearrange("(kc p) e -> p kc e", p=128)),
                         (subg[:, :], sublnT), (lamrow[:, :], lamv), (rpermf[:, :], rpermd)],
                  [], ['consts'], 'ld_c')
            P.op('pool', lambda e: e.memset(onesf[:, :], 1.0), [], ['onesf'])
            P.op('pool', lambda e: e.memset(onesb[:, :], 1.0), [], ['onesb'])
            P.op('dve', lambda e: e.tensor_copy(out=identb[:, :], in_=identf[:, :]), ['consts'], ['identb'])
            P.op('dve', lambda e: e.tensor_copy(out=rpermb[:, :], in_=rpermf[:, :]), ['consts'], ['rpermb'])
            P.op('act', lambda e: e.activation(out=scT[:, :], in_=scT[:, :], func=AF.Silu), ['consts'], ['consts'])
            P.op('dve', lambda e: e.tensor_copy(out=scTb[:, :], in_=scT[:, :]), ['consts'], ['scTb'])
            P.op('pool', lambda e: e.memset(mhalf[:, :], -0.5), [], ['mhalf'])
            P.op('act', lambda e: e.activation(out=cneg[:, :], in_=lamt[:, :], func=AF.Exp, scale=-1.0), ['consts'], ['cneg'])
            P.op('act', lambda e: e.activation(out=cneg[:, :], in_=cneg[:, :], func=AF.Ln, bias=1.0), ['cneg'], ['cneg'])
            P.op('dve', lambda e: e.tensor_scalar(out=cneg[:, :], in0=cneg[:, :], scalar1=-8.0, scalar2=None, op0=ALU.mult),
                 ['cneg'], ['cneg'])
            P.op('dve', lambda e: e.tensor_scalar(out=subg[:, :], in0=subg[:, :], scalar1=(1.0 - LAM_INIT), scalar2=None, op0=ALU.mult),
                 ['consts'], ['consts'])
            P.op('dve', lambda e: e.tensor_tensor(out=lamrow[:, 0:128], in0=lamrow[:, 0:128], in1=lamrow[:, 128:256], op=ALU.mult),
                 ['consts'], ['lr1'])
            P.op('dve', lambda e: e.tensor_tensor(out=lamrow[:, 256:384], in0=lamrow[:, 256:384], in1=lamrow[:, 384:512], op=ALU.mult),
                 ['consts'], ['lr2'])
            P.op('dve', lambda e: e.tensor_reduce(out=lamtmp[:, 0:1], in_=lamrow[:, 0:128], axis=AX.X, op=ALU.add), ['lr1'], ['lt0'])
            P.op('dve', lambda e: e.tensor_reduce(out=lamtmp[:, 1:2], in_=lamrow[:, 256:384], axis=AX.X, op=ALU.add), ['lr2'], ['lt1'])
            P.op('act', lambda e: e.activation(out=lamtmp[:, 2:4], in_=lamtmp[:, 0:2], func=AF.Exp), ['lt0', 'lt1'], ['lt2'])
            P.op('dve', lambda e: e.scalar_tensor_tensor(out=lamtmp[:, 4:5], in0=lamtmp[:, 3:4], scalar=-LAM_INIT, in1=lamtmp[:, 2:3],
                                                         op0=ALU.add, op1=ALU.subtract), ['lt2'], ['lt4'])
            P.op('pe', lambda e: e.matmul(pl[:, 0:1], lhsT=onesf[0:1, :], rhs=lamtmp[0:1, 4:5], start=True, stop=True),
                 ['lt4', 'onesf'], ['pl'])
            P.op('act', lambda e: e.activation(out=neglam[:, :], in_=pl[:, 0:1], func=AF.Copy), ['pl'], ['neglam'])
            P.barrier()
            P.flush()

    def ada_begin(l, st):
        wa = [sb(f"wa{i}", [128, 16, 384], BF16, stack=st) for i in range(2)]
        psA = ps("psA", [128, 512], stack=st)
        modT = sb("modT", [128, 96, 4], stack=st)
        return (l, wa, psA, modT)

    def ada_piece(state, piece):
        l, wa, psA, modT = state
        sl = piece % 2
        src = adaw[l * D:(l + 1) * D, piece * 384:(piece + 1) * 384].rearrange("(kc p) c -> p kc c", p=128)
        P.dma('pool', [(wa[sl][:, :, :], src)], [], [('wa', sl)], f'ld_wa{sl}')
        for cbk in range(3):
            j = piece * 3 + cbk

            def fn(e, j=j, cbk=cbk, sl=sl):
                for kc in range(16):
                    ins = e.matmul(psA[:, j * 4:j * 4 + 3], lhsT=wa[sl][:, kc, cbk * 128:(cbk + 1) * 128],
                                   rhs=scTb[:, kc * 3:(kc + 1) * 3], start=(kc == 0), stop=(kc == 15))
                return ins
            P.op('pe', fn, [('wa', sl), 'scTb'], ['psA'])

    def ada_finish(state):
        l, wa, psA, modT = state
        psA3 = psA[:, 0:384].rearrange("p (j r) -> p j r", r=4)
        P.op('dve', lambda e: e.tensor_tensor(out=modT[:, :, 0:3], in0=psA3[:, :, 0:3], in1=bc(adab[:, l * 96:(l + 1) * 96], 3), op=ALU.add),
             ['psA', 'consts'], ['modT'])
        for which, (sec_sc, sec_sh, gi) in enumerate([(1, 0, 0), (4, 3, 1)]):
            gsc = modc[:, l, 2 * which, :].rearrange("p (k r) -> p k r", r=3)
            sh = modc[:, l, 2 * which + 1, :].rearrange("p (k r) -> p k r", r=3)
            gcol = ng[:, (l * 2 + gi) * 16:(l * 2 + gi + 1) * 16]
            P.op('dve', lambda e, gsc=gsc, sec_sc=sec_sc: e.tensor_scalar(out=gsc, in0=modT[:, sec_sc * 16:(sec_sc + 1) * 16, 0:3],
                                                                          scalar1=1.0, scalar2=None, op0=ALU.add),
                 ['modT'], [('gsc', l, which)])
            P.op('dve', lambda e, gsc=gsc, gcol=gcol: e.tensor_tensor(out=gsc, in0=gsc, in1=bc(gcol, 3), op=ALU.mult),
                 [('gsc', l, which), 'consts'], [('gsc', l, which)])
            P.op('dve', lambda e, sh=sh, sec_sh=sec_sh: e.tensor_copy(out=sh, in_=modT[:, sec_sh * 16:(sec_sh + 1) * 16, 0:3]),
                 ['modT'], [('sh', l, which)])
        pairs = []
        for gi, sec in enumerate((2, 5)):
            for r in range(3):
                row = (l * 2 + gi) * 3 + r
                dst = grow[row:row + 1, :].rearrange("o (j p) -> p (o j)", p=128)
                pairs.append((dst, modT[:, sec * 16:(sec + 1) * 16, r]))
        P.dma('sp', pairs, ['modT'], [('grow', l)], 'st_g')

    def phase_ada(l):
        with contextlib.ExitStack() as st:
            state = ada_begin(l, st)
            for piece in range(32):
                ada_piece(state, piece)
            ada_finish(state)
            with nc.allow_non_contiguous_dma(reason="tiny modulation rows"):
                P.barrier()
                P.flush()

    def norm_tile(st_tiles, x_tile_key, xt, l, which, r, dst_fn, dst_key, tp, tp_key, npass=1, junk_key='junk', kp=''):
        junk, stat, xn = st_tiles
        gsc = modc[:, l, 2 * which, :]
        sh = modc[:, l, 2 * which + 1, :]
        P.op('act', lambda e: e.activation(out=junk[:, :], in_=xt[:, :], func=AF.Square, accum_out=stat[:, 0:1]),
             [x_tile_key], [(kp, junk_key), (kp, 'stat0')])
        P.op('dve', lambda e: e.tensor_scalar(out=stat[:, 1:2], in0=stat[:, 0:1], scalar1=1.0 / D, scalar2=EPS, op0=ALU.mult, op1=ALU.add),
             [(kp, 'stat0')], [(kp, 'stat1')])
        P.op('act', lambda e: e.activation(out=stat[:, 2:3], in_=stat[:, 1:2], func=AF.Sqrt), [(kp, 'stat1')], [(kp, 'stat2')])
        P.op('dve', lambda e: e.reciprocal(out=stat[:, 3:4], in_=stat[:, 2:3]), [(kp, 'stat2')], [(kp, 'stat3')])
        P.op('act', lambda e: e.activation(out=xn[:, :], in_=xt[:, :], func=AF.Copy, scale=stat[:, 3:4]),
             [x_tile_key, (kp, 'stat3')], [(kp, 'xn')])
        per = 16 // npass
        for ps_i in range(npass):
            def tfn(e, ps_i=ps_i):
                for k in range(per):
                    kc = ps_i * per + k
                    ins = e.transpose(tp[:, k * 128:(k + 1) * 128], xn[:, kc * 128:(kc + 1) * 128], identf[:, :])
                return ins
            P.op('pe', tfn, [(kp, 'xn'), 'consts'], [tp_key])

            def efn(e, ps_i=ps_i):
                for k in range(per):
                    kc = ps_i * per + k
                    ins = e.tensor_scalar(out=dst_fn(kc), in0=tp[:, k * 128:(k + 1) * 128],
                                          scalar1=gsc[:, kc * 3 + r:kc * 3 + r + 1], scalar2=sh[:, kc * 3 + r:kc * 3 + r + 1],
                                          op0=ALU.mult, op1=ALU.add)
                return ins
            P.op('dve', efn, [tp_key, ('gsc', l, which), ('sh', l, which)], [dst_key])

    def phase_norm1(l, s, hT, st):
        with contextlib.ExitStack() as st2:
            xt = [sb(f"n1xt{i}", [128, D], stack=st2) for i in range(2)]
            junk = [sb(f"n1junk{i}", [128, D], BF16, stack=st2) for i in range(2)]
            stat = [sb(f"n1stat{i}", [128, 4], stack=st2) for i in range(2)]
            xn = [sb(f"n1xn{i}", [128, D], stack=st2) for i in range(2)]
            tp = [ps(f"n1tp{i}", [128, D], stack=st2) for i in range(2)]
            for tt in range(NT):
                sl = tt % 2
                if l == 0:
                    src = ctxs[s * CTX + tt * 128: s * CTX + (tt + 1) * 128, :] if tt < 2 else \
                        xs[s * SEQ + (tt - 2) * 128: s * SEQ + (tt - 1) * 128, :]
                else:
                    src = x2_d[s * TOK + tt * 128: s * TOK + (tt + 1) * 128, :]
                r = 2 if tt < 2 else s
                P.dma('sp', [(xt[sl][:, :], src)], [], [('xt', sl)], f'ld_xt{sl}')
                norm_tile((junk[sl], stat[sl], xn[sl]), ('xt', sl), xt[sl], l, 0, r,
                          lambda kc, tt=tt: hT[:, kc, tt * 128:(tt + 1) * 128], ('hT', tt), tp[sl], ('tp', sl), kp=f'n{sl}')
            P.barrier()
            P.flush()

    BLKS = [(0, 256), (256, 768), (768, 1280), (1280, 1792), (1792, 2304)]

    def blk_tiles(t0, t1):
        return [('hT', t) for t in range(t0 // 128, t1 // 128)]

    def phase_even(s, hT):
        with contextlib.ExitStack() as st:
            w = sb("ew0", [128, 16, 5, 128], BF16, stack=st)
            lw = sb("elw", [128, 32, 128], BF16, stack=st)
            L = [sb(f"eL{i}", [128, TOK], (F32 if i == 4 else BF16), stack=st) for i in range(5)]
            B = [None if i == 3 else sb(f"eB{i}", [128, TOK], F32, stack=st) for i in range(7)]
            ubf = sb("eubf", [128, TOK], BF16, stack=st)
            outb = sb("eob0", [128, TOK], BF16, stack=st)
            pp = [ps(f"epp{i}", [128, 512], stack=st) for i in range(8)]
            P.dma('pool', [(lw[:, :, :], lruw.rearrange("(g i) j -> i g j", i=128))], [], ['lw'], 'ld_lw')
            bank = [0]

            def nextbank():
                b = bank[0]
                bank[0] = (b + 1) % 8
                return b
            SEGS = [(0, CTX), (CTX, TOK)]

            def load_w(j):
                pairs = []
                for sec in range(5):
                    col0 = sec * 1024 + j * 128
                    pairs.append((w[:, :, sec, :], wine[:, col0:col0 + 128].rearrange("(kc p) c -> p kc c", p=128)))
                P.dma('pool', pairs, [], ['w'], 'ld_ew0')

            def proj_groups(secs):
                out = []
                for sec in secs:
                    for (t0, t1) in BLKS:
                        def g(sec=sec, t0=t0, t1=t1):
                            b = nextbank()
                            n = t1 - t0

                            def fn(e):
                                for kc in range(16):
                                    ins = e.matmul(pp[b][:, 0:n], lhsT=w[:, kc, sec, :], rhs=hT[:, kc, t0:t1],
                                                   start=(kc == 0), stop=(kc == 15))
                                return ins
                            P.op('pe', fn, ['w'] + blk_tiles(t0, t1), [('pp', b)])
                            P.op('act', lambda e: e.activation(out=L[sec][:, t0:t1], in_=pp[b][:, 0:n], func=AF.Copy),
                                 [('pp', b)], [('L', sec)])
                        out.append(g)
                return out

            def early_chain(j):
                P.op('dve', lambda e: e.tensor_tensor(out=B[6][:, :], in0=L[3][:, :], in1=L[3][:, :], op=ALU.mult), [('L', 3)], [('B', 6)])
                P.op('dve', lambda e: e.tensor_scalar(out=B[6][:, :], in0=B[6][:, :], scalar1=0.044715, scalar2=1.0, op0=ALU.mult, op1=ALU.add),
                     [('B', 6)], [('B', 6)])
                P.op('dve', lambda e: e.tensor_tensor(out=B[6][:, :], in0=B[6][:, :], in1=L[3][:, :], op=ALU.mult), [('B', 6), ('L', 3)], [('B', 6)])
                P.op('act', lambda e: e.activation(out=B[6][:, :], in_=B[6][:, :], func=AF.Sigmoid, scale=1.5957691216057308),
                     [('B', 6)], [('B', 6)])
                P.op('dve', lambda e: e.tensor_tensor(out=L[3][:, :], in0=L[3][:, :], in1=B[6][:, :], op=ALU.mult), [('B', 6), ('L', 3)], [('L', 3)])
                P.op('dve', lambda e: e.tensor_tensor(out=B[1][:, :], in0=L[1][:, :], in1=L[2][:, :], op=ALU.mult), [('L', 1), ('L', 2)], [('B', 1)])
                P.op('dve', lambda e: e.tensor_scalar(out=B[2][:, :], in0=B[1][:, :], scalar1=caw[:, j * 3 + 1:j * 3 + 2], scalar2=None, op0=ALU.mult),
                     [('B', 1), 'consts'], [('B', 2)])
                for (a0, a1) in SEGS:
                    P.op('dve', lambda e, a0=a0, a1=a1: e.scalar_tensor_tensor(
                        out=B[2][:, a0 + 1:a1], in0=B[1][:, a0:a1 - 1], scalar=caw[:, j * 3:j * 3 + 1], in1=B[2][:, a0 + 1:a1],
                        op0=ALU.mult, op1=ALU.add), [('B', 1), ('B', 2)], [('B', 2)])
                    P.op('dve', lambda e, a0=a0, a1=a1: e.scalar_tensor_tensor(
                        out=B[2][:, a0:a1 - 1], in0=B[1][:, a0 + 1:a1], scalar=caw[:, j * 3 + 2:j * 3 + 3], in1=B[2][:, a0:a1 - 1],
                        op0=ALU.mult, op1=ALU.add), [('B', 1), ('B', 2)], [('B', 2)])
                P.op('pool', lambda e: e.tensor_tensor(out=outb[:, :], in0=L[0][:, :], in1=B[2][:, :], op=ALU.mult),
                     [('L', 0), ('B', 2)], ['ob'])
                P.dma('sp', [(mixT_d[(s * 16 + j) * 128:(s * 16 + j + 1) * 128, :], outb[:, :])], ['ob'], ['mixT_d'], 'st_ob0')
                P.op('dve', lambda e: e.tensor_scalar(out=B[5][:, :], in0=L[4][:, :], scalar1=cbw[:, j * 4 + 2:j * 4 + 3], scalar2=cbb[:, j:j + 1],
                                                      op0=ALU.mult, op1=ALU.add), [('L', 4), 'consts'], [('B', 5)])
                for (a0, a1) in SEGS:
                    for (kk, sh_) in ((0, 2), (1, 1)):
                        P.op('dve', lambda e, a0=a0, a1=a1, kk=kk, sh_=sh_: e.scalar_tensor_tensor(
                            out=B[5][:, a0 + sh_:a1], in0=L[4][:, a0:a1 - sh_], scalar=cbw[:, j * 4 + kk:j * 4 + kk + 1], in1=B[5][:, a0 + sh_:a1],
                            op0=ALU.mult, op1=ALU.add), [('L', 4), ('B', 5)], [('B', 5)])
                    P.op('dve', lambda e, a0=a0, a1=a1: e.scalar_tensor_tensor(
                        out=B[5][:, a0:a1 - 1], in0=L[4][:, a0 + 1:a1], scalar=cbw[:, j * 4 + 3:j * 4 + 4], in1=B[5][:, a0:a1 - 1],
                        op0=ALU.mult, op1=ALU.add), [('L', 4), ('B', 5)], [('B', 5)])
                P.op('pool', lambda e: e.tensor_copy(out=ubf[:, :], in_=B[5][:, :]), [('B', 5)], ['ubf'])

            def late_ops(j):
                ops = []
                for d in range(2):
                    for gate, dst in ((0, 0), (1, 1)):
                        g = (gate * 2 + d) * 8 + j
                        for (t0, t1) in BLKS:
                            def gg(g=g, t0=t0, t1=t1, dst=dst):
                                b = nextbank()
                                n = t1 - t0
                                P.op('pe', lambda e: e.matmul(pp[b][:, 0:n], lhsT=lw[:, g, :], rhs=ubf[:, t0:t1], start=True, stop=True),
                                     ['lw', 'ubf'], [('pp', b)])
                                P.op('act', lambda e: e.activation(out=B[dst][:, t0:t1], in_=pp[b][:, 0:n], func=AF.Sigmoid, bias=lrub[:, g:g + 1]),
                                     [('pp', b), 'consts'], [('B', dst)])
                            ops.append(gg)
                    cn = cneg[:, d * 8 + j:d * 8 + j + 1]
                    ops.append(lambda cn=cn: P.op('act', lambda e: e.activation(out=B[0][:, :], in_=B[0][:, :], func=AF.Exp, scale=cn), [('B', 0), 'cneg'], [('B', 0)]))
                    ops.append(lambda: P.op('act', lambda e: e.activation(out=B[2][:, :], in_=B[0][:, :], func=AF.Square), [('B', 0)], [('B', 2)]))
                    ops.append(lambda: P.op('act', lambda e: e.activation(out=B[2][:, :], in_=B[2][:, :], func=AF.Sqrt, scale=-1.0, bias=1.0), [('B', 2)], [('B', 2)]))
                    ops.append(lambda: P.op('dve', lambda e: e.tensor_tensor(out=B[1][:, :], in0=B[1][:, :], in1=B[5][:, :], op=ALU.mult), [('B', 1), ('B', 5)], [('B', 1)]))
                    ops.append(lambda: P.op('dve', lambda e: e.tensor_tensor(out=B[1][:, :], in0=B[1][:, :], in1=B[2][:, :], op=ALU.mult), [('B', 1), ('B', 2)], [('B', 1)]))
                    if d == 0:
                        ops.append(lambda: P.op('dve', lambda e: e.tensor_tensor_scan(out=B[4][:, :], data0=B[0][:, :], data1=B[1][:, :], initial=0.0,
                                                                                       op0=ALU.mult, op1=ALU.add), [('B', 0), ('B', 1)], [('B', 4)]))
                    else:
                        ops.append(lambda: P.op('dve', lambda e: e.tensor_tensor_scan(out=rev(B[6][:, 0:CTX]), data0=rev(B[0][:, 0:CTX]), data1=rev(B[1][:, 0:CTX]),
                                                                                       initial=0.0, op0=ALU.mult, op1=ALU.add), [('B', 0), ('B', 1)], [('B', 6)]))
                        ops.append(lambda: P.op('dve', lambda e: e.tensor_tensor_scan(out=rev(B[6][:, CTX:TOK]), data0=rev(B[0][:, CTX:TOK]), data1=rev(B[1][:, CTX:TOK]),
                                                                                       initial=B[6][:, 0:1], op0=ALU.mult, op1=ALU.add), [('B', 0), ('B', 1), ('B', 6)], [('B', 6)]))
                ops.append(lambda: P.op('pool', lambda e: e.tensor_tensor(out=B[4][:, :], in0=B[4][:, :], in1=B[6][:, :], op=ALU.add), [('B', 4), ('B', 6)], [('B', 4)]))
                ops.append(lambda: P.op('pool', lambda e: e.tensor_tensor(out=outb[:, :], in0=L[3][:, :], in1=B[4][:, :], op=ALU.mult),
                                        [('L', 3), ('B', 4)], ['ob']))
                ops.append(lambda: P.dma('sp', [(mixT_d[(s * 16 + 8 + j) * 128:(s * 16 + 9 + j) * 128, :], outb[:, :])], ['ob'], ['mixT_d'], 'st_ob0'))
                return ops

            load_w(0)
            for g in proj_groups((0, 1, 2, 4, 3)):
                g()
            for j in range(8):
                early_chain(j)
                late = late_ops(j)
                if j + 1 < 8:
                    load_w(j + 1)
                    pg = proj_groups((0, 1, 2, 4))
                    pg3 = proj_groups((3,))
                else:
                    pg, pg3 = [], []
                for op_ in late:
                    op_()
                    if pg:
                        pg.pop(0)()
                for g in pg:
                    g()
                for g in pg3:
                    g()
            P.barrier()
            P.flush()

    def phase_outproj(l, s):
        ntile = NT if l == 0 else 16
        with contextlib.ExitStack() as st:
            wo = sb("owo", [128, 16, D], BF16, stack=st)
            aTt = [sb(f"oaT{i}", [128, 16, 128], BF16, stack=st) for i in range(2)]
            g1b = [sb(f"og1b{i}", [128, D], stack=st) for i in range(2)]
            xt = [sb(f"oxt{i}", [128, D], stack=st) for i in range(2)]
            xn = [sb(f"oxn{i}", [128, D], stack=st) for i in range(2)]
            stat = [sb(f"ostat{i}", [128, 4], stack=st) for i in range(2)]
            f32t = [sb(f"of32t{i}", [128, 16, 128], stack=st) for i in range(2)]
            fTt = [sb(f"ofTt{i}", [128, 16, 128], BF16, stack=st) for i in range(2)]
            scores = sb("oscores", [128, NT, 4, 4], stack=st)
            yp = ps("oyp", [128, D], stack=st)
            tp = ps("otp", [128, 1024], stack=st)
            lg = ps("olg", [128, 512], stack=st)
            wsrc = (woute if l == 0 else wouto).rearrange("(kc p) c -> p kc c", p=128)
            P.dma('pool', [(wo[:, kc, :], wsrc[:, kc, :]) for kc in range(16)], [], ['wo'], 'ld_wo')
            rows = [(l * 2 + 0) * 3 + 2, (l * 2 + 0) * 3 + s]
            P.dma('sp', [(g1b[i][:, :], grow[rows[i]:rows[i] + 1, :].partition_broadcast(128)) for i in range(2)], [], ['g1b'], 'ld_g1b')
            fT3 = fT_d[s * 128:(s + 1) * 128, :].rearrange("p (kc t) -> p kc t", kc=16)
            asrc = mixT_d[s * 2048:(s + 1) * 2048, :].rearrange("(c p) t -> p c t", p=128)
            ada_state = ada_begin(1, st) if (l == 0 and s == 0 and EMBED_ADA1) else None
            for tt in range(ntile):
                sl = tt % 2
                kp = f'o{sl}'
                if ada_state is not None:
                    for pc in (2 * tt, 2 * tt + 1):
                        if pc < 32:
                            ada_piece(ada_state, pc)
                if l == 0:
                    isctx = tt < 2
                    src = ctxs[s * CTX + tt * 128: s * CTX + (tt + 1) * 128, :] if isctx else \
                        xs[s * SEQ + (tt - 2) * 128: s * SEQ + (tt - 1) * 128, :]
                    dst = x1_d[s * TOK + tt * 128: s * TOK + (tt + 1) * 128, :]
                else:
                    isctx = False
                    src = x2_d[s * TOK + CTX + tt * 128: s * TOK + CTX + (tt + 1) * 128, :]
                    dst = x3_d[s * SEQ + tt * 128: s * SEQ + (tt + 1) * 128, :]
                ftcol = tt * 128
                r = 2 if isctx else s
                gb_ = g1b[0] if isctx else g1b[1]
                a_, xt_, xn_, f32_, fT_, stat_ = aTt[sl], xt[sl], xn[sl], f32t[sl], fTt[sl], stat[sl]
                junk = fT_[:, :, :].rearrange("p a b -> p (a b)")
                P.dma('sp', [(a_[:, :, :], asrc[:, :, tt * 128:(tt + 1) * 128])], [], [('aT', sl)], f'ld_aT{sl}')
                P.dma('sp', [(xt_[:, :], src)], [], [('xt', sl)], f'ld_oxt{sl}')

                def yfn(e, a_=a_):
                    for cb in range(4):
                        for kc in range(16):
                            ins = e.matmul(yp[:, cb * 512:(cb + 1) * 512], lhsT=a_[:, kc, :],
                                           rhs=wo[:, kc, cb * 512:(cb + 1) * 512], start=(kc == 0), stop=(kc == 15))
                    return ins
                P.op('pe', yfn, [('aT', sl), 'wo'], ['yp'])
                P.op('dve', lambda e, gb_=gb_, xn_=xn_: e.tensor_tensor(out=xn_[:, :], in0=yp[:, :], in1=gb_[:, :], op=ALU.mult), ['yp', 'g1b'], [(kp, 'xn')])
                P.op('pool', lambda e, xn_=xn_, xt_=xt_: e.tensor_tensor(out=xt_[:, :], in0=xn_[:, :], in1=xt_[:, :], op=ALU.add), [(kp, 'xn'), ('xt', sl)], [('xt', sl)])
                P.dma('sp', [(dst, xt_[:, :])], [('xt', sl)], [('x1_d', sl)], f'st_x1{sl}')
                norm_tile((junk, stat_, xn_), ('xt', sl), xt_, l, 1, r, lambda kc, f32_=f32_: f32_[:, kc, :], ('f32t', sl), tp, 'tp', npass=2,
                          junk_key='fTt', kp=kp)
                P.op('pool', lambda e, fT_=fT_, f32_=f32_: e.tensor_copy(out=fT_[:, :, :], in_=f32_[:, :, :]), [('f32t', sl)], [(kp, 'fTt')])
                P.dma('sp', [(fT3[:, :, ftcol:ftcol + 128], fT_[:, :, :])], [(kp, 'fTt')], [('fT_d', sl)], f'st_fT{sl}')

                def lfn(e, f32_=f32_):
                    for kc in range(16):
                        ins = e.matmul(lg[:, 0:16], lhsT=f32_[:, kc, :], rhs=rwt[:, kc, :], start=(kc == 0), stop=(kc == 15))
                    return ins
                P.op('pe', lfn, [('f32t', sl), 'consts'], ['lg'])
                P.op('act', lambda e, tt=tt: e.activation(out=scores[:, tt, :, :].rearrange("p a b -> p (a b)"), in_=lg[:, 0:16], func=AF.Sigmoid),
                     ['lg'], ['scores'])
            if ada_state is not None:
                ada_finish(ada_state)
            routing(st, scores, ntile, s)
            with nc.allow_non_contiguous_dma(reason="tiny modulation rows"):
                P.barrier()
                P.flush()

    def routing(st, scores, T, s):
        sel = sb("r_sel", [128, NT, 4, 4], stack=st)
        sel2 = sb("r_sel2", [128, NT, 4, 4], stack=st)
        m1k = sb("r_m1k", [128, NT, 4, 4], stack=st)
        m2k = sb("r_m2k", [128, NT, 4, 4], stack=st)
        t1 = sb("r_t1", [128, NT, 4], stack=st)
        gs = sb("r_gs", [128, NT, 4], stack=st)
        gmask = sb("r_gmask", [128, NT, 4], stack=st)
        red = sb("r_red", [128, NT], stack=st)
        k = ['rt']

        def V(fn):
            P.op('dve', fn, ['scores', 'consts'] + k, k)
        sc_ = scores[:, 0:T]
        sl_, s2_, a1_, a2_ = sel[:, 0:T], sel2[:, 0:T], m1k[:, 0:T], m2k[:, 0:T]
        t1_, gs_, gm_, rd_ = t1[:, 0:T], gs[:, 0:T], gmask[:, 0:T], red[:, 0:T]

        def f16(a):
            return a.rearrange("p t a b -> p t (a b)")
        V(lambda e: e.tensor_tensor(out=f16(sl_), in0=f16(sc_), in1=bc_mid(rbb[:, :], T), op=ALU.add))
        pairs = [(0, 1), (0, 2), (0, 3), (1, 2), (1, 3), (2, 3)]
        for i, (a, b) in enumerate(pairs):
            dst = gs_ if i == 0 else t1_
            V(lambda e, a=a, b=b, dst=dst: e.tensor_tensor(out=dst, in0=sl_[:, :, :, a], in1=sl_[:, :, :, b], op=ALU.add))
            if i > 0:
                V(lambda e: e.tensor_tensor(out=gs_, in0=gs_, in1=t1_, op=ALU.max))
        V(lambda e: e.tensor_reduce(out=rd_, in_=gs_, axis=AX.X, op=ALU.max))
        V(lambda e: e.tensor_tensor(out=gm_, in0=gs_, in1=bc(rd_, 4), op=ALU.is_equal))
        V(lambda e: e.tensor_scalar(out=f16(s2_), in0=f16(sl_), scalar1=2.0, scalar2=None, op0=ALU.add))
        V(lambda e: e.tensor_tensor(out=s2_, in0=s2_, in1=bc(gm_, 4), op=ALU.mult))
        V(lambda e: e.tensor_reduce(out=rd_, in_=f16(s2_), axis=AX.X, op=ALU.max))
        V(lambda e: e.tensor_tensor(out=f16(a1_), in0=f16(s2_), in1=bc(rd_, 16), op=ALU.is_equal))
        V(lambda e: e.scalar_tensor_tensor(out=f16(s2_), in0=f16(a1_), scalar=-4.0, in1=f16(s2_), op0=ALU.mult, op1=ALU.add))
        V(lambda e: e.tensor_reduce(out=rd_, in_=f16(s2_), axis=AX.X, op=ALU.max))
        V(lambda e: e.tensor_tensor(out=f16(a2_), in0=f16(s2_), in1=bc(rd_, 16), op=ALU.is_equal))
        V(lambda e: e.tensor_tensor(out=f16(a1_), in0=f16(a1_), in1=f16(a2_), op=ALU.add))
        V(lambda e: e.tensor_tensor(out=f16(a1_), in0=f16(a1_), in1=f16(sc_), op=ALU.mult))
        V(lambda e: e.tensor_reduce(out=rd_, in_=f16(a1_), axis=AX.X, op=ALU.add))
        V(lambda e: e.reciprocal(out=rd_, in_=rd_))
        P.op('dve', lambda e: e.tensor_tensor(out=comb[:, s, 0:T, :], in0=f16(a1_), in1=bc(rd_, 16), op=ALU.mult), k, [('comb', s)])

    def phase_moe(l, s):
        if l == 0:
            TB, NBK, NSUB, SUBW, ntok = 1152, 2, 3, 384, TOK
        else:
            TB, NBK, NSUB, SUBW, ntok = 1024, 2, 2, 512, SEQ
        ntl = TB // 128
        with contextlib.ExitStack() as st:
            acc = sb("macc", [128, ntl, D], stack=st)
            fT = sb("mfT", [128, 16, TB], BF16, stack=st)
            WGU = [(sb(f"mwg{i}", [128, 16, 256], BF16, stack=st), sb(f"mwu{i}", [128, 16, 256], BF16, stack=st)) for i in range(2)]
            WD = [sb(f"mwd{i}", [128, 2, D], BF16, stack=st) for i in range(3)]
            hid = [sb(f"mhid{i}", [128, 2, TB], BF16, stack=st) for i in range(2)]
            sg = [sb(f"msg{i}", [128, 512], stack=st) for i in range(2)]
            g2b = [sb(f"mg2b{i}", [128, D], stack=st) for i in range(2)]
            stat = sb("mstat", [128, 8], stack=st)
            if l == 1:
                junk = sb("mjunk", [128, D], BF16, stack=st)
                fgb = sb("mfgb", [128, D], stack=st)
            gps = [ps(f"mgps{i}", [128, 512], stack=st) for i in range(2)]
            ups = [ps(f"mups{i}", [128, 512], stack=st) for i in range(2)]
            ops_ = [ps(f"mops{i}", [128, 1024], stack=st) for i in range(2)]
            rows = [(l * 2 + 1) * 3 + 2, (l * 2 + 1) * 3 + s]
            P.dma('sp', [(g2b[i][:, :], grow[rows[i]:rows[i] + 1, :].partition_broadcast(128)) for i in range(2)], [], ['g2b'], 'ld_g2b')
            if l == 1:
                P.dma('sp', [(fgb[:, :], fgrow.partition_broadcast(128))], [], ['fgb'], 'ld_fgb')
            fT3 = fT_d[s * 128:(s + 1) * 128, :].rearrange("p (kc t) -> p kc t", kc=16)
            ucount = [0]
            gi = [0]
            for bk in range(NBK):
                tok0 = bk * TB
                P.dma('sp', [(fT[:, :, 0:TB], fT3[:, :, tok0:tok0 + TB])], [], ['fT'], 'ld_mfT')
                prev_wd = []
                for ex in range(NE):
                    for q in range(4):
                        u = ucount[0]
                        ucount[0] += 1
                        sl = u % 2
                        sl3 = u % 3
                        wg, wu = WGU[sl]
                        wd = WD[sl3]
                        rg = (l * NE + ex) * D
                        rd = (l * NE + ex) * FF + q * 256
                        P.dma('pool', [(wg[:, :, :], ewg[rg:rg + D, q * 256:(q + 1) * 256].rearrange("(kc p) c -> p kc c", p=128)),
                                       (wu[:, :, :], ewu[rg:rg + D, q * 256:(q + 1) * 256].rearrange("(kc p) c -> p kc c", p=128))],
                              [], [('Wg', sl), ('Wu', sl)], f'ld_mw{sl}')
                        P.dma('pool', [(wd[:, :, :], ewd[rd:rd + 256, :].rearrange("(fc p) c -> p fc c", p=128))],
                              [], [('Wd', sl3)], f'ld_md{sl3}')
                        hs = hid[sl]

                        def gu_item(fc, nb, sl=sl, wg=wg, wu=wu, hs=hs):
                            i = gi[0] % 2
                            gi[0] += 1
                            c0 = nb * SUBW

                            def gfn(e):
                                for kc in range(16):
                                    e.matmul(gps[i][:, 0:SUBW], lhsT=wg[:, kc, fc * 128:(fc + 1) * 128], rhs=fT[:, kc, c0:c0 + SUBW],
                                             start=(kc == 0), stop=(kc == 15))
                                for kc in range(16):
                                    ins = e.matmul(ups[i][:, 0:SUBW], lhsT=wu[:, kc, fc * 128:(fc + 1) * 128], rhs=fT[:, kc, c0:c0 + SUBW],
                                                   start=(kc == 0), stop=(kc == 15))
                                return ins
                            P.op('pe', gfn, [('Wg', sl), ('Wu', sl), 'fT'], [('gps', i), ('ups', i)])
                            P.op('act', lambda e: e.activation(out=sg[i][:, 0:SUBW], in_=gps[i][:, 0:SUBW], func=AF.Silu),
                                 [('gps', i)], [('sg', i)])
                            P.op('dve', lambda e: e.tensor_tensor(out=hs[:, fc, c0:c0 + SUBW], in0=ups[i][:, 0:SUBW],
                                                                  in1=sg[i][:, 0:SUBW], op=ALU.mult),
                                 [('ups', i), ('sg', i)], [('hid', sl)])

                        def wd_item(tt, half, sl=sl, sl3=sl3, hs=hs, wd=wd, ex=ex, first=(ex == 0 and q == 0)):
                            gt = bk * ntl + tt

                            def ofn(e):
                                for fc in range(2):
                                    for c2 in range(2):
                                        cb = half * 2 + c2
                                        ins = e.matmul(ops_[half][:, c2 * 512:(c2 + 1) * 512], lhsT=hs[:, fc, tt * 128:(tt + 1) * 128],
                                                       rhs=wd[:, fc, cb * 512:(cb + 1) * 512], start=(fc == 0), stop=(fc == 1))
                                return ins
                            P.op('pe', ofn, [('hid', sl), ('Wd', sl3)], [('ops', half)])
                            if first:
                                P.op('dve', lambda e: e.tensor_scalar(
                                    out=acc[:, tt, half * 1024:(half + 1) * 1024], in0=ops_[half][:, :], scalar1=comb[:, s, gt, ex:ex + 1],
                                    scalar2=None, op0=ALU.mult),
                                    [('ops', half), ('comb', s)], [('acc', tt, half)])
                            else:
                                P.op('dve', lambda e: e.scalar_tensor_tensor(
                                    out=acc[:, tt, half * 1024:(half + 1) * 1024], in0=ops_[half][:, :], scalar=comb[:, s, gt, ex:ex + 1],
                                    in1=acc[:, tt, half * 1024:(half + 1) * 1024], op0=ALU.mult, op1=ALU.add),
                                    [('ops', half), ('comb', s), ('acc', tt, half)], [('acc', tt, half)])
                        gu_list = [(fc, nb) for fc in range(2) for nb in range(NSUB)]
                        per = -(-len(prev_wd) // len(gu_list)) if prev_wd else 0
                        for (fc, nb) in gu_list:
                            gu_item(fc, nb)
                            for _ in range(per):
                                if prev_wd:
                                    prev_wd.pop(0)()
                        while prev_wd:
                            prev_wd.pop(0)()
                        prev_wd = [(lambda tt=tt, half=half, f=wd_item: f(tt, half)) for tt in range(ntl) for half in range(2)]
                while prev_wd:
                    prev_wd.pop(0)()
                sl_last = (ucount[0] - 1) % 2
                xbuf = [WGU[sl_last][i][:, :, :].bitcast(F32).rearrange("p a b -> p (a b)") for i in range(2)]
                for tt in range(ntl):
                    gt = bk * ntl + tt
                    xi = tt % 2
                    xb_ = xbuf[xi]
                    gk = ('Wg', sl_last) if xi == 0 else ('Wu', sl_last)
                    if l == 0:
                        isctx = gt < 2
                        src = x1_d[s * TOK + gt * 128: s * TOK + (gt + 1) * 128, :]
                        dst = x2_d[s * TOK + gt * 128: s * TOK + (gt + 1) * 128, :]
                    else:
                        isctx = False
                        src = x3_d[s * SEQ + gt * 128: s * SEQ + (gt + 1) * 128, :]
                        dst = outd[s * SEQ + gt * 128: s * SEQ + (gt + 1) * 128, :]
                    gb_ = g2b[0] if isctx else g2b[1]
                    P.dma('sp', [(xb_, src)], [], [('x1t', xi), gk], f'ld_mx{xi}')
                    P.op('dve', lambda e, tt=tt, gb_=gb_: e.tensor_tensor(out=acc[:, tt, :], in0=acc[:, tt, :], in1=gb_[:, :], op=ALU.mult),
                         [('acc', tt, 0), ('acc', tt, 1), 'g2b'], [('acc', tt, 0), ('acc', tt, 1)])
                    P.op('pool', lambda e, tt=tt, xb_=xb_: e.tensor_tensor(out=xb_, in0=acc[:, tt, :], in1=xb_, op=ALU.add),
                         [('acc', tt, 0), ('acc', tt, 1), ('x1t', xi)], [('x1t', xi)])
                    if l == 1:
                        P.op('act', lambda e, xb_=xb_, xi=xi: e.activation(out=junk[:, :], in_=xb_, func=AF.Square, accum_out=stat[:, xi * 4:xi * 4 + 1]),
                             [('x1t', xi)], ['junk', ('st0', xi)])
                        P.op('act', lambda e, xi=xi: e.activation(out=stat[:, xi * 4 + 1:xi * 4 + 2], in_=stat[:, xi * 4:xi * 4 + 1], func=AF.Ln, scale=1.0 / D, bias=EPS),
                             [('st0', xi)], [('st1', xi)])
                        P.op('act', lambda e, xi=xi: e.activation(out=stat[:, xi * 4 + 2:xi * 4 + 3], in_=stat[:, xi * 4 + 1:xi * 4 + 2], func=AF.Exp, scale=-0.5),
                             [('st1', xi)], [('st2', xi)])
                        P.op('act', lambda e, xb_=xb_, xi=xi: e.activation(out=xb_, in_=xb_, func=AF.Copy, scale=stat[:, xi * 4 + 2:xi * 4 + 3]),
                             [('x1t', xi), ('st2', xi)], [('x1t', xi)])
                        P.op('pool', lambda e, xb_=xb_: e.tensor_tensor(out=xb_, in0=xb_, in1=fgb[:, :], op=ALU.mult), [('x1t', xi), 'fgb'], [('x1t', xi)])
                    P.dma('sp', [(dst, xb_)], [('x1t', xi), gk], [('mout', xi)], f'st_mo{xi}')
            P.barrier()
            P.flush()

    def phase_attn(s, hT):
        SCALE = 128 ** -0.5
        with contextlib.ExitStack() as st:
            w = [sb(f"aw{i}", [128, 16, 768], BF16, stack=st) for i in range(2)]
            qT = sb("aqT", [128, 2, SEQ], BF16, stack=st)
            kT = sb("akT", [128, 2, TOK], BF16, stack=st)
            V_ = sb("aV", [128, NT, 256], BF16, stack=st)
            cosT = sb("acos", [128, SEQ], stack=st)
            sinT = sb("asin", [128, SEQ], stack=st)
            qb16 = sb("aqb16", [128, 512], BF16, stack=st)
            t1 = sb("at1", [128, 512], stack=st)
            t2 = sb("at2", [128, 512], stack=st)
            PT = [sb(f"aPT{i}", [128, 512], BF16, stack=st) for i in range(2)]
            rs = sb("ars", [128, 512], stack=st)
            oc = [sb(f"aoc{i}", [128, 512], stack=st) for i in range(2)]
            otmp = sb("aotmp", [128, 512], stack=st)
            sq = [sb(f"asq{i}", [128, 512], stack=st) for i in range(2)]
            ofst = [sb(f"aofst{i}", [128, 512], BF16, stack=st) for i in range(2)]
            A = [ps(f"aA{i}", [128, 512], stack=st) for i in range(2)]
            O = [[ps(f"aO{i}_{c}", [128, 512], stack=st) for c in range(3)] for i in range(2)]
            P.dma('sp', [(cosT[:, :], ropec), (sinT[:, :], ropes)], [], ['rope'], 'ld_rope')
            ai = [0]

            def nextA():
                a = ai[0] % 2
                ai[0] += 1
                return a
            oset = [0]
            LATB = [(0, 512), (512, 1024), (1024, 1536), (1536, 2048)]
            ATT_STAGE = int(os.environ.get('ATT_STAGE', '9'))
            for hd in range(int(os.environ.get('ATT_HEADS', '8'))):
                sl = hd % 2
                P.dma('pool', [(w[sl][:, :, sec * 256:(sec + 1) * 256],
                                wino[:, sec * 2048 + hd * 256: sec * 2048 + (hd + 1) * 256].rearrange("(kc p) c -> p kc c", p=128))
                               for sec in range(3)], [], [('w', sl)], f'ld_aw{sl}')
                for sec, dstT in ((0, qT), (1, kT)):
                    for m in range(2):
                        wc0 = sec * 256 + m * 128
                        if sec == 1:
                            a = nextA()

                            def fnc(e, a=a, wc0=wc0, sl=sl):
                                for kc in range(16):
                                    ins = e.matmul(A[a][:, 0:256], lhsT=w[sl][:, kc, wc0:wc0 + 128], rhs=hT[:, kc, 0:256], start=(kc == 0), stop=(kc == 15))
                                return ins
                            P.op('pe', fnc, [('w', sl), ('hT', 0), ('hT', 1)], [('A', a)])
                            P.op('act', lambda e, a=a, m=m: e.activation(out=kT[:, m, 0:256], in_=A[a][:, 0:256], func=AF.Copy), [('A', a)], [('kT', m)])
                        for (l0, l1) in LATB:
                            a = nextA()
                            a2 = nextA()
                            toff = 0 if sec == 0 else CTX

                            def fnp(e, a=a, wc0=wc0, sl=sl, l0=l0, l1=l1):
                                for kc in range(16):
                                    ins = e.matmul(A[a][:, :], lhsT=w[sl][:, kc, wc0:wc0 + 128], rhs=hT[:, kc, CTX + l0:CTX + l1], start=(kc == 0), stop=(kc == 15))
                                return ins
                            P.op('pe', fnp, [('w', sl)] + blk_tiles(CTX + l0, CTX + l1), [('A', a)])
                            P.op('act', lambda e, a=a: e.activation(out=qb16[:, :], in_=A[a][:, :], func=AF.Copy), [('A', a)], ['qb16'])
                            P.op('dve', lambda e, a=a, l0=l0, l1=l1: e.tensor_tensor(out=t1[:, :], in0=A[a][:, :], in1=cosT[:, l0:l1], op=ALU.mult),
                                 [('A', a), 'rope', 'qb16'], ['t1'])
                            P.op('pe', lambda e, a2=a2: e.matmul(A[a2][:, :], lhsT=rpermb[:, :], rhs=qb16[:, :], start=True, stop=True),
                                 ['qb16', 'rpermb'], [('A', a2)])
                            P.op('dve', lambda e, a2=a2, l0=l0, l1=l1: e.tensor_tensor(out=t2[:, :], in0=A[a2][:, :], in1=sinT[:, l0:l1], op=ALU.mult),
                                 [('A', a2), 'rope'], ['t2'])
                            P.op('pool', lambda e, dstT=dstT, m=m, toff=toff, l0=l0, l1=l1: e.tensor_tensor(
                                out=dstT[:, m, toff + l0:toff + l1], in0=t1[:, :], in1=t2[:, :], op=ALU.add),
                                ['t1', 't2'], [('qk', sec, m)] if sec == 0 else [('kT', m)])
                for tt in range(NT):
                    a = nextA()

                    def fnv(e, a=a, tt=tt, sl=sl):
                        for kc in range(16):
                            ins = e.matmul(A[a][:, 0:256], lhsT=hT[:, kc, tt * 128:(tt + 1) * 128], rhs=w[sl][:, kc, 512:768], start=(kc == 0), stop=(kc == 15))
                        return ins
                    P.op('pe', fnv, [('w', sl), ('hT', tt)], [('A', a)])
                    P.op('act', lambda e, a=a, tt=tt: e.activation(out=V_[:, tt, :], in_=A[a][:, 0:256], func=AF.Copy), [('A', a)], ['V'])
                for qb in range(4 if ATT_STAGE >= 2 else 0):
                    q0 = qb * 512
                    for m in range(2):
                        os_ = oset[0] % 2
                        oset[0] += 1
                        Oc = O[os_]
                        def emitS(kt, m=m, q0=q0):
                            a = nextA()
                            P.op('pe', lambda e, a=a, m=m, kt=kt, q0=q0: e.matmul(A[a][:, :], lhsT=kT[:, m, kt * 128:(kt + 1) * 128], rhs=qT[:, m, q0:q0 + 512],
                                                                                    start=True, stop=True), [('kT', m), ('qk', 0, m)], [('A', a)])
                            return a
                        a_next = emitS(0)
                        for kt in range(NT):
                            a = a_next
                            if kt + 1 < NT:
                                a_next = emitS(kt + 1)
                            P.op('act', lambda e, a=a: e.activation(out=PT[a][:, :], in_=A[a][:, :], func=AF.Exp, scale=SCALE), [('A', a)], [('PT', a)])

                            def fno(e, a=a, kt=kt, Oc=Oc):
                                e.matmul(Oc[0][:, :], lhsT=V_[:, kt, 0:128], rhs=PT[a][:, :], start=(kt == 0), stop=(kt == NT - 1))
                                e.matmul(Oc[1][:, :], lhsT=V_[:, kt, 128:256], rhs=PT[a][:, :], start=(kt == 0), stop=(kt == NT - 1))
                                return e.matmul(Oc[2][:, :], lhsT=onesb[:, :], rhs=PT[a][:, :], start=(kt == 0), stop=(kt == NT - 1))
                            P.op('pe', fno, ['V', ('PT', a), 'onesb'], [('O', os_)])
                        P.op('dve', lambda e, Oc=Oc: e.reciprocal(out=rs[:, :], in_=Oc[2][:, :]), [('O', os_)], ['rs'])
                        if m == 0:
                            for c in range(2):
                                P.op('dve', lambda e, c=c, Oc=Oc: e.tensor_tensor(out=oc[c][:, :], in0=Oc[c][:, :], in1=rs[:, :], op=ALU.mult),
                                     [('O', os_), 'rs'], [('oc', c)])
                        else:
                            P.op('dve', lambda e: e.tensor_scalar(out=rs[:, :], in0=rs[:, :], scalar1=neglam[:, 0:1], scalar2=None, op0=ALU.mult),
                                 ['rs', 'neglam'], ['rs'])
                            for c in range(2):
                                P.op('dve', lambda e, c=c, Oc=Oc: e.tensor_tensor(out=otmp[:, :], in0=Oc[c][:, :], in1=rs[:, :], op=ALU.mult),
                                     [('O', os_), 'rs'], ['otmp'])
                                P.op('pool', lambda e, c=c: e.tensor_tensor(out=oc[c][:, :], in0=oc[c][:, :], in1=otmp[:, :], op=ALU.add),
                                     [('oc', c), 'otmp'], [('oc', c)])
                    if ATT_STAGE < 3:
                        continue
                    for c in range(2):
                        P.op('act', lambda e, c=c: e.activation(out=sq[c][:, :], in_=oc[c][:, :], func=AF.Square), [('oc', c)], [('sq', c)])
                    a = nextA()

                    def fnn(e, a=a):
                        e.matmul(A[a][:, :], lhsT=onesf[:, :], rhs=sq[0][:, :], start=True, stop=False)
                        return e.matmul(A[a][:, :], lhsT=onesf[:, :], rhs=sq[1][:, :], start=False, stop=True)
                    P.op('pe', fnn, [('sq', 0), ('sq', 1), 'onesf'], [('A', a)])
                    P.op('dve', lambda e, a=a: e.tensor_scalar(out=rs[:, :], in0=A[a][:, :], scalar1=1.0 / 256, scalar2=EPS, op0=ALU.mult, op1=ALU.add),
                         [('A', a)], ['rs'])
                    P.op('act', lambda e: e.activation(out=rs[:, :], in_=rs[:, :], func=AF.Sqrt), ['rs'], ['rs'])
                    P.op('dve', lambda e: e.reciprocal(out=rs[:, :], in_=rs[:, :]), ['rs'], ['rs'])
                    for c in range(2):
                        P.op('dve', lambda e, c=c: e.scalar_tensor_tensor(out=ofst[c][:, :], in0=oc[c][:, :], scalar=subg[:, c:c + 1], in1=rs[:, :],
                                                                          op0=ALU.mult, op1=ALU.mult), [('oc', c), 'rs', 'consts'], [('ofst', c)])
                        ch = hd * 2 + c
                        P.dma('sp', [(mixT_d[(s * 16 + ch) * 128:(s * 16 + ch + 1) * 128, q0:q0 + 512], ofst[c][:, :])], [('ofst', c)], ['mixT_d'], f'st_of{c}')
            P.barrier()
            P.flush()

    stages = []
    phase_setup()
    stages.append('setup')
    done = [False]

    def chk(name):
        if stop_after == name:
            done[0] = True
        return done[0]

    for l in range(start_layer, 2):
        if done[0]:
            break
        if not (l == 1 and EMBED_ADA1 and start_layer == 0):
            phase_ada(l)
        if chk(f'ada{l}'):
            break
        for s in range(2):
            with contextlib.ExitStack() as sth:
                hT = sb(f"hT_{l}_{s}", [128, 16, TOK], BF16, stack=sth)
                phase_norm1(l, s, hT, sth)
                if chk(f'norm1_{l}_{s}'):
                    break
                if l == 0:
                    phase_even(s, hT)
                else:
                    phase_attn(s, hT)
            if chk(f'mix_{l}_{s}'):
                break
            phase_outproj(l, s)
            if chk(f'outproj_{l}_{s}'):
                break
            phase_moe(l, s)
            if chk(f'moe_{l}_{s}'):
                break
    P.barrier()
    P.flush()
    es.close()
    return nc


def _fm(v, n=16):
    return np.ascontiguousarray(np.asarray(v, np.float32).reshape(n, 128).T)


def prep_inputs(inputs):
    g = {k: np.asarray(v) for k, v in inputs.items()}
    rep = {}
    rep['adaw'] = np.ascontiguousarray(g['ada_w'].reshape(2 * D, 6 * D))
    rep['adabT'] = np.ascontiguousarray(np.concatenate([_fm(g['ada_b'][l], 96) for l in range(2)], axis=1))
    rep['ngT'] = np.ascontiguousarray(np.concatenate([_fm(g['norm_mix_g'][0]), _fm(g['norm_ffn_g'][0]),
                                                       _fm(g['norm_mix_g'][1]), _fm(g['norm_ffn_g'][1])], axis=1))
    rep['fgrow'] = np.ascontiguousarray(g['final_g'].reshape(1, D))
    rep['wine'] = np.ascontiguousarray(g['w_in_e'][0])
    rep['cawT'] = np.ascontiguousarray(g['conv_a_w'][0].reshape(3, 8, 128).transpose(2, 1, 0).reshape(128, 24))
    rep['cbwT'] = np.ascontiguousarray(g['conv_b_w'][0].reshape(4, 8, 128).transpose(2, 1, 0).reshape(128, 32))
    rep['cbbT'] = _fm(g['conv_b_b'][0], 8)
    rep['lruw'] = np.ascontiguousarray(np.stack([g['lru_wa'][0], g['lru_wi'][0]], axis=0).reshape(4096, 128))
    rep['lrubT'] = np.ascontiguousarray(np.stack([g['lru_ba'][0], g['lru_bi'][0]], axis=0).reshape(2, 2, 8, 128).transpose(3, 0, 1, 2).reshape(128, 32))
    rep['lamT'] = np.ascontiguousarray(g['lru_lam'][0].reshape(2, 8, 128).transpose(2, 0, 1).reshape(128, 16))
    rep['woute'] = np.ascontiguousarray(g['w_out_e'][0])
    rep['wino'] = np.ascontiguousarray(g['w_in_o'][0])
    rep['lamv'] = np.ascontiguousarray(np.concatenate([g['lam_q1'][0], g['lam_k1'][0], g['lam_q2'][0], g['lam_k2'][0]]).reshape(1, 512))
    rep['sublnT'] = _fm(g['subln_g'][0], 2)
    rep['wouto'] = np.ascontiguousarray(g['w_out_o'][0])
    rep['rw'] = np.ascontiguousarray(g['router_w'])
    rep['rb'] = np.ascontiguousarray(g['router_b'].reshape(1, NE))
    rep['ewg'] = np.ascontiguousarray(g['exp_w_gate'].reshape(2 * NE * D, FF))
    rep['ewu'] = np.ascontiguousarray(g['exp_w_up'].reshape(2 * NE * D, FF))
    rep['ewd'] = np.ascontiguousarray(g['exp_w_down'].reshape(2 * NE * FF, D))
    rep['identd'] = np.eye(128, dtype=np.float32)
    t = np.arange(SEQ)
    row = (t // 64).astype(np.float32)
    col = (t % 64).astype(np.float32)
    inv = (10000.0 ** (-np.arange(0, 64, 2, dtype=np.float32) / 64)).astype(np.float32)
    ang_r = row[:, None] * inv
    ang_c = col[:, None] * inv
    ang = np.concatenate([ang_r, ang_r, ang_c, ang_c], axis=-1)
    rep['ropec'] = np.ascontiguousarray(np.cos(ang).astype(np.float32).T)
    rep['ropes'] = np.ascontiguousarray(np.sin(ang).astype(np.float32).T)
    rp = np.zeros((128, 128), np.float32)
    for m in range(128):
        if (m % 64) < 32:
            rp[m + 32, m] = -1.0
        else:
            rp[m - 32, m] = 1.0
    rep['rpermd'] = rp
    maps = []
    for c in range(8):
        mp = dict(rep)
        mp['xs'] = np.ascontiguousarray(g['x'][2 * c:2 * c + 2].reshape(2 * SEQ, D))
        mp['ctxs'] = np.ascontiguousarray(g['ctx'][2 * c:2 * c + 2].reshape(2 * CTX, D))
        c3 = np.stack([g['c'][2 * c], g['c'][2 * c + 1], g['c_ctx']], axis=0)
        mp['c3T'] = np.ascontiguousarray(c3.reshape(3, 16, 128).transpose(2, 1, 0).reshape(128, 48))
        maps.append(mp)
    return maps


def kernel(**inputs):
    maps = prep_inputs(inputs)
    nc = build()
    res = run_bass_kernel_spmd(nc, maps, core_ids=list(range(8)))
    out = np.stack([r["out"].reshape(2, SEQ, D) for r in res.results], axis=0).reshape(16, SEQ, D)
    return out.astype(np.float32)
```

```python
import math
import os
import contextlib
import numpy as np
import concourse.bass as bass
import concourse.mybir as mybir
from concourse.bass_utils import run_bass_kernel_spmd

F32 = mybir.dt.float32
BF16 = mybir.dt.bfloat16
ALU = mybir.AluOpType
AF = mybir.ActivationFunctionType
AX = mybir.AxisListType

D = 2048
SEQ = 2048
CTX = 256
TOK = SEQ + CTX
NT = TOK // 128
EPS = 1e-6
NE = 16
FF = 1024


def bc(ap, n):
    return bass.AP(ap.tensor, ap.offset, [list(x) for x in ap.ap] + [[0, n]])


def bc_mid(ap, n):
    a = [list(x) for x in ap.ap]
    return bass.AP(ap.tensor, ap.offset, [a[0], [0, n]] + a[1:])


def rev(ap):
    a = [list(x) for x in ap.ap]
    st, n = a[-1]
    a[-1] = [-st, n]
    return bass.AP(ap.tensor, ap.offset + st * (n - 1), a)


class Prog:
    def __init__(self, nc, es):
        self.nc = nc
        self.es = es
        self.E = {'pe': nc.tensor, 'act': nc.scalar, 'dve': nc.vector, 'pool': nc.gpsimd, 'sp': nc.sync}
        self.sems = {}
        self.cnt = {}
        self.waited = {e: {} for e in self.E}
        self.lastw = {}
        self.readers = {}
        self.q = {e: [] for e in self.E}

    def sem(self, name):
        if name not in self.sems:
            self.sems[name] = self.es.enter_context(self.nc.semaphore(name))
            self.cnt[name] = 0
        return self.sems[name]

    def _wait(self, eng, s, v):
        if self.waited[eng].get(s, 0) >= v:
            return
        self.waited[eng][s] = v
        self.q[eng].append(('wait', s, v))

    def _deps(self, eng, reads, writes):
        need = {}

        def add(tok):
            if tok is None:
                return
            s, v = tok
            if eng == 'pe' and s == 'c_pe':
                return
            if need.get(s, 0) < v:
                need[s] = v
        for k in reads:
            add(self.lastw.get(k))
        for k in writes:
            add(self.lastw.get(k))
            for s, v in self.readers.get(k, {}).items():
                add((s, v))
        for s, v in need.items():
            self._wait(eng, s, v)

    def _commit(self, tok, reads, writes):
        s, v = tok
        for k in writes:
            self.lastw[k] = tok
            self.readers[k] = {}
        for k in reads:
            r = self.readers.setdefault(k, {})
            if r.get(s, 0) < v:
                r[s] = v

    def op(self, eng, fn, reads=(), writes=()):
        self._deps(eng, reads, writes)
        s = 'c_' + eng
        self.sem(s)
        self.cnt[s] += 1
        self.q[eng].append(('op', fn, s, 1))
        self._commit((s, self.cnt[s]), reads, writes)

    def dma(self, eng, pairs, reads, writes, sem, **kw):
        self._deps(eng, reads, writes)
        self.sem(sem)
        self._wait(eng, sem, self.cnt[sem])
        for (o, i) in pairs:
            self.cnt[sem] += 16
            self.q[eng].append(('op', (lambda e, o=o, i=i: e.dma_start(out=o, in_=i, **kw)), sem, 16))
        self._commit((sem, self.cnt[sem]), reads, writes)

    def barrier(self):
        for eng in self.E:
            for s, v in self.cnt.items():
                if v > 0:
                    self._wait(eng, s, v)
        self.lastw.clear()
        self.readers.clear()

    def flush(self):
        sems = self.sems
        with self.nc.Block() as block:
            for eng, deco in (('pe', block.tensor), ('act', block.scalar), ('dve', block.vector),
                              ('pool', block.gpsimd), ('sp', block.sync)):
                items = self.q[eng]
                self.q[eng] = []
                if not items:
                    continue

                def body(e, items=items):
                    for it in items:
                        if it[0] == 'wait':
                            e.wait_ge(sems[it[1]], it[2])
                        else:
                            ins = it[1](e)
                            ins.then_inc(sems[it[2]], it[3])
                deco(body)


def build(stop_after=None, debug=False, start_layer=0):
    nc = bass.Bass("TRN2", target_bir_lowering=False)
    es = contextlib.ExitStack()
    P = Prog(nc, es)

    def din(name, shape, dt=F32):
        return nc.dram_tensor(name, list(shape), dt, kind="ExternalInput").ap()

    def dscr(name, shape, dt=F32):
        return nc.dram_tensor(name, list(shape), dt, kind=("ExternalOutput" if debug else "Internal")).ap()

    xs = din("xs", [2 * SEQ, D])
    ctxs = din("ctxs", [2 * CTX, D])
    c3T = din("c3T", [128, 48])
    adaw = din("adaw", [2 * D, 6 * D])
    adabT = din("adabT", [128, 192])
    ngT = din("ngT", [128, 64])
    fgrow = din("fgrow", [1, D])
    wine = din("wine", [D, 5120])
    cawT = din("cawT", [128, 24])
    cbwT = din("cbwT", [128, 32])
    cbbT = din("cbbT", [128, 8])
    lruw = din("lruw", [4096, 128])
    lrubT = din("lrubT", [128, 32])
    lamT = din("lamT", [128, 16])
    woute = din("woute", [D, D])
    wino = din("wino", [D, 6144])
    lamv = din("lamv", [1, 512])
    sublnT = din("sublnT", [128, 2])
    wouto = din("wouto", [D, D])
    rw = din("rw", [D, NE])
    rb = din("rb", [1, NE])
    ewg = din("ewg", [2 * NE * D, FF])
    ewu = din("ewu", [2 * NE * D, FF])
    ewd = din("ewd", [2 * NE * FF, D])
    identd = din("identd", [128, 128])
    ropec = din("ropec", [128, SEQ])
    ropes = din("ropes", [128, SEQ])
    rpermd = din("rpermd", [128, 128])

    outd = nc.dram_tensor("out", [2 * SEQ, D], F32, kind="ExternalOutput").ap()

    grow = dscr("grow", [2 * 2 * 3, D])
    mixT_d = dscr("mixT_d", [2 * 16 * 128, TOK], BF16)
    fT_d = dscr("fT_d", [2 * 128, 16 * TOK], BF16)
    x1_d = dscr("x1_d", [2 * TOK, D])
    x2_d = din("x2_d", [2 * TOK, D]) if start_layer == 1 else dscr("x2_d", [2 * TOK, D])
    x3_d = dscr("x3_d", [2 * SEQ, D])

    uid = [0]

    def sb(name, shape, dt=F32, stack=es):
        uid[0] += 1
        return stack.enter_context(nc.sbuf_tensor(f"{name}_{uid[0]}", list(shape), dt))

    def ps(name, shape, dt=F32, stack=es):
        uid[0] += 1
        return stack.enter_context(nc.psum_tensor(f"{name}_{uid[0]}", list(shape), dt))

    identf = sb("identf", [128, 128])
    identb = sb("identb", [128, 128], BF16)
    onesf = sb("onesf", [128, 128])
    onesb = sb("onesb", [128, 128], BF16)
    rpermb = sb("rpermb", [128, 128], BF16)
    rpermf = sb("rpermf", [128, 128])
    scT = sb("scT", [128, 48])
    scTb = sb("scTb", [128, 48], BF16)
    mhalf = sb("mhalf", [128, 1])
    adab = sb("adab", [128, 192])
    ng = sb("ng", [128, 64])
    modc = sb("modc", [128, 2, 4, 48])
    comb = sb("comb", [128, 2, NT, 16])
    caw = sb("caw", [128, 24])
    cbw = sb("cbw", [128, 32])
    cbb = sb("cbb", [128, 8])
    lrub = sb("lrub", [128, 32])
    cneg = sb("cneg", [128, 16])
    lamt = sb("lamt", [128, 16])
    rbb = sb("rbb", [128, 16])
    rwt = sb("rwt", [128, 16, 16])
    subg = sb("subg", [128, 2])
    neglam = sb("neglam", [128, 1])
    lamrow = sb("lamrow", [1, 512])
    lamtmp = sb("lamtmp", [1, 8])

    LAM_INIT = 0.8 - 0.6 * math.exp(-0.3 * 1)

    def phase_setup():
        with contextlib.ExitStack() as st:
            pl = ps("pl", [128, 512], stack=st)
            P.dma('sp', [(identf[:, :], identd), (scT[:, :], c3T), (adab[:, :], adabT), (ng[:, :], ngT),
                         (caw[:, :], cawT), (cbw[:, :], cbwT), (cbb[:, :], cbbT), (lrub[:, :], lrubT),
                         (lamt[:, :], lamT), (rbb[:, :], rb.partition_broadcast(128)),
                         (rwt[:, :, :], rw.rearrange("(kc p) e -> p kc e", p=128)),
                         (subg[:, :], sublnT), (lamrow[:, :], lamv), (rpermf[:, :], rpermd)],
                  [], ['consts'], 'ld_c')
            P.op('pool', lambda e: e.memset(onesf[:, :], 1.0), [], ['onesf'])
            P.op('pool', lambda e: e.memset(onesb[:, :], 1.0), [], ['onesb'])
            P.op('dve', lambda e: e.tensor_copy(out=identb[:, :], in_=identf[:, :]), ['consts'], ['identb'])
            P.op('dve', lambda e: e.tensor_copy(out=rpermb[:, :], in_=rpermf[:, :]), ['consts'], ['rpermb'])
            P.op('act', lambda e: e.activation(out=scT[:, :], in_=scT[:, :], func=AF.Silu), ['consts'], ['consts'])
            P.op('dve', lambda e: e.tensor_copy(out=scTb[:, :], in_=scT[:, :]), ['consts'], ['scTb'])
            P.op('pool', lambda e: e.memset(mhalf[:, :], -0.5), [], ['mhalf'])
            P.op('act', lambda e: e.activation(out=cneg[:, :], in_=lamt[:, :], func=AF.Exp, scale=-1.0), ['consts'], ['cneg'])
            P.op('act', lambda e: e.activation(out=cneg[:, :], in_=cneg[:, :], func=AF.Ln, bias=1.0), ['cneg'], ['cneg'])
            P.op('dve', lambda e: e.tensor_scalar(out=cneg[:, :], in0=cneg[:, :], scalar1=-8.0, scalar2=None, op0=ALU.mult),
                 ['cneg'], ['cneg'])
            P.op('dve', lambda e: e.tensor_scalar(out=subg[:, :], in0=subg[:, :], scalar1=(1.0 - LAM_INIT), scalar2=None, op0=ALU.mult),
                 ['consts'], ['consts'])
            P.op('dve', lambda e: e.tensor_tensor(out=lamrow[:, 0:128], in0=lamrow[:, 0:128], in1=lamrow[:, 128:256], op=ALU.mult),
                 ['consts'], ['lr1'])
            P.op('dve', lambda e: e.tensor_tensor(out=lamrow[:, 256:384], in0=lamrow[:, 256:384], in1=lamrow[:, 384:512], op=ALU.mult),
                 ['consts'], ['lr2'])
            P.op('dve', lambda e: e.tensor_reduce(out=lamtmp[:, 0:1], in_=lamrow[:, 0:128], axis=AX.X, op=ALU.add), ['lr1'], ['lt0'])
            P.op('dve', lambda e: e.tensor_reduce(out=lamtmp[:, 1:2], in_=lamrow[:, 256:384], axis=AX.X, op=ALU.add), ['lr2'], ['lt1'])
            P.op('act', lambda e: e.activation(out=lamtmp[:, 2:4], in_=lamtmp[:, 0:2], func=AF.Exp), ['lt0', 'lt1'], ['lt2'])
            P.op('dve', lambda e: e.scalar_tensor_tensor(out=lamtmp[:, 4:5], in0=lamtmp[:, 3:4], scalar=-LAM_INIT, in1=lamtmp[:, 2:3],
                                                         op0=ALU.add, op1=ALU.subtract), ['lt2'], ['lt4'])
            P.op('pe', lambda e: e.matmul(pl[:, 0:1], lhsT=onesf[0:1, :], rhs=lamtmp[0:1, 4:5], start=True, stop=True),
                 ['lt4', 'onesf'], ['pl'])
            P.op('act', lambda e: e.activation(out=neglam[:, :], in_=pl[:, 0:1], func=AF.Copy), ['pl'], ['neglam'])
            P.barrier()
            P.flush()

    def phase_ada(l):
        with contextlib.ExitStack() as st:
            wa = [sb(f"wa{i}", [128, 16, 384], BF16, stack=st) for i in range(2)]
            psA = ps("psA", [128, 512], stack=st)
            modT = sb("modT", [128, 96, 4], stack=st)
            gtmp = sb("gtmp", [128, 96], stack=st)
            psA3 = psA[:, 0:384].rearrange("p (j r) -> p j r", r=4)
            for piece in range(32):
                sl = piece % 2
                src = adaw[l * D:(l + 1) * D, piece * 384:(piece + 1) * 384].rearrange("(kc p) c -> p kc c", p=128)
                P.dma('pool', [(wa[sl][:, :, :], src)], [], [('wa', sl)], f'ld_wa{sl}')
                for cbk in range(3):
                    j = piece * 3 + cbk

                    def fn(e, j=j, cbk=cbk, sl=sl):
                        for kc in range(16):
                            ins = e.matmul(psA[:, j * 4:j * 4 + 3], lhsT=wa[sl][:, kc, cbk * 128:(cbk + 1) * 128],
                                           rhs=scTb[:, kc * 3:(kc + 1) * 3], start=(kc == 0), stop=(kc == 15))
                        return ins
                    P.op('pe', fn, [('wa', sl), 'scTb'], ['psA'])
            P.op('dve', lambda e: e.tensor_tensor(out=modT[:, :, 0:3], in0=psA3[:, :, 0:3], in1=bc(adab[:, l * 96:(l + 1) * 96], 3), op=ALU.add),
                 ['psA', 'consts'], ['modT'])
            for which, (sec_sc, sec_sh, gi) in enumerate([(1, 0, 0), (4, 3, 1)]):
                gsc = modc[:, l, 2 * which, :].rearrange("p (k r) -> p k r", r=3)
                sh = modc[:, l, 2 * which + 1, :].rearrange("p (k r) -> p k r", r=3)
                gcol = ng[:, (l * 2 + gi) * 16:(l * 2 + gi + 1) * 16]
                P.op('dve', lambda e, gsc=gsc, sec_sc=sec_sc: e.tensor_scalar(out=gsc, in0=modT[:, sec_sc * 16:(sec_sc + 1) * 16, 0:3],
                                                                              scalar1=1.0, scalar2=None, op0=ALU.add),
                     ['modT'], [('gsc', which)])
                P.op('dve', lambda e, gsc=gsc, gcol=gcol: e.tensor_tensor(out=gsc, in0=gsc, in1=bc(gcol, 3), op=ALU.mult),
                     [('gsc', which), 'consts'], [('gsc', which)])
                P.op('dve', lambda e, sh=sh, sec_sh=sec_sh: e.tensor_copy(out=sh, in_=modT[:, sec_sh * 16:(sec_sh + 1) * 16, 0:3]),
                     ['modT'], [('sh', which)])
            pairs = []
            for gi, sec in enumerate((2, 5)):
                for r in range(3):
                    row = (l * 2 + gi) * 3 + r
                    dst = grow[row:row + 1, :].rearrange("o (j p) -> p (o j)", p=128)
                    pairs.append((dst, modT[:, sec * 16:(sec + 1) * 16, r]))
            with nc.allow_non_contiguous_dma(reason="tiny modulation rows"):
                P.dma('sp', pairs, ['modT'], ['grow'], 'st_g')
                P.barrier()
                P.flush()

    def norm_tile(st_tiles, x_tile_key, xt, l, which, r, dst_fn, dst_key, tp, tp_key, npass=1, junk_key='junk', kp=''):
        junk, stat, xn = st_tiles
        gsc = modc[:, l, 2 * which, :]
        sh = modc[:, l, 2 * which + 1, :]
        P.op('act', lambda e: e.activation(out=junk[:, :], in_=xt[:, :], func=AF.Square, accum_out=stat[:, 0:1]),
             [x_tile_key], [(kp, junk_key), (kp, 'stat0')])
        P.op('dve', lambda e: e.tensor_scalar(out=stat[:, 1:2], in0=stat[:, 0:1], scalar1=1.0 / D, scalar2=EPS, op0=ALU.mult, op1=ALU.add),
             [(kp, 'stat0')], [(kp, 'stat1')])
        P.op('act', lambda e: e.activation(out=stat[:, 2:3], in_=stat[:, 1:2], func=AF.Sqrt), [(kp, 'stat1')], [(kp, 'stat2')])
        P.op('dve', lambda e: e.reciprocal(out=stat[:, 3:4], in_=stat[:, 2:3]), [(kp, 'stat2')], [(kp, 'stat3')])
        P.op('act', lambda e: e.activation(out=xn[:, :], in_=xt[:, :], func=AF.Copy, scale=stat[:, 3:4]),
             [x_tile_key, (kp, 'stat3')], [(kp, 'xn')])
        per = 16 // npass
        for ps_i in range(npass):
            def tfn(e, ps_i=ps_i):
                for k in range(per):
                    kc = ps_i * per + k
                    ins = e.transpose(tp[:, k * 128:(k + 1) * 128], xn[:, kc * 128:(kc + 1) * 128], identf[:, :])
                return ins
            P.op('pe', tfn, [(kp, 'xn'), 'consts'], [tp_key])

            def efn(e, ps_i=ps_i):
                for k in range(per):
                    kc = ps_i * per + k
                    ins = e.tensor_scalar(out=dst_fn(kc), in0=tp[:, k * 128:(k + 1) * 128],
                                          scalar1=gsc[:, kc * 3 + r:kc * 3 + r + 1], scalar2=sh[:, kc * 3 + r:kc * 3 + r + 1],
                                          op0=ALU.mult, op1=ALU.add)
                return ins
            P.op('dve', efn, [tp_key, ('gsc', which), ('sh', which)], [dst_key])

    def phase_norm1(l, s, hT, st):
        with contextlib.ExitStack() as st2:
            xt = [sb(f"n1xt{i}", [128, D], stack=st2) for i in range(2)]
            junk = [sb(f"n1junk{i}", [128, D], BF16, stack=st2) for i in range(2)]
            stat = [sb(f"n1stat{i}", [128, 4], stack=st2) for i in range(2)]
            xn = [sb(f"n1xn{i}", [128, D], stack=st2) for i in range(2)]
            tp = [ps(f"n1tp{i}", [128, D], stack=st2) for i in range(2)]
            for tt in range(NT):
                sl = tt % 2
                if l == 0:
                    src = ctxs[s * CTX + tt * 128: s * CTX + (tt + 1) * 128, :] if tt < 2 else \
                        xs[s * SEQ + (tt - 2) * 128: s * SEQ + (tt - 1) * 128, :]
                else:
                    src = x2_d[s * TOK + tt * 128: s * TOK + (tt + 1) * 128, :]
                r = 2 if tt < 2 else s
                P.dma('sp', [(xt[sl][:, :], src)], [], [('xt', sl)], f'ld_xt{sl}')
                norm_tile((junk[sl], stat[sl], xn[sl]), ('xt', sl), xt[sl], l, 0, r,
                          lambda kc, tt=tt: hT[:, kc, tt * 128:(tt + 1) * 128], ('hT', tt), tp[sl], ('tp', sl), kp=f'n{sl}')
            P.barrier()
            P.flush()

    BLKS = [(0, 256), (256, 768), (768, 1280), (1280, 1792), (1792, 2304)]

    def blk_tiles(t0, t1):
        return [('hT', t) for t in range(t0 // 128, t1 // 128)]

    def phase_even(s, hT):
        with contextlib.ExitStack() as st:
            w = sb("ew0", [128, 16, 5, 128], BF16, stack=st)
            lw = sb("elw", [128, 32, 128], BF16, stack=st)
            L = [sb(f"eL{i}", [128, TOK], (F32 if i == 4 else BF16), stack=st) for i in range(5)]
            B = [None if i == 3 else sb(f"eB{i}", [128, TOK], F32, stack=st) for i in range(7)]
            ubf = sb("eubf", [128, TOK], BF16, stack=st)
            outb = sb("eob0", [128, TOK], BF16, stack=st)
            pp = [ps(f"epp{i}", [128, 512], stack=st) for i in range(8)]
            P.dma('pool', [(lw[:, :, :], lruw.rearrange("(g i) j -> i g j", i=128))], [], ['lw'], 'ld_lw')
            bank = [0]

            def nextbank():
                b = bank[0]
                bank[0] = (b + 1) % 8
                return b
            SEGS = [(0, CTX), (CTX, TOK)]

            def load_w(j):
                pairs = []
                for sec in range(5):
                    col0 = sec * 1024 + j * 128
                    pairs.append((w[:, :, sec, :], wine[:, col0:col0 + 128].rearrange("(kc p) c -> p kc c", p=128)))
                P.dma('pool', pairs, [], ['w'], 'ld_ew0')

            def proj_groups(secs):
                out = []
                for sec in secs:
                    for (t0, t1) in BLKS:
                        def g(sec=sec, t0=t0, t1=t1):
                            b = nextbank()
                            n = t1 - t0

                            def fn(e):
                                for kc in range(16):
                                    ins = e.matmul(pp[b][:, 0:n], lhsT=w[:, kc, sec, :], rhs=hT[:, kc, t0:t1],
                                                   start=(kc == 0), stop=(kc == 15))
                                return ins
                            P.op('pe', fn, ['w'] + blk_tiles(t0, t1), [('pp', b)])
                            P.op('act', lambda e: e.activation(out=L[sec][:, t0:t1], in_=pp[b][:, 0:n], func=AF.Copy),
                                 [('pp', b)], [('L', sec)])
                        out.append(g)
                return out

            def early_chain(j):
                P.op('dve', lambda e: e.tensor_tensor(out=B[6][:, :], in0=L[3][:, :], in1=L[3][:, :], op=ALU.mult), [('L', 3)], [('B', 6)])
                P.op('dve', lambda e: e.tensor_scalar(out=B[6][:, :], in0=B[6][:, :], scalar1=0.044715, scalar2=1.0, op0=ALU.mult, op1=ALU.add),
                     [('B', 6)], [('B', 6)])
                P.op('dve', lambda e: e.tensor_tensor(out=B[6][:, :], in0=B[6][:, :], in1=L[3][:, :], op=ALU.mult), [('B', 6), ('L', 3)], [('B', 6)])
                P.op('act', lambda e: e.activation(out=B[6][:, :], in_=B[6][:, :], func=AF.Sigmoid, scale=1.5957691216057308),
                     [('B', 6)], [('B', 6)])
                P.op('dve', lambda e: e.tensor_tensor(out=L[3][:, :], in0=L[3][:, :], in1=B[6][:, :], op=ALU.mult), [('B', 6), ('L', 3)], [('L', 3)])
                P.op('dve', lambda e: e.tensor_tensor(out=B[1][:, :], in0=L[1][:, :], in1=L[2][:, :], op=ALU.mult), [('L', 1), ('L', 2)], [('B', 1)])
                P.op('dve', lambda e: e.tensor_scalar(out=B[2][:, :], in0=B[1][:, :], scalar1=caw[:, j * 3 + 1:j * 3 + 2], scalar2=None, op0=ALU.mult),
                     [('B', 1), 'consts'], [('B', 2)])
                for (a0, a1) in SEGS:
                    P.op('dve', lambda e, a0=a0, a1=a1: e.scalar_tensor_tensor(
                        out=B[2][:, a0 + 1:a1], in0=B[1][:, a0:a1 - 1], scalar=caw[:, j * 3:j * 3 + 1], in1=B[2][:, a0 + 1:a1],
                        op0=ALU.mult, op1=ALU.add), [('B', 1), ('B', 2)], [('B', 2)])
                    P.op('dve', lambda e, a0=a0, a1=a1: e.scalar_tensor_tensor(
                        out=B[2][:, a0:a1 - 1], in0=B[1][:, a0 + 1:a1], scalar=caw[:, j * 3 + 2:j * 3 + 3], in1=B[2][:, a0:a1 - 1],
                        op0=ALU.mult, op1=ALU.add), [('B', 1), ('B', 2)], [('B', 2)])
                P.op('pool', lambda e: e.tensor_tensor(out=outb[:, :], in0=L[0][:, :], in1=B[2][:, :], op=ALU.mult),
                     [('L', 0), ('B', 2)], ['ob'])
                P.dma('sp', [(mixT_d[(s * 16 + j) * 128:(s * 16 + j + 1) * 128, :], outb[:, :])], ['ob'], ['mixT_d'], 'st_ob0')
                P.op('dve', lambda e: e.tensor_scalar(out=B[5][:, :], in0=L[4][:, :], scalar1=cbw[:, j * 4 + 2:j * 4 + 3], scalar2=cbb[:, j:j + 1],
                                                      op0=ALU.mult, op1=ALU.add), [('L', 4), 'consts'], [('B', 5)])
                for (a0, a1) in SEGS:
                    for (kk, sh_) in ((0, 2), (1, 1)):
                        P.op('dve', lambda e, a0=a0, a1=a1, kk=kk, sh_=sh_: e.scalar_tensor_tensor(
                            out=B[5][:, a0 + sh_:a1], in0=L[4][:, a0:a1 - sh_], scalar=cbw[:, j * 4 + kk:j * 4 + kk + 1], in1=B[5][:, a0 + sh_:a1],
                            op0=ALU.mult, op1=ALU.add), [('L', 4), ('B', 5)], [('B', 5)])
                    P.op('dve', lambda e, a0=a0, a1=a1: e.scalar_tensor_tensor(
                        out=B[5][:, a0:a1 - 1], in0=L[4][:, a0 + 1:a1], scalar=cbw[:, j * 4 + 3:j * 4 + 4], in1=B[5][:, a0:a1 - 1],
                        op0=ALU.mult, op1=ALU.add), [('L', 4), ('B', 5)], [('B', 5)])
                P.op('pool', lambda e: e.tensor_copy(out=ubf[:, :], in_=B[5][:, :]), [('B', 5)], ['ubf'])

            def late_ops(j):
                ops = []
                for d in range(2):
                    for gate, dst in ((0, 0), (1, 1)):
                        g = (gate * 2 + d) * 8 + j
                        for (t0, t1) in BLKS:
                            def gg(g=g, t0=t0, t1=t1, dst=dst):
                                b = nextbank()
                                n = t1 - t0
                                P.op('pe', lambda e: e.matmul(pp[b][:, 0:n], lhsT=lw[:, g, :], rhs=ubf[:, t0:t1], start=True, stop=True),
                                     ['lw', 'ubf'], [('pp', b)])
                                P.op('act', lambda e: e.activation(out=B[dst][:, t0:t1], in_=pp[b][:, 0:n], func=AF.Sigmoid, bias=lrub[:, g:g + 1]),
                                     [('pp', b), 'consts'], [('B', dst)])
                            ops.append(gg)
                    cn = cneg[:, d * 8 + j:d * 8 + j + 1]
                    ops.append(lambda cn=cn: P.op('act', lambda e: e.activation(out=B[0][:, :], in_=B[0][:, :], func=AF.Exp, scale=cn), [('B', 0), 'cneg'], [('B', 0)]))
                    ops.append(lambda: P.op('act', lambda e: e.activation(out=B[2][:, :], in_=B[0][:, :], func=AF.Square), [('B', 0)], [('B', 2)]))
                    ops.append(lambda: P.op('act', lambda e: e.activation(out=B[2][:, :], in_=B[2][:, :], func=AF.Sqrt, scale=-1.0, bias=1.0), [('B', 2)], [('B', 2)]))
                    ops.append(lambda: P.op('dve', lambda e: e.tensor_tensor(out=B[1][:, :], in0=B[1][:, :], in1=B[5][:, :], op=ALU.mult), [('B', 1), ('B', 5)], [('B', 1)]))
                    ops.append(lambda: P.op('dve', lambda e: e.tensor_tensor(out=B[1][:, :], in0=B[1][:, :], in1=B[2][:, :], op=ALU.mult), [('B', 1), ('B', 2)], [('B', 1)]))
                    if d == 0:
                        ops.append(lambda: P.op('dve', lambda e: e.tensor_tensor_scan(out=B[4][:, :], data0=B[0][:, :], data1=B[1][:, :], initial=0.0,
                                                                                       op0=ALU.mult, op1=ALU.add), [('B', 0), ('B', 1)], [('B', 4)]))
                    else:
                        ops.append(lambda: P.op('dve', lambda e: e.tensor_tensor_scan(out=rev(B[6][:, 0:CTX]), data0=rev(B[0][:, 0:CTX]), data1=rev(B[1][:, 0:CTX]),
                                                                                       initial=0.0, op0=ALU.mult, op1=ALU.add), [('B', 0), ('B', 1)], [('B', 6)]))
                        ops.append(lambda: P.op('dve', lambda e: e.tensor_tensor_scan(out=rev(B[6][:, CTX:TOK]), data0=rev(B[0][:, CTX:TOK]), data1=rev(B[1][:, CTX:TOK]),
                                                                                       initial=B[6][:, 0:1], op0=ALU.mult, op1=ALU.add), [('B', 0), ('B', 1), ('B', 6)], [('B', 6)]))
                ops.append(lambda: P.op('pool', lambda e: e.tensor_tensor(out=B[4][:, :], in0=B[4][:, :], in1=B[6][:, :], op=ALU.add), [('B', 4), ('B', 6)], [('B', 4)]))
                ops.append(lambda: P.op('pool', lambda e: e.tensor_tensor(out=outb[:, :], in0=L[3][:, :], in1=B[4][:, :], op=ALU.mult),
                                        [('L', 3), ('B', 4)], ['ob']))
                ops.append(lambda: P.dma('sp', [(mixT_d[(s * 16 + 8 + j) * 128:(s * 16 + 9 + j) * 128, :], outb[:, :])], ['ob'], ['mixT_d'], 'st_ob0'))
                return ops

            load_w(0)
            for g in proj_groups((0, 1, 2, 4, 3)):
                g()
            for j in range(8):
                early_chain(j)
                late = late_ops(j)
                if j + 1 < 8:
                    load_w(j + 1)
                    pg = proj_groups((0, 1, 2, 4))
                    pg3 = proj_groups((3,))
                else:
                    pg, pg3 = [], []
                for op_ in late:
                    op_()
                    if pg:
                        pg.pop(0)()
                for g in pg:
                    g()
                for g in pg3:
                    g()
            P.barrier()
            P.flush()

    def phase_outproj(l, s):
        ntile = NT if l == 0 else 16
        with contextlib.ExitStack() as st:
            wo = sb("owo", [128, 16, D], BF16, stack=st)
            aTt = [sb(f"oaT{i}", [128, 16, 128], BF16, stack=st) for i in range(2)]
            g1b = [sb(f"og1b{i}", [128, D], stack=st) for i in range(2)]
            xt = [sb(f"oxt{i}", [128, D], stack=st) for i in range(2)]
            xn = [sb(f"oxn{i}", [128, D], stack=st) for i in range(2)]
            stat = [sb(f"ostat{i}", [128, 4], stack=st) for i in range(2)]
            f32t = [sb(f"of32t{i}", [128, 16, 128], stack=st) for i in range(2)]
            fTt = [sb(f"ofTt{i}", [128, 16, 128], BF16, stack=st) for i in range(2)]
            scores = sb("oscores", [128, NT, 4, 4], stack=st)
            yp = ps("oyp", [128, D], stack=st)
            tp = ps("otp", [128, 1024], stack=st)
            lg = ps("olg", [128, 512], stack=st)
            wsrc = (woute if l == 0 else wouto).rearrange("(kc p) c -> p kc c", p=128)
            P.dma('pool', [(wo[:, kc, :], wsrc[:, kc, :]) for kc in range(16)], [], ['wo'], 'ld_wo')
            rows = [(l * 2 + 0) * 3 + 2, (l * 2 + 0) * 3 + s]
            P.dma('sp', [(g1b[i][:, :], grow[rows[i]:rows[i] + 1, :].partition_broadcast(128)) for i in range(2)], [], ['g1b'], 'ld_g1b')
            fT3 = fT_d[s * 128:(s + 1) * 128, :].rearrange("p (kc t) -> p kc t", kc=16)
            asrc = mixT_d[s * 2048:(s + 1) * 2048, :].rearrange("(c p) t -> p c t", p=128)
            for tt in range(ntile):
                sl = tt % 2
                kp = f'o{sl}'
                if l == 0:
                    isctx = tt < 2
                    src = ctxs[s * CTX + tt * 128: s * CTX + (tt + 1) * 128, :] if isctx else \
                        xs[s * SEQ + (tt - 2) * 128: s * SEQ + (tt - 1) * 128, :]
                    dst = x1_d[s * TOK + tt * 128: s * TOK + (tt + 1) * 128, :]
                else:
                    isctx = False
                    src = x2_d[s * TOK + CTX + tt * 128: s * TOK + CTX + (tt + 1) * 128, :]
                    dst = x3_d[s * SEQ + tt * 128: s * SEQ + (tt + 1) * 128, :]
                ftcol = tt * 128
                r = 2 if isctx else s
                gb_ = g1b[0] if isctx else g1b[1]
                a_, xt_, xn_, f32_, fT_, stat_ = aTt[sl], xt[sl], xn[sl], f32t[sl], fTt[sl], stat[sl]
                junk = fT_[:, :, :].rearrange("p a b -> p (a b)")
                P.dma('sp', [(a_[:, :, :], asrc[:, :, tt * 128:(tt + 1) * 128])], [], [('aT', sl)], f'ld_aT{sl}')
                P.dma('sp', [(xt_[:, :], src)], [], [('xt', sl)], f'ld_oxt{sl}')

                def yfn(e, a_=a_):
                    for cb in range(4):
                        for kc in range(16):
                            ins = e.matmul(yp[:, cb * 512:(cb + 1) * 512], lhsT=a_[:, kc, :],
                                           rhs=wo[:, kc, cb * 512:(cb + 1) * 512], start=(kc == 0), stop=(kc == 15))
                    return ins
                P.op('pe', yfn, [('aT', sl), 'wo'], ['yp'])
                P.op('dve', lambda e, gb_=gb_, xn_=xn_: e.tensor_tensor(out=xn_[:, :], in0=yp[:, :], in1=gb_[:, :], op=ALU.mult), ['yp', 'g1b'], [(kp, 'xn')])
                P.op('pool', lambda e, xn_=xn_, xt_=xt_: e.tensor_tensor(out=xt_[:, :], in0=xn_[:, :], in1=xt_[:, :], op=ALU.add), [(kp, 'xn'), ('xt', sl)], [('xt', sl)])
                P.dma('sp', [(dst, xt_[:, :])], [('xt', sl)], [('x1_d', sl)], f'st_x1{sl}')
                norm_tile((junk, stat_, xn_), ('xt', sl), xt_, l, 1, r, lambda kc, f32_=f32_: f32_[:, kc, :], ('f32t', sl), tp, 'tp', npass=2,
                          junk_key='fTt', kp=kp)
                P.op('pool', lambda e, fT_=fT_, f32_=f32_: e.tensor_copy(out=fT_[:, :, :], in_=f32_[:, :, :]), [('f32t', sl)], [(kp, 'fTt')])
                P.dma('sp', [(fT3[:, :, ftcol:ftcol + 128], fT_[:, :, :])], [(kp, 'fTt')], [('fT_d', sl)], f'st_fT{sl}')

                def lfn(e, f32_=f32_):
                    for kc in range(16):
                        ins = e.matmul(lg[:, 0:16], lhsT=f32_[:, kc, :], rhs=rwt[:, kc, :], start=(kc == 0), stop=(kc == 15))
                    return ins
                P.op('pe', lfn, [('f32t', sl), 'consts'], ['lg'])
                P.op('act', lambda e, tt=tt: e.activation(out=scores[:, tt, :, :].rearrange("p a b -> p (a b)"), in_=lg[:, 0:16], func=AF.Sigmoid),
                     ['lg'], ['scores'])
            routing(st, scores, ntile, s)
            P.barrier()
            P.flush()

    def routing(st, scores, T, s):
        sel = sb("r_sel", [128, NT, 4, 4], stack=st)
        sel2 = sb("r_sel2", [128, NT, 4, 4], stack=st)
        m1k = sb("r_m1k", [128, NT, 4, 4], stack=st)
        m2k = sb("r_m2k", [128, NT, 4, 4], stack=st)
        t1 = sb("r_t1", [128, NT, 4], stack=st)
        gs = sb("r_gs", [128, NT, 4], stack=st)
        gmask = sb("r_gmask", [128, NT, 4], stack=st)
        red = sb("r_red", [128, NT], stack=st)
        k = ['rt']

        def V(fn):
            P.op('dve', fn, ['scores', 'consts'] + k, k)
        sc_ = scores[:, 0:T]
        sl_, s2_, a1_, a2_ = sel[:, 0:T], sel2[:, 0:T], m1k[:, 0:T], m2k[:, 0:T]
        t1_, gs_, gm_, rd_ = t1[:, 0:T], gs[:, 0:T], gmask[:, 0:T], red[:, 0:T]

        def f16(a):
            return a.rearrange("p t a b -> p t (a b)")
        V(lambda e: e.tensor_tensor(out=f16(sl_), in0=f16(sc_), in1=bc_mid(rbb[:, :], T), op=ALU.add))
        pairs = [(0, 1), (0, 2), (0, 3), (1, 2), (1, 3), (2, 3)]
        for i, (a, b) in enumerate(pairs):
            dst = gs_ if i == 0 else t1_
            V(lambda e, a=a, b=b, dst=dst: e.tensor_tensor(out=dst, in0=sl_[:, :, :, a], in1=sl_[:, :, :, b], op=ALU.add))
            if i > 0:
                V(lambda e: e.tensor_tensor(out=gs_, in0=gs_, in1=t1_, op=ALU.max))
        V(lambda e: e.tensor_reduce(out=rd_, in_=gs_, axis=AX.X, op=ALU.max))
        V(lambda e: e.tensor_tensor(out=gm_, in0=gs_, in1=bc(rd_, 4), op=ALU.is_equal))
        V(lambda e: e.tensor_scalar(out=f16(s2_), in0=f16(sl_), scalar1=2.0, scalar2=None, op0=ALU.add))
        V(lambda e: e.tensor_tensor(out=s2_, in0=s2_, in1=bc(gm_, 4), op=ALU.mult))
        V(lambda e: e.tensor_reduce(out=rd_, in_=f16(s2_), axis=AX.X, op=ALU.max))
        V(lambda e: e.tensor_tensor(out=f16(a1_), in0=f16(s2_), in1=bc(rd_, 16), op=ALU.is_equal))
        V(lambda e: e.scalar_tensor_tensor(out=f16(s2_), in0=f16(a1_), scalar=-4.0, in1=f16(s2_), op0=ALU.mult, op1=ALU.add))
        V(lambda e: e.tensor_reduce(out=rd_, in_=f16(s2_), axis=AX.X, op=ALU.max))
        V(lambda e: e.tensor_tensor(out=f16(a2_), in0=f16(s2_), in1=bc(rd_, 16), op=ALU.is_equal))
        V(lambda e: e.tensor_tensor(out=f16(a1_), in0=f16(a1_), in1=f16(a2_), op=ALU.add))
        V(lambda e: e.tensor_tensor(out=f16(a1_), in0=f16(a1_), in1=f16(sc_), op=ALU.mult))
        V(lambda e: e.tensor_reduce(out=rd_, in_=f16(a1_), axis=AX.X, op=ALU.add))
        V(lambda e: e.reciprocal(out=rd_, in_=rd_))
        P.op('dve', lambda e: e.tensor_tensor(out=comb[:, s, 0:T, :], in0=f16(a1_), in1=bc(rd_, 16), op=ALU.mult), k, [('comb', s)])

    def phase_moe(l, s):
        if l == 0:
            TB, NBK, NSUB, SUBW, ntok = 1152, 2, 3, 384, TOK
        else:
            TB, NBK, NSUB, SUBW, ntok = 1024, 2, 2, 512, SEQ
        ntl = TB // 128
        with contextlib.ExitStack() as st:
            acc = sb("macc", [128, ntl, D], stack=st)
            fT = sb("mfT", [128, 16, TB], BF16, stack=st)
            WGU = [(sb(f"mwg{i}", [128, 16, 256], BF16, stack=st), sb(f"mwu{i}", [128, 16, 256], BF16, stack=st)) for i in range(2)]
            WD = [sb(f"mwd{i}", [128, 2, D], BF16, stack=st) for i in range(3)]
            hid = [sb(f"mhid{i}", [128, 2, TB], BF16, stack=st) for i in range(2)]
            sg = [sb(f"msg{i}", [128, 512], stack=st) for i in range(2)]
            g2b = [sb(f"mg2b{i}", [128, D], stack=st) for i in range(2)]
            stat = sb("mstat", [128, 8], stack=st)
            if l == 1:
                junk = sb("mjunk", [128, D], BF16, stack=st)
                fgb = sb("mfgb", [128, D], stack=st)
            gps = [ps(f"mgps{i}", [128, 512], stack=st) for i in range(2)]
            ups = [ps(f"mups{i}", [128, 512], stack=st) for i in range(2)]
            ops_ = [ps(f"mops{i}", [128, 1024], stack=st) for i in range(2)]
            rows = [(l * 2 + 1) * 3 + 2, (l * 2 + 1) * 3 + s]
            P.dma('sp', [(g2b[i][:, :], grow[rows[i]:rows[i] + 1, :].partition_broadcast(128)) for i in range(2)], [], ['g2b'], 'ld_g2b')
            if l == 1:
                P.dma('sp', [(fgb[:, :], fgrow.partition_broadcast(128))], [], ['fgb'], 'ld_fgb')
            fT3 = fT_d[s * 128:(s + 1) * 128, :].rearrange("p (kc t) -> p kc t", kc=16)
            ucount = [0]
            gi = [0]
            for bk in range(NBK):
                tok0 = bk * TB
                P.dma('sp', [(fT[:, :, 0:TB], fT3[:, :, tok0:tok0 + TB])], [], ['fT'], 'ld_mfT')
                prev_wd = []
                for ex in range(NE):
                    for q in range(4):
                        u = ucount[0]
                        ucount[0] += 1
                        sl = u % 2
                        sl3 = u % 3
                        wg, wu = WGU[sl]
                        wd = WD[sl3]
                        rg = (l * NE + ex) * D
                        rd = (l * NE + ex) * FF + q * 256
                        P.dma('pool', [(wg[:, :, :], ewg[rg:rg + D, q * 256:(q + 1) * 256].rearrange("(kc p) c -> p kc c", p=128)),
                                       (wu[:, :, :], ewu[rg:rg + D, q * 256:(q + 1) * 256].rearrange("(kc p) c -> p kc c", p=128))],
                              [], [('Wg', sl), ('Wu', sl)], f'ld_mw{sl}')
                        P.dma('pool', [(wd[:, :, :], ewd[rd:rd + 256, :].rearrange("(fc p) c -> p fc c", p=128))],
                              [], [('Wd', sl3)], f'ld_md{sl3}')
                        hs = hid[sl]

                        def gu_item(fc, nb, mid_cb, sl=sl, wg=wg, wu=wu, hs=hs):
                            i = gi[0] % 2
                            gi[0] += 1
                            c0 = nb * SUBW

                            def gfn(e):
                                for kc in range(16):
                                    ins = e.matmul(gps[i][:, 0:SUBW], lhsT=wg[:, kc, fc * 128:(fc + 1) * 128], rhs=fT[:, kc, c0:c0 + SUBW],
                                                   start=(kc == 0), stop=(kc == 15))
                                return ins

                            def ufn(e):
                                for kc in range(16):
                                    ins = e.matmul(ups[i][:, 0:SUBW], lhsT=wu[:, kc, fc * 128:(fc + 1) * 128], rhs=fT[:, kc, c0:c0 + SUBW],
                                                   start=(kc == 0), stop=(kc == 15))
                                return ins
                            P.op('pe', gfn, [('Wg', sl), 'fT'], [('gps', i)])
                            P.op('act', lambda e: e.activation(out=sg[i][:, 0:SUBW], in_=gps[i][:, 0:SUBW], func=AF.Silu),
                                 [('gps', i)], [('sg', i)])
                            mid_cb()
                            P.op('pe', ufn, [('Wu', sl), 'fT'], [('ups', i)])
                            P.op('dve', lambda e: e.tensor_tensor(out=hs[:, fc, c0:c0 + SUBW], in0=ups[i][:, 0:SUBW],
                                                                  in1=sg[i][:, 0:SUBW], op=ALU.mult),
                                 [('ups', i), ('sg', i)], [('hid', sl)])

                        def wd_item(tt, half, sl=sl, sl3=sl3, hs=hs, wd=wd, ex=ex, first=(ex == 0 and q == 0)):
                            gt = bk * ntl + tt

                            def ofn(e):
                                for fc in range(2):
                                    for c2 in range(2):
                                        cb = half * 2 + c2
                                        ins = e.matmul(ops_[half][:, c2 * 512:(c2 + 1) * 512], lhsT=hs[:, fc, tt * 128:(tt + 1) * 128],
                                                       rhs=wd[:, fc, cb * 512:(cb + 1) * 512], start=(fc == 0), stop=(fc == 1))
                                return ins
                            P.op('pe', ofn, [('hid', sl), ('Wd', sl3)], [('ops', half)])
                            if first:
                                P.op('dve', lambda e: e.tensor_scalar(
                                    out=acc[:, tt, half * 1024:(half + 1) * 1024], in0=ops_[half][:, :], scalar1=comb[:, s, gt, ex:ex + 1],
                                    scalar2=None, op0=ALU.mult),
                                    [('ops', half), ('comb', s)], [('acc', tt, half)])
                            else:
                                P.op('dve', lambda e: e.scalar_tensor_tensor(
                                    out=acc[:, tt, half * 1024:(half + 1) * 1024], in0=ops_[half][:, :], scalar=comb[:, s, gt, ex:ex + 1],
                                    in1=acc[:, tt, half * 1024:(half + 1) * 1024], op0=ALU.mult, op1=ALU.add),
                                    [('ops', half), ('comb', s), ('acc', tt, half)], [('acc', tt, half)])
                        gu_list = [(fc, nb) for fc in range(2) for nb in range(NSUB)]
                        nslots = 2 * len(gu_list)
                        nwd = len(prev_wd)
                        slot_i = [0]

                        def drain_slot():
                            k = slot_i[0]
                            slot_i[0] += 1
                            cnt = ((k + 1) * nwd) // nslots - (k * nwd) // nslots
                            for _ in range(cnt):
                                if prev_wd:
                                    prev_wd.pop(0)()
                        for (fc, nb) in gu_list:
                            gu_item(fc, nb, drain_slot)
                            drain_slot()
                        while prev_wd:
                            prev_wd.pop(0)()
                        prev_wd = [(lambda tt=tt, half=half, f=wd_item: f(tt, half)) for tt in range(ntl) for half in range(2)]
                while prev_wd:
                    prev_wd.pop(0)()
                sl_last = (ucount[0] - 1) % 2
                xbuf = [WGU[sl_last][i][:, :, :].bitcast(F32).rearrange("p a b -> p (a b)") for i in range(2)]
                for tt in range(ntl):
                    gt = bk * ntl + tt
                    xi = tt % 2
                    xb_ = xbuf[xi]
                    gk = ('Wg', sl_last) if xi == 0 else ('Wu', sl_last)
                    if l == 0:
                        isctx = gt < 2
                        src = x1_d[s * TOK + gt * 128: s * TOK + (gt + 1) * 128, :]
                        dst = x2_d[s * TOK + gt * 128: s * TOK + (gt + 1) * 128, :]
                    else:
                        isctx = False
                        src = x3_d[s * SEQ + gt * 128: s * SEQ + (gt + 1) * 128, :]
                        dst = outd[s * SEQ + gt * 128: s * SEQ + (gt + 1) * 128, :]
                    gb_ = g2b[0] if isctx else g2b[1]
                    P.dma('sp', [(xb_, src)], [], [('x1t', xi), gk], f'ld_mx{xi}')
                    P.op('dve', lambda e, tt=tt, gb_=gb_: e.tensor_tensor(out=acc[:, tt, :], in0=acc[:, tt, :], in1=gb_[:, :], op=ALU.mult),
                         [('acc', tt, 0), ('acc', tt, 1), 'g2b'], [('acc', tt, 0), ('acc', tt, 1)])
                    P.op('pool', lambda e, tt=tt, xb_=xb_: e.tensor_tensor(out=xb_, in0=acc[:, tt, :], in1=xb_, op=ALU.add),
                         [('acc', tt, 0), ('acc', tt, 1), ('x1t', xi)], [('x1t', xi)])
                    if l == 1:
                        P.op('act', lambda e, xb_=xb_, xi=xi: e.activation(out=junk[:, :], in_=xb_, func=AF.Square, accum_out=stat[:, xi * 4:xi * 4 + 1]),
                             [('x1t', xi)], ['junk', ('st0', xi)])
                        P.op('act', lambda e, xi=xi: e.activation(out=stat[:, xi * 4 + 1:xi * 4 + 2], in_=stat[:, xi * 4:xi * 4 + 1], func=AF.Ln, scale=1.0 / D, bias=EPS),
                             [('st0', xi)], [('st1', xi)])
                        P.op('act', lambda e, xi=xi: e.activation(out=stat[:, xi * 4 + 2:xi * 4 + 3], in_=stat[:, xi * 4 + 1:xi * 4 + 2], func=AF.Exp, scale=-0.5),
                             [('st1', xi)], [('st2', xi)])
                        P.op('act', lambda e, xb_=xb_, xi=xi: e.activation(out=xb_, in_=xb_, func=AF.Copy, scale=stat[:, xi * 4 + 2:xi * 4 + 3]),
                             [('x1t', xi), ('st2', xi)], [('x1t', xi)])
                        P.op('pool', lambda e, xb_=xb_: e.tensor_tensor(out=xb_, in0=xb_, in1=fgb[:, :], op=ALU.mult), [('x1t', xi), 'fgb'], [('x1t', xi)])
                    P.dma('sp', [(dst, xb_)], [('x1t', xi), gk], [('mout', xi)], f'st_mo{xi}')
            P.barrier()
            P.flush()

    def phase_attn(s, hT):
        SCALE = 128 ** -0.5
        with contextlib.ExitStack() as st:
            w = [sb(f"aw{i}", [128, 16, 768], BF16, stack=st) for i in range(2)]
            qT = sb("aqT", [128, 2, SEQ], BF16, stack=st)
            kT = sb("akT", [128, 2, TOK], BF16, stack=st)
            V_ = sb("aV", [128, NT, 256], BF16, stack=st)
            cosT = sb("acos", [128, SEQ], stack=st)
            sinT = sb("asin", [128, SEQ], stack=st)
            qb16 = sb("aqb16", [128, 512], BF16, stack=st)
            t1 = sb("at1", [128, 512], stack=st)
            t2 = sb("at2", [128, 512], stack=st)
            PT = [sb(f"aPT{i}", [128, 512], BF16, stack=st) for i in range(2)]
            rs = sb("ars", [128, 512], stack=st)
            oc = [sb(f"aoc{i}", [128, 512], stack=st) for i in range(2)]
            otmp = sb("aotmp", [128, 512], stack=st)
            sq = [sb(f"asq{i}", [128, 512], stack=st) for i in range(2)]
            ofst = [sb(f"aofst{i}", [128, 512], BF16, stack=st) for i in range(2)]
            A = [ps(f"aA{i}", [128, 512], stack=st) for i in range(2)]
            O = [[ps(f"aO{i}_{c}", [128, 512], stack=st) for c in range(3)] for i in range(2)]
            P.dma('sp', [(cosT[:, :], ropec), (sinT[:, :], ropes)], [], ['rope'], 'ld_rope')
            ai = [0]

            def nextA():
                a = ai[0] % 2
                ai[0] += 1
                return a
            oset = [0]
            LATB = [(0, 512), (512, 1024), (1024, 1536), (1536, 2048)]
            ATT_STAGE = int(os.environ.get('ATT_STAGE', '9'))
            pending = []
            for hd in range(int(os.environ.get('ATT_HEADS', '8'))):
                sl = hd % 2
                P.dma('pool', [(w[sl][:, :, sec * 256:(sec + 1) * 256],
                                wino[:, sec * 2048 + hd * 256: sec * 2048 + (hd + 1) * 256].rearrange("(kc p) c -> p kc c", p=128))
                               for sec in range(3)], [], [('w', sl)], f'ld_aw{sl}')
                for sec, dstT in ((0, qT), (1, kT)):
                    for m in range(2):
                        wc0 = sec * 256 + m * 128
                        if sec == 1:
                            a = nextA()

                            def fnc(e, a=a, wc0=wc0, sl=sl):
                                for kc in range(16):
                                    ins = e.matmul(A[a][:, 0:256], lhsT=w[sl][:, kc, wc0:wc0 + 128], rhs=hT[:, kc, 0:256], start=(kc == 0), stop=(kc == 15))
                                return ins
                            P.op('pe', fnc, [('w', sl), ('hT', 0), ('hT', 1)], [('A', a)])
                            P.op('act', lambda e, a=a, m=m: e.activation(out=kT[:, m, 0:256], in_=A[a][:, 0:256], func=AF.Copy), [('A', a)], [('kT', m)])
                        for (l0, l1) in LATB:
                            a = nextA()
                            a2 = nextA()
                            toff = 0 if sec == 0 else CTX

                            def fnp(e, a=a, wc0=wc0, sl=sl, l0=l0, l1=l1):
                                for kc in range(16):
                                    ins = e.matmul(A[a][:, :], lhsT=w[sl][:, kc, wc0:wc0 + 128], rhs=hT[:, kc, CTX + l0:CTX + l1], start=(kc == 0), stop=(kc == 15))
                                return ins
                            P.op('pe', fnp, [('w', sl)] + blk_tiles(CTX + l0, CTX + l1), [('A', a)])
                            P.op('act', lambda e, a=a: e.activation(out=qb16[:, :], in_=A[a][:, :], func=AF.Copy), [('A', a)], ['qb16'])
                            P.op('dve', lambda e, a=a, l0=l0, l1=l1: e.tensor_tensor(out=t1[:, :], in0=A[a][:, :], in1=cosT[:, l0:l1], op=ALU.mult),
                                 [('A', a), 'rope', 'qb16'], ['t1'])
                            P.op('pe', lambda e, a2=a2: e.matmul(A[a2][:, :], lhsT=rpermb[:, :], rhs=qb16[:, :], start=True, stop=True),
                                 ['qb16', 'rpermb'], [('A', a2)])
                            P.op('dve', lambda e, a2=a2, l0=l0, l1=l1: e.tensor_tensor(out=t2[:, :], in0=A[a2][:, :], in1=sinT[:, l0:l1], op=ALU.mult),
                                 [('A', a2), 'rope'], ['t2'])
                            P.op('pool', lambda e, dstT=dstT, m=m, toff=toff, l0=l0, l1=l1: e.tensor_tensor(
                                out=dstT[:, m, toff + l0:toff + l1], in0=t1[:, :], in1=t2[:, :], op=ALU.add),
                                ['t1', 't2'], [('qk', sec, m)] if sec == 0 else [('kT', m)])
                for tt in range(NT):
                    a = nextA()

                    def fnv(e, a=a, tt=tt, sl=sl):
                        for kc in range(16):
                            ins = e.matmul(A[a][:, 0:256], lhsT=hT[:, kc, tt * 128:(tt + 1) * 128], rhs=w[sl][:, kc, 512:768], start=(kc == 0), stop=(kc == 15))
                        return ins
                    P.op('pe', fnv, [('w', sl), ('hT', tt)], [('A', a)])
                    P.op('act', lambda e, a=a, tt=tt: e.activation(out=V_[:, tt, :], in_=A[a][:, 0:256], func=AF.Copy), [('A', a)], ['V'])
                for qb in range(4 if ATT_STAGE >= 2 else 0):
                    q0 = qb * 512
                    for m in range(2):
                        os_ = oset[0] % 2
                        oset[0] += 1
                        Oc = O[os_]
                        def emitS(kt, m=m, q0=q0):
                            a = nextA()
                            P.op('pe', lambda e, a=a, m=m, kt=kt, q0=q0: e.matmul(A[a][:, :], lhsT=kT[:, m, kt * 128:(kt + 1) * 128], rhs=qT[:, m, q0:q0 + 512],
                                                                                    start=True, stop=True), [('kT', m), ('qk', 0, m)], [('A', a)])
                            return a
                        a_next = emitS(0)
                        for kt in range(NT):
                            a = a_next
                            if kt + 1 < NT:
                                a_next = emitS(kt + 1)
                            P.op('act', lambda e, a=a: e.activation(out=PT[a][:, :], in_=A[a][:, :], func=AF.Exp, scale=SCALE), [('A', a)], [('PT', a)])

                            def fno(e, a=a, kt=kt, Oc=Oc):
                                e.matmul(Oc[0][:, :], lhsT=V_[:, kt, 0:128], rhs=PT[a][:, :], start=(kt == 0), stop=(kt == NT - 1))
                                e.matmul(Oc[1][:, :], lhsT=V_[:, kt, 128:256], rhs=PT[a][:, :], start=(kt == 0), stop=(kt == NT - 1))
                                return e.matmul(Oc[2][:, :], lhsT=onesb[:, :], rhs=PT[a][:, :], start=(kt == 0), stop=(kt == NT - 1))
                            P.op('pe', fno, ['V', ('PT', a), 'onesb'], [('O', os_)])
                            if kt == 3 and pending:
                                pending.pop(0)()
                        P.op('dve', lambda e, Oc=Oc: e.reciprocal(out=rs[:, :], in_=Oc[2][:, :]), [('O', os_)], ['rs'])
                        if m == 0:
                            for c in range(2):
                                P.op('dve', lambda e, c=c, Oc=Oc: e.tensor_tensor(out=oc[c][:, :], in0=Oc[c][:, :], in1=rs[:, :], op=ALU.mult),
                                     [('O', os_), 'rs'], [('oc', c)])
                        else:
                            P.op('dve', lambda e: e.tensor_scalar(out=rs[:, :], in0=rs[:, :], scalar1=neglam[:, 0:1], scalar2=None, op0=ALU.mult),
                                 ['rs', 'neglam'], ['rs'])
                            for c in range(2):
                                P.op('dve', lambda e, c=c, Oc=Oc: e.tensor_tensor(out=otmp[:, :], in0=Oc[c][:, :], in1=rs[:, :], op=ALU.mult),
                                     [('O', os_), 'rs'], ['otmp'])
                                P.op('pool', lambda e, c=c: e.tensor_tensor(out=oc[c][:, :], in0=oc[c][:, :], in1=otmp[:, :], op=ALU.add),
                                     [('oc', c), 'otmp'], [('oc', c)])
                    def tail(q0=q0, hd=hd):
                        for c in range(2):
                            P.op('act', lambda e, c=c: e.activation(out=sq[c][:, :], in_=oc[c][:, :], func=AF.Square), [('oc', c)], [('sq', c)])
                        a = nextA()
                        def fnn(e, a=a):
                            e.matmul(A[a][:, :], lhsT=onesf[:, :], rhs=sq[0][:, :], start=True, stop=False)
                            return e.matmul(A[a][:, :], lhsT=onesf[:, :], rhs=sq[1][:, :], start=False, stop=True)
                        P.op('pe', fnn, [('sq', 0), ('sq', 1), 'onesf'], [('A', a)])
                        P.op('dve', lambda e, a=a: e.tensor_scalar(out=rs[:, :], in0=A[a][:, :], scalar1=1.0 / 256, scalar2=EPS, op0=ALU.mult, op1=ALU.add),
                             [('A', a)], ['rs'])
                        P.op('act', lambda e: e.activation(out=rs[:, :], in_=rs[:, :], func=AF.Sqrt), ['rs'], ['rs'])
                        P.op('dve', lambda e: e.reciprocal(out=rs[:, :], in_=rs[:, :]), ['rs'], ['rs'])
                        for c in range(2):
                            P.op('dve', lambda e, c=c: e.scalar_tensor_tensor(out=ofst[c][:, :], in0=oc[c][:, :], scalar=subg[:, c:c + 1], in1=rs[:, :],
                                                                              op0=ALU.mult, op1=ALU.mult), [('oc', c), 'rs', 'consts'], [('ofst', c)])
                            ch = hd * 2 + c
                            P.dma('sp', [(mixT_d[(s * 16 + ch) * 128:(s * 16 + ch + 1) * 128, q0:q0 + 512], ofst[c][:, :])], [('ofst', c)], ['mixT_d'], f'st_of{c}')
                        nextA()
                    if ATT_STAGE >= 3:
                        pending.append(tail)
                while pending:
                    pending.pop(0)()
            P.barrier()
            P.flush()

    stages = []
    phase_setup()
    stages.append('setup')
    done = [False]

    def chk(name):
        if stop_after == name:
            done[0] = True
        return done[0]

    for l in range(start_layer, 2):
        if done[0]:
            break
        phase_ada(l)
        if chk(f'ada{l}'):
            break
        for s in range(2):
            with contextlib.ExitStack() as sth:
                hT = sb(f"hT_{l}_{s}", [128, 16, TOK], BF16, stack=sth)
                phase_norm1(l, s, hT, sth)
                if chk(f'norm1_{l}_{s}'):
                    break
                if l == 0:
                    phase_even(s, hT)
                else:
                    phase_attn(s, hT)
            if chk(f'mix_{l}_{s}'):
                break
            phase_outproj(l, s)
            if chk(f'outproj_{l}_{s}'):
                break
            phase_moe(l, s)
            if chk(f'moe_{l}_{s}'):
                break
    P.barrier()
    P.flush()
    es.close()
    return nc


def _fm(v, n=16):
    return np.ascontiguousarray(np.asarray(v, np.float32).reshape(n, 128).T)


def prep_inputs(inputs):
    g = {k: np.asarray(v) for k, v in inputs.items()}
    rep = {}
    rep['adaw'] = np.ascontiguousarray(g['ada_w'].reshape(2 * D, 6 * D))
    rep['adabT'] = np.ascontiguousarray(np.concatenate([_fm(g['ada_b'][l], 96) for l in range(2)], axis=1))
    rep['ngT'] = np.ascontiguousarray(np.concatenate([_fm(g['norm_mix_g'][0]), _fm(g['norm_ffn_g'][0]),
                                                       _fm(g['norm_mix_g'][1]), _fm(g['norm_ffn_g'][1])], axis=1))
    rep['fgrow'] = np.ascontiguousarray(g['final_g'].reshape(1, D))
    rep['wine'] = np.ascontiguousarray(g['w_in_e'][0])
    rep['cawT'] = np.ascontiguousarray(g['conv_a_w'][0].reshape(3, 8, 128).transpose(2, 1, 0).reshape(128, 24))
    rep['cbwT'] = np.ascontiguousarray(g['conv_b_w'][0].reshape(4, 8, 128).transpose(2, 1, 0).reshape(128, 32))
    rep['cbbT'] = _fm(g['conv_b_b'][0], 8)
    rep['lruw'] = np.ascontiguousarray(np.stack([g['lru_wa'][0], g['lru_wi'][0]], axis=0).reshape(4096, 128))
    rep['lrubT'] = np.ascontiguousarray(np.stack([g['lru_ba'][0], g['lru_bi'][0]], axis=0).reshape(2, 2, 8, 128).transpose(3, 0, 1, 2).reshape(128, 32))
    rep['lamT'] = np.ascontiguousarray(g['lru_lam'][0].reshape(2, 8, 128).transpose(2, 0, 1).reshape(128, 16))
    rep['woute'] = np.ascontiguousarray(g['w_out_e'][0])
    rep['wino'] = np.ascontiguousarray(g['w_in_o'][0])
    rep['lamv'] = np.ascontiguousarray(np.concatenate([g['lam_q1'][0], g['lam_k1'][0], g['lam_q2'][0], g['lam_k2'][0]]).reshape(1, 512))
    rep['sublnT'] = _fm(g['subln_g'][0], 2)
    rep['wouto'] = np.ascontiguousarray(g['w_out_o'][0])
    rep['rw'] = np.ascontiguousarray(g['router_w'])
    rep['rb'] = np.ascontiguousarray(g['router_b'].reshape(1, NE))
    rep['ewg'] = np.ascontiguousarray(g['exp_w_gate'].reshape(2 * NE * D, FF))
    rep['ewu'] = np.ascontiguousarray(g['exp_w_up'].reshape(2 * NE * D, FF))
    rep['ewd'] = np.ascontiguousarray(g['exp_w_down'].reshape(2 * NE * FF, D))
    rep['identd'] = np.eye(128, dtype=np.float32)
    t = np.arange(SEQ)
    row = (t // 64).astype(np.float32)
    col = (t % 64).astype(np.float32)
    inv = (10000.0 ** (-np.arange(0, 64, 2, dtype=np.float32) / 64)).astype(np.float32)
    ang_r = row[:, None] * inv
    ang_c = col[:, None] * inv
    ang = np.concatenate([ang_r, ang_r, ang_c, ang_c], axis=-1)
    rep['ropec'] = np.ascontiguousarray(np.cos(ang).astype(np.float32).T)
    rep['ropes'] = np.ascontiguousarray(np.sin(ang).astype(np.float32).T)
    rp = np.zeros((128, 128), np.float32)
    for m in range(128):
        if (m % 64) < 32:
            rp[m + 32, m] = -1.0
        else:
            rp[m - 32, m] = 1.0
    rep['rpermd'] = rp
    maps = []
    for c in range(8):
        mp = dict(rep)
        mp['xs'] = np.ascontiguousarray(g['x'][2 * c:2 * c + 2].reshape(2 * SEQ, D))
        mp['ctxs'] = np.ascontiguousarray(g['ctx'][2 * c:2 * c + 2].reshape(2 * CTX, D))
        c3 = np.stack([g['c'][2 * c], g['c'][2 * c + 1], g['c_ctx']], axis=0)
        mp['c3T'] = np.ascontiguousarray(c3.reshape(3, 16, 128).transpose(2, 1, 0).reshape(128, 48))
        maps.append(mp)
    return maps


def kernel(**inputs):
    maps = prep_inputs(inputs)
    nc = build()
    res = run_bass_kernel_spmd(nc, maps, core_ids=list(range(8)))
    out = np.stack([r["out"].reshape(2, SEQ, D) for r in res.results], axis=0).reshape(16, SEQ, D)
    return out.astype(np.float32)
```

```python
import math
import os
import contextlib
import numpy as np
import concourse.bass as bass
import concourse.mybir as mybir
from concourse.bass_utils import run_bass_kernel_spmd

F32 = mybir.dt.float32
BF16 = mybir.dt.bfloat16
ALU = mybir.AluOpType
AF = mybir.ActivationFunctionType
AX = mybir.AxisListType

D = 2048
SEQ = 2048
CTX = 256
TOK = SEQ + CTX
NT = TOK // 128
EPS = 1e-6
NE = 16
FF = 1024


def bc(ap, n):
    return bass.AP(ap.tensor, ap.offset, [list(x) for x in ap.ap] + [[0, n]])


def bc_mid(ap, n):
    a = [list(x) for x in ap.ap]
    return bass.AP(ap.tensor, ap.offset, [a[0], [0, n]] + a[1:])


def rev(ap):
    a = [list(x) for x in ap.ap]
    st, n = a[-1]
    a[-1] = [-st, n]
    return bass.AP(ap.tensor, ap.offset + st * (n - 1), a)


class Prog:
    def __init__(self, nc, es):
        self.nc = nc
        self.es = es
        self.E = {'pe': nc.tensor, 'act': nc.scalar, 'dve': nc.vector, 'pool': nc.gpsimd, 'sp': nc.sync}
        self.sems = {}
        self.cnt = {}
        self.waited = {e: {} for e in self.E}
        self.lastw = {}
        self.readers = {}
        self.q = {e: [] for e in self.E}

    def sem(self, name):
        if name not in self.sems:
            self.sems[name] = self.es.enter_context(self.nc.semaphore(name))
            self.cnt[name] = 0
        return self.sems[name]

    def _wait(self, eng, s, v):
        if self.waited[eng].get(s, 0) >= v:
            return
        self.waited[eng][s] = v
        self.q[eng].append(('wait', s, v))

    def _deps(self, eng, reads, writes):
        need = {}

        def add(tok):
            if tok is None:
                return
            s, v = tok
            if eng == 'pe' and s == 'c_pe':
                return
            if need.get(s, 0) < v:
                need[s] = v
        for k in reads:
            add(self.lastw.get(k))
        for k in writes:
            add(self.lastw.get(k))
            for s, v in self.readers.get(k, {}).items():
                add((s, v))
        for s, v in need.items():
            self._wait(eng, s, v)

    def _commit(self, tok, reads, writes):
        s, v = tok
        for k in writes:
            self.lastw[k] = tok
            self.readers[k] = {}
        for k in reads:
            r = self.readers.setdefault(k, {})
            if r.get(s, 0) < v:
                r[s] = v

    def op(self, eng, fn, reads=(), writes=()):
        self._deps(eng, reads, writes)
        s = 'c_' + eng
        self.sem(s)
        self.cnt[s] += 1
        self.q[eng].append(('op', fn, s, 1))
        self._commit((s, self.cnt[s]), reads, writes)

    def dma(self, eng, pairs, reads, writes, sem, **kw):
        self._deps(eng, reads, writes)
        self.sem(sem)
        self._wait(eng, sem, self.cnt[sem])
        for (o, i) in pairs:
            self.cnt[sem] += 16
            self.q[eng].append(('op', (lambda e, o=o, i=i: e.dma_start(out=o, in_=i, **kw)), sem, 16))
        self._commit((sem, self.cnt[sem]), reads, writes)

    def barrier(self):
        for eng in self.E:
            for s, v in self.cnt.items():
                if v > 0:
                    self._wait(eng, s, v)
        self.lastw.clear()
        self.readers.clear()

    def flush(self):
        sems = self.sems
        with self.nc.Block() as block:
            for eng, deco in (('pe', block.tensor), ('act', block.scalar), ('dve', block.vector),
                              ('pool', block.gpsimd), ('sp', block.sync)):
                items = self.q[eng]
                self.q[eng] = []
                if not items:
                    continue

                def body(e, items=items):
                    for it in items:
                        if it[0] == 'wait':
                            e.wait_ge(sems[it[1]], it[2])
                        else:
                            ins = it[1](e)
                            ins.then_inc(sems[it[2]], it[3])
                deco(body)


def build(stop_after=None, debug=False, start_layer=0):
    nc = bass.Bass("TRN2", target_bir_lowering=False)
    es = contextlib.ExitStack()
    P = Prog(nc, es)

    def din(name, shape, dt=F32):
        return nc.dram_tensor(name, list(shape), dt, kind="ExternalInput").ap()

    def dscr(name, shape, dt=F32):
        return nc.dram_tensor(name, list(shape), dt, kind=("ExternalOutput" if debug else "Internal")).ap()

    xs = din("xs", [2 * SEQ, D])
    ctxs = din("ctxs", [2 * CTX, D])
    c3T = din("c3T", [128, 48])
    adaw = din("adaw", [2 * D, 6 * D])
    adabT = din("adabT", [128, 192])
    ngT = din("ngT", [128, 64])
    fgrow = din("fgrow", [1, D])
    wine = din("wine", [D, 5120])
    cawT = din("cawT", [128, 24])
    cbwT = din("cbwT", [128, 32])
    cbbT = din("cbbT", [128, 8])
    lruw = din("lruw", [4096, 128])
    lrubT = din("lrubT", [128, 32])
    lamT = din("lamT", [128, 16])
    woute = din("woute", [D, D])
    wino = din("wino", [D, 6144])
    lamv = din("lamv", [1, 512])
    sublnT = din("sublnT", [128, 2])
    wouto = din("wouto", [D, D])
    rw = din("rw", [D, NE])
    rb = din("rb", [1, NE])
    ewg = din("ewg", [2 * NE * D, FF])
    ewu = din("ewu", [2 * NE * D, FF])
    ewd = din("ewd", [2 * NE * FF, D])
    identd = din("identd", [128, 128])
    ropec = din("ropec", [128, SEQ])
    ropes = din("ropes", [128, SEQ])
    rpermd = din("rpermd", [128, 128])

    outd = nc.dram_tensor("out", [2 * SEQ, D], F32, kind="ExternalOutput").ap()

    grow = dscr("grow", [2 * 2 * 3, D])
    mixT_d = dscr("mixT_d", [2 * 16 * 128, TOK], BF16)
    fT_d = dscr("fT_d", [2 * 128, 16 * TOK], BF16)
    x1_d = dscr("x1_d", [2 * TOK, D])
    x2_d = din("x2_d", [2 * TOK, D]) if start_layer == 1 else dscr("x2_d", [2 * TOK, D])
    x3_d = dscr("x3_d", [2 * SEQ, D])

    uid = [0]

    def sb(name, shape, dt=F32, stack=es):
        uid[0] += 1
        return stack.enter_context(nc.sbuf_tensor(f"{name}_{uid[0]}", list(shape), dt))

    def ps(name, shape, dt=F32, stack=es):
        uid[0] += 1
        return stack.enter_context(nc.psum_tensor(f"{name}_{uid[0]}", list(shape), dt))

    identf = sb("identf", [128, 128])
    identb = sb("identb", [128, 128], BF16)
    onesf = sb("onesf", [128, 128])
    onesb = sb("onesb", [128, 128], BF16)
    rpermb = sb("rpermb", [128, 128], BF16)
    rpermf = sb("rpermf", [128, 128])
    scT = sb("scT", [128, 48])
    scTb = sb("scTb", [128, 48], BF16)
    mhalf = sb("mhalf", [128, 1])
    adab = sb("adab", [128, 192])
    ng = sb("ng", [128, 64])
    modc = sb("modc", [128, 2, 4, 48])
    comb = sb("comb", [128, 2, NT, 16])
    caw = sb("caw", [128, 24])
    cbw = sb("cbw", [128, 32])
    cbb = sb("cbb", [128, 8])
    lrub = sb("lrub", [128, 32])
    cneg = sb("cneg", [128, 16])
    lamt = sb("lamt", [128, 16])
    rbb = sb("rbb", [128, 16])
    rwt = sb("rwt", [128, 16, 16])
    subg = sb("subg", [128, 2])
    neglam = sb("neglam", [128, 1])
    lamrow = sb("lamrow", [1, 512])
    lamtmp = sb("lamtmp", [1, 8])

    LAM_INIT = 0.8 - 0.6 * math.exp(-0.3 * 1)

    def phase_setup():
        with contextlib.ExitStack() as st:
            pl = ps("pl", [128, 512], stack=st)
            P.dma('sp', [(identf[:, :], identd), (scT[:, :], c3T), (adab[:, :], adabT), (ng[:, :], ngT),
                         (caw[:, :], cawT), (cbw[:, :], cbwT), (cbb[:, :], cbbT), (lrub[:, :], lrubT),
                         (lamt[:, :], lamT), (rbb[:, :], rb.partition_broadcast(128)),
                         (rwt[:, :, :], rw.rearrange("(kc p) e -> p kc e", p=128)),
                         (subg[:, :], sublnT), (lamrow[:, :], lamv), (rpermf[:, :], rpermd)],
                  [], ['consts'], 'ld_c')
            P.op('pool', lambda e: e.memset(onesf[:, :], 1.0), [], ['onesf'])
            P.op('pool', lambda e: e.memset(onesb[:, :], 1.0), [], ['onesb'])
            P.op('dve', lambda e: e.tensor_copy(out=identb[:, :], in_=identf[:, :]), ['consts'], ['identb'])
            P.op('dve', lambda e: e.tensor_copy(out=rpermb[:, :], in_=rpermf[:, :]), ['consts'], ['rpermb'])
            P.op('act', lambda e: e.activation(out=scT[:, :], in_=scT[:, :], func=AF.Silu), ['consts'], ['consts'])
            P.op('dve', lambda e: e.tensor_copy(out=scTb[:, :], in_=scT[:, :]), ['consts'], ['scTb'])
            P.op('pool', lambda e: e.memset(mhalf[:, :], -0.5), [], ['mhalf'])
            P.op('act', lambda e: e.activation(out=cneg[:, :], in_=lamt[:, :], func=AF.Exp, scale=-1.0), ['consts'], ['cneg'])
            P.op('act', lambda e: e.activation(out=cneg[:, :], in_=cneg[:, :], func=AF.Ln, bias=1.0), ['cneg'], ['cneg'])
            P.op('dve', lambda e: e.tensor_scalar(out=cneg[:, :], in0=cneg[:, :], scalar1=-8.0, scalar2=None, op0=ALU.mult),
                 ['cneg'], ['cneg'])
            P.op('dve', lambda e: e.tensor_scalar(out=subg[:, :], in0=subg[:, :], scalar1=(1.0 - LAM_INIT), scalar2=None, op0=ALU.mult),
                 ['consts'], ['consts'])
            P.op('dve', lambda e: e.tensor_tensor(out=lamrow[:, 0:128], in0=lamrow[:, 0:128], in1=lamrow[:, 128:256], op=ALU.mult),
                 ['consts'], ['lr1'])
            P.op('dve', lambda e: e.tensor_tensor(out=lamrow[:, 256:384], in0=lamrow[:, 256:384], in1=lamrow[:, 384:512], op=ALU.mult),
                 ['consts'], ['lr2'])
            P.op('dve', lambda e: e.tensor_reduce(out=lamtmp[:, 0:1], in_=lamrow[:, 0:128], axis=AX.X, op=ALU.add), ['lr1'], ['lt0'])
            P.op('dve', lambda e: e.tensor_reduce(out=lamtmp[:, 1:2], in_=lamrow[:, 256:384], axis=AX.X, op=ALU.add), ['lr2'], ['lt1'])
            P.op('act', lambda e: e.activation(out=lamtmp[:, 2:4], in_=lamtmp[:, 0:2], func=AF.Exp), ['lt0', 'lt1'], ['lt2'])
            P.op('dve', lambda e: e.scalar_tensor_tensor(out=lamtmp[:, 4:5], in0=lamtmp[:, 3:4], scalar=-LAM_INIT, in1=lamtmp[:, 2:3],
                                                         op0=ALU.add, op1=ALU.subtract), ['lt2'], ['lt4'])
            P.op('pe', lambda e: e.matmul(pl[:, 0:1], lhsT=onesf[0:1, :], rhs=lamtmp[0:1, 4:5], start=True, stop=True),
                 ['lt4', 'onesf'], ['pl'])
            P.op('act', lambda e: e.activation(out=neglam[:, :], in_=pl[:, 0:1], func=AF.Copy), ['pl'], ['neglam'])
            P.barrier()
            P.flush()

    def phase_ada(l):
        with contextlib.ExitStack() as st:
            wa = [sb(f"wa{i}", [128, 16, 384], BF16, stack=st) for i in range(2)]
            psA = ps("psA", [128, 512], stack=st)
            modT = sb("modT", [128, 96, 4], stack=st)
            gtmp = sb("gtmp", [128, 96], stack=st)
            psA3 = psA[:, 0:384].rearrange("p (j r) -> p j r", r=4)
            for piece in range(32):
                sl = piece % 2
                src = adaw[l * D:(l + 1) * D, piece * 384:(piece + 1) * 384].rearrange("(kc p) c -> p kc c", p=128)
                P.dma('pool', [(wa[sl][:, :, :], src)], [], [('wa', sl)], f'ld_wa{sl}')
                for cbk in range(3):
                    j = piece * 3 + cbk

                    def fn(e, j=j, cbk=cbk, sl=sl):
                        for kc in range(16):
                            ins = e.matmul(psA[:, j * 4:j * 4 + 3], lhsT=wa[sl][:, kc, cbk * 128:(cbk + 1) * 128],
                                           rhs=scTb[:, kc * 3:(kc + 1) * 3], start=(kc == 0), stop=(kc == 15))
                        return ins
                    P.op('pe', fn, [('wa', sl), 'scTb'], ['psA'])
            P.op('dve', lambda e: e.tensor_tensor(out=modT[:, :, 0:3], in0=psA3[:, :, 0:3], in1=bc(adab[:, l * 96:(l + 1) * 96], 3), op=ALU.add),
                 ['psA', 'consts'], ['modT'])
            for which, (sec_sc, sec_sh, gi) in enumerate([(1, 0, 0), (4, 3, 1)]):
                gsc = modc[:, l, 2 * which, :].rearrange("p (k r) -> p k r", r=3)
                sh = modc[:, l, 2 * which + 1, :].rearrange("p (k r) -> p k r", r=3)
                gcol = ng[:, (l * 2 + gi) * 16:(l * 2 + gi + 1) * 16]
                P.op('dve', lambda e, gsc=gsc, sec_sc=sec_sc: e.tensor_scalar(out=gsc, in0=modT[:, sec_sc * 16:(sec_sc + 1) * 16, 0:3],
                                                                              scalar1=1.0, scalar2=None, op0=ALU.add),
                     ['modT'], [('gsc', which)])
                P.op('dve', lambda e, gsc=gsc, gcol=gcol: e.tensor_tensor(out=gsc, in0=gsc, in1=bc(gcol, 3), op=ALU.mult),
                     [('gsc', which), 'consts'], [('gsc', which)])
                P.op('dve', lambda e, sh=sh, sec_sh=sec_sh: e.tensor_copy(out=sh, in_=modT[:, sec_sh * 16:(sec_sh + 1) * 16, 0:3]),
                     ['modT'], [('sh', which)])
            pairs = []
            for gi, sec in enumerate((2, 5)):
                for r in range(3):
                    row = (l * 2 + gi) * 3 + r
                    dst = grow[row:row + 1, :].rearrange("o (j p) -> p (o j)", p=128)
                    pairs.append((dst, modT[:, sec * 16:(sec + 1) * 16, r]))
            with nc.allow_non_contiguous_dma(reason="tiny modulation rows"):
                P.dma('sp', pairs, ['modT'], ['grow'], 'st_g')
                P.barrier()
                P.flush()

    def norm_tile(st_tiles, x_tile_key, xt, l, which, r, dst_fn, dst_key, tp, tp_key, npass=1, junk_key='junk', kp=''):
        junk, stat, xn = st_tiles
        gsc = modc[:, l, 2 * which, :]
        sh = modc[:, l, 2 * which + 1, :]
        P.op('act', lambda e: e.activation(out=junk[:, :], in_=xt[:, :], func=AF.Square, accum_out=stat[:, 0:1]),
             [x_tile_key], [(kp, junk_key), (kp, 'stat0')])
        P.op('dve', lambda e: e.tensor_scalar(out=stat[:, 1:2], in0=stat[:, 0:1], scalar1=1.0 / D, scalar2=EPS, op0=ALU.mult, op1=ALU.add),
             [(kp, 'stat0')], [(kp, 'stat1')])
        P.op('act', lambda e: e.activation(out=stat[:, 2:3], in_=stat[:, 1:2], func=AF.Sqrt), [(kp, 'stat1')], [(kp, 'stat2')])
        P.op('dve', lambda e: e.reciprocal(out=stat[:, 3:4], in_=stat[:, 2:3]), [(kp, 'stat2')], [(kp, 'stat3')])
        P.op('act', lambda e: e.activation(out=xn[:, :], in_=xt[:, :], func=AF.Copy, scale=stat[:, 3:4]),
             [x_tile_key, (kp, 'stat3')], [(kp, 'xn')])
        per = 16 // npass
        for ps_i in range(npass):
            def tfn(e, ps_i=ps_i):
                for k in range(per):
                    kc = ps_i * per + k
                    ins = e.transpose(tp[:, k * 128:(k + 1) * 128], xn[:, kc * 128:(kc + 1) * 128], identf[:, :])
                return ins
            P.op('pe', tfn, [(kp, 'xn'), 'consts'], [tp_key])

            def efn(e, ps_i=ps_i):
                for k in range(per):
                    kc = ps_i * per + k
                    ins = e.tensor_scalar(out=dst_fn(kc), in0=tp[:, k * 128:(k + 1) * 128],
                                          scalar1=gsc[:, kc * 3 + r:kc * 3 + r + 1], scalar2=sh[:, kc * 3 + r:kc * 3 + r + 1],
                                          op0=ALU.mult, op1=ALU.add)
                return ins
            P.op('dve', efn, [tp_key, ('gsc', which), ('sh', which)], [dst_key])

    def phase_norm1(l, s, hT, st):
        with contextlib.ExitStack() as st2:
            xt = [sb(f"n1xt{i}", [128, D], stack=st2) for i in range(2)]
            junk = [sb(f"n1junk{i}", [128, D], BF16, stack=st2) for i in range(2)]
            stat = [sb(f"n1stat{i}", [128, 4], stack=st2) for i in range(2)]
            xn = [sb(f"n1xn{i}", [128, D], stack=st2) for i in range(2)]
            tp = [ps(f"n1tp{i}", [128, D], stack=st2) for i in range(2)]
            for tt in range(NT):
                sl = tt % 2
                if l == 0:
                    src = ctxs[s * CTX + tt * 128: s * CTX + (tt + 1) * 128, :] if tt < 2 else \
                        xs[s * SEQ + (tt - 2) * 128: s * SEQ + (tt - 1) * 128, :]
                else:
                    src = x2_d[s * TOK + tt * 128: s * TOK + (tt + 1) * 128, :]
                r = 2 if tt < 2 else s
                P.dma('sp', [(xt[sl][:, :], src)], [], [('xt', sl)], f'ld_xt{sl}')
                norm_tile((junk[sl], stat[sl], xn[sl]), ('xt', sl), xt[sl], l, 0, r,
                          lambda kc, tt=tt: hT[:, kc, tt * 128:(tt + 1) * 128], ('hT', tt), tp[sl], ('tp', sl), kp=f'n{sl}')
            P.barrier()
            P.flush()

    BLKS = [(0, 256), (256, 768), (768, 1280), (1280, 1792), (1792, 2304)]

    def blk_tiles(t0, t1):
        return [('hT', t) for t in range(t0 // 128, t1 // 128)]

    def phase_even(s, hT):
        with contextlib.ExitStack() as st:
            w = sb("ew0", [128, 16, 5, 128], BF16, stack=st)
            lw = sb("elw", [128, 32, 128], BF16, stack=st)
            L = [sb(f"eL{i}", [128, TOK], (F32 if i == 4 else BF16), stack=st) for i in range(5)]
            B = [None if i == 3 else sb(f"eB{i}", [128, TOK], F32, stack=st) for i in range(7)]
            ubf = sb("eubf", [128, TOK], BF16, stack=st)
            outb = sb("eob0", [128, TOK], BF16, stack=st)
            pp = [ps(f"epp{i}", [128, 512], stack=st) for i in range(8)]
            P.dma('pool', [(lw[:, :, :], lruw.rearrange("(g i) j -> i g j", i=128))], [], ['lw'], 'ld_lw')
            bank = [0]

            def nextbank():
                b = bank[0]
                bank[0] = (b + 1) % 8
                return b
            SEGS = [(0, CTX), (CTX, TOK)]

            def load_w(j):
                pairs = []
                for sec in range(5):
                    col0 = sec * 1024 + j * 128
                    pairs.append((w[:, :, sec, :], wine[:, col0:col0 + 128].rearrange("(kc p) c -> p kc c", p=128)))
                P.dma('pool', pairs, [], ['w'], 'ld_ew0')

            def proj_groups(secs):
                out = []
                for sec in secs:
                    for (t0, t1) in BLKS:
                        def g(sec=sec, t0=t0, t1=t1):
                            b = nextbank()
                            n = t1 - t0

                            def fn(e):
                                for kc in range(16):
                                    ins = e.matmul(pp[b][:, 0:n], lhsT=w[:, kc, sec, :], rhs=hT[:, kc, t0:t1],
                                                   start=(kc == 0), stop=(kc == 15))
                                return ins
                            P.op('pe', fn, ['w'] + blk_tiles(t0, t1), [('pp', b)])
                            P.op('act', lambda e: e.activation(out=L[sec][:, t0:t1], in_=pp[b][:, 0:n], func=AF.Copy),
                                 [('pp', b)], [('L', sec)])
                        out.append(g)
                return out

            def early_chain(j):
                P.op('dve', lambda e: e.tensor_tensor(out=B[6][:, :], in0=L[3][:, :], in1=L[3][:, :], op=ALU.mult), [('L', 3)], [('B', 6)])
                P.op('dve', lambda e: e.tensor_scalar(out=B[6][:, :], in0=B[6][:, :], scalar1=0.044715, scalar2=1.0, op0=ALU.mult, op1=ALU.add),
                     [('B', 6)], [('B', 6)])
                P.op('dve', lambda e: e.tensor_tensor(out=B[6][:, :], in0=B[6][:, :], in1=L[3][:, :], op=ALU.mult), [('B', 6), ('L', 3)], [('B', 6)])
                P.op('act', lambda e: e.activation(out=B[6][:, :], in_=B[6][:, :], func=AF.Sigmoid, scale=1.5957691216057308),
                     [('B', 6)], [('B', 6)])
                P.op('dve', lambda e: e.tensor_tensor(out=L[3][:, :], in0=L[3][:, :], in1=B[6][:, :], op=ALU.mult), [('B', 6), ('L', 3)], [('L', 3)])
                P.op('dve', lambda e: e.tensor_tensor(out=B[1][:, :], in0=L[1][:, :], in1=L[2][:, :], op=ALU.mult), [('L', 1), ('L', 2)], [('B', 1)])
                P.op('dve', lambda e: e.tensor_scalar(out=B[2][:, :], in0=B[1][:, :], scalar1=caw[:, j * 3 + 1:j * 3 + 2], scalar2=None, op0=ALU.mult),
                     [('B', 1), 'consts'], [('B', 2)])
                for (a0, a1) in SEGS:
                    P.op('dve', lambda e, a0=a0, a1=a1: e.scalar_tensor_tensor(
                        out=B[2][:, a0 + 1:a1], in0=B[1][:, a0:a1 - 1], scalar=caw[:, j * 3:j * 3 + 1], in1=B[2][:, a0 + 1:a1],
                        op0=ALU.mult, op1=ALU.add), [('B', 1), ('B', 2)], [('B', 2)])
                    P.op('dve', lambda e, a0=a0, a1=a1: e.scalar_tensor_tensor(
                        out=B[2][:, a0:a1 - 1], in0=B[1][:, a0 + 1:a1], scalar=caw[:, j * 3 + 2:j * 3 + 3], in1=B[2][:, a0:a1 - 1],
                        op0=ALU.mult, op1=ALU.add), [('B', 1), ('B', 2)], [('B', 2)])
                P.op('pool', lambda e: e.tensor_tensor(out=outb[:, :], in0=L[0][:, :], in1=B[2][:, :], op=ALU.mult),
                     [('L', 0), ('B', 2)], ['ob'])
                P.dma('sp', [(mixT_d[(s * 16 + j) * 128:(s * 16 + j + 1) * 128, :], outb[:, :])], ['ob'], ['mixT_d'], 'st_ob0')
                P.op('dve', lambda e: e.tensor_scalar(out=B[5][:, :], in0=L[4][:, :], scalar1=cbw[:, j * 4 + 2:j * 4 + 3], scalar2=cbb[:, j:j + 1],
                                                      op0=ALU.mult, op1=ALU.add), [('L', 4), 'consts'], [('B', 5)])
                for (a0, a1) in SEGS:
                    for (kk, sh_) in ((0, 2), (1, 1)):
                        P.op('dve', lambda e, a0=a0, a1=a1, kk=kk, sh_=sh_: e.scalar_tensor_tensor(
                            out=B[5][:, a0 + sh_:a1], in0=L[4][:, a0:a1 - sh_], scalar=cbw[:, j * 4 + kk:j * 4 + kk + 1], in1=B[5][:, a0 + sh_:a1],
                            op0=ALU.mult, op1=ALU.add), [('L', 4), ('B', 5)], [('B', 5)])
                    P.op('dve', lambda e, a0=a0, a1=a1: e.scalar_tensor_tensor(
                        out=B[5][:, a0:a1 - 1], in0=L[4][:, a0 + 1:a1], scalar=cbw[:, j * 4 + 3:j * 4 + 4], in1=B[5][:, a0:a1 - 1],
                        op0=ALU.mult, op1=ALU.add), [('L', 4), ('B', 5)], [('B', 5)])
                P.op('pool', lambda e: e.tensor_copy(out=ubf[:, :], in_=B[5][:, :]), [('B', 5)], ['ubf'])

            def late_ops(j):
                ops = []
                for d in range(2):
                    for gate, dst in ((0, 0), (1, 1)):
                        g = (gate * 2 + d) * 8 + j
                        for (t0, t1) in BLKS:
                            def gg(g=g, t0=t0, t1=t1, dst=dst):
                                b = nextbank()
                                n = t1 - t0
                                P.op('pe', lambda e: e.matmul(pp[b][:, 0:n], lhsT=lw[:, g, :], rhs=ubf[:, t0:t1], start=True, stop=True),
                                     ['lw', 'ubf'], [('pp', b)])
                                P.op('act', lambda e: e.activation(out=B[dst][:, t0:t1], in_=pp[b][:, 0:n], func=AF.Sigmoid, bias=lrub[:, g:g + 1]),
                                     [('pp', b), 'consts'], [('B', dst)])
                            ops.append(gg)
                    cn = cneg[:, d * 8 + j:d * 8 + j + 1]
                    ops.append(lambda cn=cn: P.op('act', lambda e: e.activation(out=B[0][:, :], in_=B[0][:, :], func=AF.Exp, scale=cn), [('B', 0), 'cneg'], [('B', 0)]))
                    ops.append(lambda: P.op('act', lambda e: e.activation(out=B[2][:, :], in_=B[0][:, :], func=AF.Square), [('B', 0)], [('B', 2)]))
                    ops.append(lambda: P.op('act', lambda e: e.activation(out=B[2][:, :], in_=B[2][:, :], func=AF.Sqrt, scale=-1.0, bias=1.0), [('B', 2)], [('B', 2)]))
                    ops.append(lambda: P.op('dve', lambda e: e.tensor_tensor(out=B[1][:, :], in0=B[1][:, :], in1=B[5][:, :], op=ALU.mult), [('B', 1), ('B', 5)], [('B', 1)]))
                    ops.append(lambda: P.op('dve', lambda e: e.tensor_tensor(out=B[1][:, :], in0=B[1][:, :], in1=B[2][:, :], op=ALU.mult), [('B', 1), ('B', 2)], [('B', 1)]))
                    if d == 0:
                        ops.append(lambda: P.op('dve', lambda e: e.tensor_tensor_scan(out=B[4][:, :], data0=B[0][:, :], data1=B[1][:, :], initial=0.0,
                                                                                       op0=ALU.mult, op1=ALU.add), [('B', 0), ('B', 1)], [('B', 4)]))
                    else:
                        ops.append(lambda: P.op('dve', lambda e: e.tensor_tensor_scan(out=rev(B[6][:, 0:CTX]), data0=rev(B[0][:, 0:CTX]), data1=rev(B[1][:, 0:CTX]),
                                                                                       initial=0.0, op0=ALU.mult, op1=ALU.add), [('B', 0), ('B', 1)], [('B', 6)]))
                        ops.append(lambda: P.op('dve', lambda e: e.tensor_tensor_scan(out=rev(B[6][:, CTX:TOK]), data0=rev(B[0][:, CTX:TOK]), data1=rev(B[1][:, CTX:TOK]),
                                                                                       initial=B[6][:, 0:1], op0=ALU.mult, op1=ALU.add), [('B', 0), ('B', 1), ('B', 6)], [('B', 6)]))
                ops.append(lambda: P.op('pool', lambda e: e.tensor_tensor(out=B[4][:, :], in0=B[4][:, :], in1=B[6][:, :], op=ALU.add), [('B', 4), ('B', 6)], [('B', 4)]))
                ops.append(lambda: P.op('pool', lambda e: e.tensor_tensor(out=outb[:, :], in0=L[3][:, :], in1=B[4][:, :], op=ALU.mult),
                                        [('L', 3), ('B', 4)], ['ob']))
                ops.append(lambda: P.dma('sp', [(mixT_d[(s * 16 + 8 + j) * 128:(s * 16 + 9 + j) * 128, :], outb[:, :])], ['ob'], ['mixT_d'], 'st_ob0'))
                return ops

            load_w(0)
            for g in proj_groups((0, 1, 2, 4, 3)):
                g()
            for j in range(8):
                early_chain(j)
                late = late_ops(j)
                if j + 1 < 8:
                    load_w(j + 1)
                    pg = proj_groups((0, 1, 2, 4))
                    pg3 = proj_groups((3,))
                else:
                    pg, pg3 = [], []
                for op_ in late:
                    op_()
                    if pg:
                        pg.pop(0)()
                for g in pg:
                    g()
                for g in pg3:
                    g()
            P.barrier()
            P.flush()

    def phase_outproj(l, s):
        ntile = NT if l == 0 else 16
        with contextlib.ExitStack() as st:
            wo = sb("owo", [128, 16, D], BF16, stack=st)
            aTt = [sb(f"oaT{i}", [128, 16, 128], BF16, stack=st) for i in range(2)]
            g1b = [sb(f"og1b{i}", [128, D], stack=st) for i in range(2)]
            xt = [sb(f"oxt{i}", [128, D], stack=st) for i in range(2)]
            xn = [sb(f"oxn{i}", [128, D], stack=st) for i in range(2)]
            stat = [sb(f"ostat{i}", [128, 4], stack=st) for i in range(2)]
            f32t = [sb(f"of32t{i}", [128, 16, 128], stack=st) for i in range(2)]
            fTt = [sb(f"ofTt{i}", [128, 16, 128], BF16, stack=st) for i in range(2)]
            scores = sb("oscores", [128, NT, 4, 4], stack=st)
            yp = ps("oyp", [128, D], stack=st)
            tp = ps("otp", [128, 1024], stack=st)
            lg = ps("olg", [128, 512], stack=st)
            wsrc = (woute if l == 0 else wouto).rearrange("(kc p) c -> p kc c", p=128)
            P.dma('pool', [(wo[:, kc, :], wsrc[:, kc, :]) for kc in range(16)], [], ['wo'], 'ld_wo')
            rows = [(l * 2 + 0) * 3 + 2, (l * 2 + 0) * 3 + s]
            P.dma('sp', [(g1b[i][:, :], grow[rows[i]:rows[i] + 1, :].partition_broadcast(128)) for i in range(2)], [], ['g1b'], 'ld_g1b')
            fT3 = fT_d[s * 128:(s + 1) * 128, :].rearrange("p (kc t) -> p kc t", kc=16)
            asrc = mixT_d[s * 2048:(s + 1) * 2048, :].rearrange("(c p) t -> p c t", p=128)
            for tt in range(ntile):
                sl = tt % 2
                kp = f'o{sl}'
                if l == 0:
                    isctx = tt < 2
                    src = ctxs[s * CTX + tt * 128: s * CTX + (tt + 1) * 128, :] if isctx else \
                        xs[s * SEQ + (tt - 2) * 128: s * SEQ + (tt - 1) * 128, :]
                    dst = x1_d[s * TOK + tt * 128: s * TOK + (tt + 1) * 128, :]
                else:
                    isctx = False
                    src = x2_d[s * TOK + CTX + tt * 128: s * TOK + CTX + (tt + 1) * 128, :]
                    dst = x3_d[s * SEQ + tt * 128: s * SEQ + (tt + 1) * 128, :]
                ftcol = tt * 128
                r = 2 if isctx else s
                gb_ = g1b[0] if isctx else g1b[1]
                a_, xt_, xn_, f32_, fT_, stat_ = aTt[sl], xt[sl], xn[sl], f32t[sl], fTt[sl], stat[sl]
                junk = fT_[:, :, :].rearrange("p a b -> p (a b)")
                P.dma('sp', [(a_[:, :, :], asrc[:, :, tt * 128:(tt + 1) * 128])], [], [('aT', sl)], f'ld_aT{sl}')
                P.dma('sp', [(xt_[:, :], src)], [], [('xt', sl)], f'ld_oxt{sl}')

                def yfn(e, a_=a_):
                    for cb in range(4):
                        for kc in range(16):
                            ins = e.matmul(yp[:, cb * 512:(cb + 1) * 512], lhsT=a_[:, kc, :],
                                           rhs=wo[:, kc, cb * 512:(cb + 1) * 512], start=(kc == 0), stop=(kc == 15))
                    return ins
                P.op('pe', yfn, [('aT', sl), 'wo'], ['yp'])
                P.op('dve', lambda e, gb_=gb_, xn_=xn_: e.tensor_tensor(out=xn_[:, :], in0=yp[:, :], in1=gb_[:, :], op=ALU.mult), ['yp', 'g1b'], [(kp, 'xn')])
                P.op('dve', lambda e, xn_=xn_, xt_=xt_: e.tensor_tensor(out=xt_[:, :], in0=xn_[:, :], in1=xt_[:, :], op=ALU.add), [(kp, 'xn'), ('xt', sl)], [('xt', sl)])
                P.dma('sp', [(dst, xt_[:, :])], [('xt', sl)], [('x1_d', sl)], f'st_x1{sl}')
                norm_tile((junk, stat_, xn_), ('xt', sl), xt_, l, 1, r, lambda kc, f32_=f32_: f32_[:, kc, :], ('f32t', sl), tp, 'tp', npass=2,
                          junk_key='fTt', kp=kp)
                P.op('pool', lambda e, fT_=fT_, f32_=f32_: e.tensor_copy(out=fT_[:, :, :], in_=f32_[:, :, :]), [('f32t', sl)], [(kp, 'fTt')])
                P.dma('sp', [(fT3[:, :, ftcol:ftcol + 128], fT_[:, :, :])], [(kp, 'fTt')], [('fT_d', sl)], f'st_fT{sl}')

                def lfn(e, f32_=f32_):
                    for kc in range(16):
                        ins = e.matmul(lg[:, 0:16], lhsT=f32_[:, kc, :], rhs=rwt[:, kc, :], start=(kc == 0), stop=(kc == 15))
                    return ins
                P.op('pe', lfn, [('f32t', sl), 'consts'], ['lg'])
                P.op('act', lambda e, tt=tt: e.activation(out=scores[:, tt, :, :].rearrange("p a b -> p (a b)"), in_=lg[:, 0:16], func=AF.Sigmoid),
                     ['lg'], ['scores'])
            routing(st, scores, ntile, s)
            P.barrier()
            P.flush()

    def routing(st, scores, T, s):
        sel = sb("r_sel", [128, NT, 4, 4], stack=st)
        sel2 = sb("r_sel2", [128, NT, 4, 4], stack=st)
        m1k = sb("r_m1k", [128, NT, 4, 4], stack=st)
        m2k = sb("r_m2k", [128, NT, 4, 4], stack=st)
        t1 = sb("r_t1", [128, NT, 4], stack=st)
        gs = sb("r_gs", [128, NT, 4], stack=st)
        gmask = sb("r_gmask", [128, NT, 4], stack=st)
        red = sb("r_red", [128, NT], stack=st)
        k = ['rt']

        def V(fn):
            P.op('dve', fn, ['scores', 'consts'] + k, k)
        sc_ = scores[:, 0:T]
        sl_, s2_, a1_, a2_ = sel[:, 0:T], sel2[:, 0:T], m1k[:, 0:T], m2k[:, 0:T]
        t1_, gs_, gm_, rd_ = t1[:, 0:T], gs[:, 0:T], gmask[:, 0:T], red[:, 0:T]

        def f16(a):
            return a.rearrange("p t a b -> p t (a b)")
        V(lambda e: e.tensor_tensor(out=f16(sl_), in0=f16(sc_), in1=bc_mid(rbb[:, :], T), op=ALU.add))
        pairs = [(0, 1), (0, 2), (0, 3), (1, 2), (1, 3), (2, 3)]
        for i, (a, b) in enumerate(pairs):
            dst = gs_ if i == 0 else t1_
            V(lambda e, a=a, b=b, dst=dst: e.tensor_tensor(out=dst, in0=sl_[:, :, :, a], in1=sl_[:, :, :, b], op=ALU.add))
            if i > 0:
                V(lambda e: e.tensor_tensor(out=gs_, in0=gs_, in1=t1_, op=ALU.max))
        V(lambda e: e.tensor_reduce(out=rd_, in_=gs_, axis=AX.X, op=ALU.max))
        V(lambda e: e.tensor_tensor(out=gm_, in0=gs_, in1=bc(rd_, 4), op=ALU.is_equal))
        V(lambda e: e.tensor_scalar(out=f16(s2_), in0=f16(sl_), scalar1=2.0, scalar2=None, op0=ALU.add))
        V(lambda e: e.tensor_tensor(out=s2_, in0=s2_, in1=bc(gm_, 4), op=ALU.mult))
        V(lambda e: e.tensor_reduce(out=rd_, in_=f16(s2_), axis=AX.X, op=ALU.max))
        V(lambda e: e.tensor_tensor(out=f16(a1_), in0=f16(s2_), in1=bc(rd_, 16), op=ALU.is_equal))
        V(lambda e: e.scalar_tensor_tensor(out=f16(s2_), in0=f16(a1_), scalar=-4.0, in1=f16(s2_), op0=ALU.mult, op1=ALU.add))
        V(lambda e: e.tensor_reduce(out=rd_, in_=f16(s2_), axis=AX.X, op=ALU.max))
        V(lambda e: e.tensor_tensor(out=f16(a2_), in0=f16(s2_), in1=bc(rd_, 16), op=ALU.is_equal))
        V(lambda e: e.tensor_tensor(out=f16(a1_), in0=f16(a1_), in1=f16(a2_), op=ALU.add))
        V(lambda e: e.tensor_tensor(out=f16(a1_), in0=f16(a1_), in1=f16(sc_), op=ALU.mult))
        V(lambda e: e.tensor_reduce(out=rd_, in_=f16(a1_), axis=AX.X, op=ALU.add))
        V(lambda e: e.reciprocal(out=rd_, in_=rd_))
        P.op('dve', lambda e: e.tensor_tensor(out=comb[:, s, 0:T, :], in0=f16(a1_), in1=bc(rd_, 16), op=ALU.mult), k, [('comb', s)])

    def phase_moe(l, s):
        if l == 0:
            TB, NBK, NSUB, SUBW, ntok = 1152, 2, 3, 384, TOK
        else:
            TB, NBK, NSUB, SUBW, ntok = 1024, 2, 2, 512, SEQ
        ntl = TB // 128
        with contextlib.ExitStack() as st:
            acc = sb("macc", [128, ntl, D], stack=st)
            fT = sb("mfT", [128, 16, TB], BF16, stack=st)
            WGU = [(sb(f"mwg{i}", [128, 16, 256], BF16, stack=st), sb(f"mwu{i}", [128, 16, 256], BF16, stack=st)) for i in range(2)]
            WD = [sb(f"mwd{i}", [128, 2, D], BF16, stack=st) for i in range(3)]
            hid = [sb(f"mhid{i}", [128, 2, TB], BF16, stack=st) for i in range(2)]
            sg = [sb(f"msg{i}", [128, 512], stack=st) for i in range(2)]
            g2b = [sb(f"mg2b{i}", [128, D], stack=st) for i in range(2)]
            stat = sb("mstat", [128, 8], stack=st)
            if l == 1:
                junk = sb("mjunk", [128, D], BF16, stack=st)
                fgb = sb("mfgb", [128, D], stack=st)
            gps = [ps(f"mgps{i}", [128, 512], stack=st) for i in range(2)]
            ups = [ps(f"mups{i}", [128, 512], stack=st) for i in range(2)]
            ops_ = [ps(f"mops{i}", [128, 1024], stack=st) for i in range(2)]
            rows = [(l * 2 + 1) * 3 + 2, (l * 2 + 1) * 3 + s]
            P.dma('sp', [(g2b[i][:, :], grow[rows[i]:rows[i] + 1, :].partition_broadcast(128)) for i in range(2)], [], ['g2b'], 'ld_g2b')
            if l == 1:
                P.dma('sp', [(fgb[:, :], fgrow.partition_broadcast(128))], [], ['fgb'], 'ld_fgb')
            fT3 = fT_d[s * 128:(s + 1) * 128, :].rearrange("p (kc t) -> p kc t", kc=16)
            ucount = [0]
            gi = [0]
            for bk in range(NBK):
                tok0 = bk * TB
                P.dma('sp', [(fT[:, :, 0:TB], fT3[:, :, tok0:tok0 + TB])], [], ['fT'], 'ld_mfT')
                prev_wd = []
                for ex in range(NE):
                    for q in range(4):
                        u = ucount[0]
                        ucount[0] += 1
                        sl = u % 2
                        sl3 = u % 3
                        wg, wu = WGU[sl]
                        wd = WD[sl3]
                        rg = (l * NE + ex) * D
                        rd = (l * NE + ex) * FF + q * 256
                        P.dma('pool', [(wg[:, :, :], ewg[rg:rg + D, q * 256:(q + 1) * 256].rearrange("(kc p) c -> p kc c", p=128)),
                                       (wu[:, :, :], ewu[rg:rg + D, q * 256:(q + 1) * 256].rearrange("(kc p) c -> p kc c", p=128))],
                              [], [('Wg', sl), ('Wu', sl)], f'ld_mw{sl}')
                        P.dma('pool', [(wd[:, :, :], ewd[rd:rd + 256, :].rearrange("(fc p) c -> p fc c", p=128))],
                              [], [('Wd', sl3)], f'ld_md{sl3}')
                        hs = hid[sl]

                        def gu_item(fc, nb, mid_cb, sl=sl, wg=wg, wu=wu, hs=hs):
                            i = gi[0] % 2
                            gi[0] += 1
                            c0 = nb * SUBW

                            def gfn(e):
                                for kc in range(16):
                                    ins = e.matmul(gps[i][:, 0:SUBW], lhsT=wg[:, kc, fc * 128:(fc + 1) * 128], rhs=fT[:, kc, c0:c0 + SUBW],
                                                   start=(kc == 0), stop=(kc == 15))
                                return ins

                            def ufn(e):
                                for kc in range(16):
                                    ins = e.matmul(ups[i][:, 0:SUBW], lhsT=wu[:, kc, fc * 128:(fc + 1) * 128], rhs=fT[:, kc, c0:c0 + SUBW],
                                                   start=(kc == 0), stop=(kc == 15))
                                return ins
                            P.op('pe', gfn, [('Wg', sl), 'fT'], [('gps', i)])
                            P.op('act', lambda e: e.activation(out=sg[i][:, 0:SUBW], in_=gps[i][:, 0:SUBW], func=AF.Silu),
                                 [('gps', i)], [('sg', i)])
                            mid_cb()
                            P.op('pe', ufn, [('Wu', sl), 'fT'], [('ups', i)])
                            P.op('dve', lambda e: e.tensor_tensor(out=hs[:, fc, c0:c0 + SUBW], in0=ups[i][:, 0:SUBW],
                                                                  in1=sg[i][:, 0:SUBW], op=ALU.mult),
                                 [('ups', i), ('sg', i)], [('hid', sl)])

                        def wd_item(tt, half, sl=sl, sl3=sl3, hs=hs, wd=wd, ex=ex, first=(ex == 0 and q == 0)):
                            gt = bk * ntl + tt

                            def ofn(e):
                                for fc in range(2):
                                    for c2 in range(2):
                                        cb = half * 2 + c2
                                        ins = e.matmul(ops_[half][:, c2 * 512:(c2 + 1) * 512], lhsT=hs[:, fc, tt * 128:(tt + 1) * 128],
                                                       rhs=wd[:, fc, cb * 512:(cb + 1) * 512], start=(fc == 0), stop=(fc == 1))
                                return ins
                            P.op('pe', ofn, [('hid', sl), ('Wd', sl3)], [('ops', half)])
                            if first:
                                P.op('dve', lambda e: e.tensor_scalar(
                                    out=acc[:, tt, half * 1024:(half + 1) * 1024], in0=ops_[half][:, :], scalar1=comb[:, s, gt, ex:ex + 1],
                                    scalar2=None, op0=ALU.mult),
                                    [('ops', half), ('comb', s)], [('acc', tt, half)])
                            else:
                                P.op('dve', lambda e: e.scalar_tensor_tensor(
                                    out=acc[:, tt, half * 1024:(half + 1) * 1024], in0=ops_[half][:, :], scalar=comb[:, s, gt, ex:ex + 1],
                                    in1=acc[:, tt, half * 1024:(half + 1) * 1024], op0=ALU.mult, op1=ALU.add),
                                    [('ops', half), ('comb', s), ('acc', tt, half)], [('acc', tt, half)])
                        gu_list = [(fc, nb) for fc in range(2) for nb in range(NSUB)]
                        nslots = 2 * len(gu_list)
                        nwd = len(prev_wd)
                        slot_i = [0]

                        def drain_slot():
                            k = slot_i[0]
                            slot_i[0] += 1
                            cnt = ((k + 1) * nwd) // nslots - (k * nwd) // nslots
                            for _ in range(cnt):
                                if prev_wd:
                                    prev_wd.pop(0)()
                        for (fc, nb) in gu_list:
                            gu_item(fc, nb, drain_slot)
                            drain_slot()
                        while prev_wd:
                            prev_wd.pop(0)()
                        prev_wd = [(lambda tt=tt, half=half, f=wd_item: f(tt, half)) for tt in range(ntl) for half in range(2)]
                while prev_wd:
                    prev_wd.pop(0)()
                sl_last = (ucount[0] - 1) % 2
                xbuf = [WGU[sl_last][i][:, :, :].bitcast(F32).rearrange("p a b -> p (a b)") for i in range(2)]
                for tt in range(ntl):
                    gt = bk * ntl + tt
                    xi = tt % 2
                    xb_ = xbuf[xi]
                    gk = ('Wg', sl_last) if xi == 0 else ('Wu', sl_last)
                    if l == 0:
                        isctx = gt < 2
                        src = x1_d[s * TOK + gt * 128: s * TOK + (gt + 1) * 128, :]
                        dst = x2_d[s * TOK + gt * 128: s * TOK + (gt + 1) * 128, :]
                    else:
                        isctx = False
                        src = x3_d[s * SEQ + gt * 128: s * SEQ + (gt + 1) * 128, :]
                        dst = outd[s * SEQ + gt * 128: s * SEQ + (gt + 1) * 128, :]
                    gb_ = g2b[0] if isctx else g2b[1]
                    P.dma('sp', [(xb_, src)], [], [('x1t', xi), gk], f'ld_mx{xi}')
                    P.op('dve', lambda e, tt=tt, gb_=gb_: e.tensor_tensor(out=acc[:, tt, :], in0=acc[:, tt, :], in1=gb_[:, :], op=ALU.mult),
                         [('acc', tt, 0), ('acc', tt, 1), 'g2b'], [('acc', tt, 0), ('acc', tt, 1)])
                    P.op('pool', lambda e, tt=tt, xb_=xb_: e.tensor_tensor(out=xb_, in0=acc[:, tt, :], in1=xb_, op=ALU.add),
                         [('acc', tt, 0), ('acc', tt, 1), ('x1t', xi)], [('x1t', xi)])
                    if l == 1:
                        P.op('act', lambda e, xb_=xb_, xi=xi: e.activation(out=junk[:, :], in_=xb_, func=AF.Square, accum_out=stat[:, xi * 4:xi * 4 + 1]),
                             [('x1t', xi)], ['junk', ('st0', xi)])
                        P.op('act', lambda e, xi=xi: e.activation(out=stat[:, xi * 4 + 1:xi * 4 + 2], in_=stat[:, xi * 4:xi * 4 + 1], func=AF.Ln, scale=1.0 / D, bias=EPS),
                             [('st0', xi)], [('st1', xi)])
                        P.op('act', lambda e, xi=xi: e.activation(out=stat[:, xi * 4 + 2:xi * 4 + 3], in_=stat[:, xi * 4 + 1:xi * 4 + 2], func=AF.Exp, scale=-0.5),
                             [('st1', xi)], [('st2', xi)])
                        P.op('act', lambda e, xb_=xb_, xi=xi: e.activation(out=xb_, in_=xb_, func=AF.Copy, scale=stat[:, xi * 4 + 2:xi * 4 + 3]),
                             [('x1t', xi), ('st2', xi)], [('x1t', xi)])
                        P.op('pool', lambda e, xb_=xb_: e.tensor_tensor(out=xb_, in0=xb_, in1=fgb[:, :], op=ALU.mult), [('x1t', xi), 'fgb'], [('x1t', xi)])
                    P.dma('sp', [(dst, xb_)], [('x1t', xi), gk], [('mout', xi)], f'st_mo{xi}')
            P.barrier()
            P.flush()

    def phase_attn(s, hT):
        SCALE = 128 ** -0.5
        with contextlib.ExitStack() as st:
            w = [sb(f"aw{i}", [128, 16, 768], BF16, stack=st) for i in range(2)]
            qT = sb("aqT", [128, 2, SEQ], BF16, stack=st)
            kT = sb("akT", [128, 2, TOK], BF16, stack=st)
            V_ = sb("aV", [128, NT, 256], BF16, stack=st)
            cosT = sb("acos", [128, SEQ], stack=st)
            sinT = sb("asin", [128, SEQ], stack=st)
            qb16 = sb("aqb16", [128, 512], BF16, stack=st)
            t1 = sb("at1", [128, 512], stack=st)
            t2 = sb("at2", [128, 512], stack=st)
            PT = [sb(f"aPT{i}", [128, 512], BF16, stack=st) for i in range(2)]
            rs = sb("ars", [128, 512], stack=st)
            oc = [sb(f"aoc{i}", [128, 512], stack=st) for i in range(2)]
            otmp = sb("aotmp", [128, 512], stack=st)
            sq = [sb(f"asq{i}", [128, 512], stack=st) for i in range(2)]
            ofst = [sb(f"aofst{i}", [128, 512], BF16, stack=st) for i in range(2)]
            A = [ps(f"aA{i}", [128, 512], stack=st) for i in range(2)]
            O = [[ps(f"aO{i}_{c}", [128, 512], stack=st) for c in range(3)] for i in range(2)]
            P.dma('sp', [(cosT[:, :], ropec), (sinT[:, :], ropes)], [], ['rope'], 'ld_rope')
            ai = [0]

            def nextA():
                a = ai[0] % 2
                ai[0] += 1
                return a
            oset = [0]
            LATB = [(0, 512), (512, 1024), (1024, 1536), (1536, 2048)]
            ATT_STAGE = int(os.environ.get('ATT_STAGE', '9'))
            pending = []
            for hd in range(int(os.environ.get('ATT_HEADS', '8'))):
                sl = hd % 2
                P.dma('pool', [(w[sl][:, :, sec * 256:(sec + 1) * 256],
                                wino[:, sec * 2048 + hd * 256: sec * 2048 + (hd + 1) * 256].rearrange("(kc p) c -> p kc c", p=128))
                               for sec in range(3)], [], [('w', sl)], f'ld_aw{sl}')
                for sec, dstT in ((0, qT), (1, kT)):
                    for m in range(2):
                        wc0 = sec * 256 + m * 128
                        if sec == 1:
                            a = nextA()

                            def fnc(e, a=a, wc0=wc0, sl=sl):
                                for kc in range(16):
                                    ins = e.matmul(A[a][:, 0:256], lhsT=w[sl][:, kc, wc0:wc0 + 128], rhs=hT[:, kc, 0:256], start=(kc == 0), stop=(kc == 15))
                                return ins
                            P.op('pe', fnc, [('w', sl), ('hT', 0), ('hT', 1)], [('A', a)])
                            P.op('act', lambda e, a=a, m=m: e.activation(out=kT[:, m, 0:256], in_=A[a][:, 0:256], func=AF.Copy), [('A', a)], [('kT', m)])
                        for (l0, l1) in LATB:
                            a = nextA()
                            a2 = nextA()
                            toff = 0 if sec == 0 else CTX

                            def fnp(e, a=a, wc0=wc0, sl=sl, l0=l0, l1=l1):
                                for kc in range(16):
                                    ins = e.matmul(A[a][:, :], lhsT=w[sl][:, kc, wc0:wc0 + 128], rhs=hT[:, kc, CTX + l0:CTX + l1], start=(kc == 0), stop=(kc == 15))
                                return ins
                            P.op('pe', fnp, [('w', sl)] + blk_tiles(CTX + l0, CTX + l1), [('A', a)])
                            P.op('act', lambda e, a=a: e.activation(out=qb16[:, :], in_=A[a][:, :], func=AF.Copy), [('A', a)], ['qb16'])
                            P.op('dve', lambda e, a=a, l0=l0, l1=l1: e.tensor_tensor(out=t1[:, :], in0=A[a][:, :], in1=cosT[:, l0:l1], op=ALU.mult),
                                 [('A', a), 'rope', 'qb16'], ['t1'])
                            P.op('pe', lambda e, a2=a2: e.matmul(A[a2][:, :], lhsT=rpermb[:, :], rhs=qb16[:, :], start=True, stop=True),
                                 ['qb16', 'rpermb'], [('A', a2)])
                            P.op('dve', lambda e, a2=a2, l0=l0, l1=l1: e.tensor_tensor(out=t2[:, :], in0=A[a2][:, :], in1=sinT[:, l0:l1], op=ALU.mult),
                                 [('A', a2), 'rope'], ['t2'])
                            P.op('pool', lambda e, dstT=dstT, m=m, toff=toff, l0=l0, l1=l1: e.tensor_tensor(
                                out=dstT[:, m, toff + l0:toff + l1], in0=t1[:, :], in1=t2[:, :], op=ALU.add),
                                ['t1', 't2'], [('qk', sec, m)] if sec == 0 else [('kT', m)])
                for tt in range(NT):
                    a = nextA()

                    def fnv(e, a=a, tt=tt, sl=sl):
                        for kc in range(16):
                            ins = e.matmul(A[a][:, 0:256], lhsT=hT[:, kc, tt * 128:(tt + 1) * 128], rhs=w[sl][:, kc, 512:768], start=(kc == 0), stop=(kc == 15))
                        return ins
                    P.op('pe', fnv, [('w', sl), ('hT', tt)], [('A', a)])
                    P.op('act', lambda e, a=a, tt=tt: e.activation(out=V_[:, tt, :], in_=A[a][:, 0:256], func=AF.Copy), [('A', a)], ['V'])
                for qb in range(4 if ATT_STAGE >= 2 else 0):
                    q0 = qb * 512
                    for m in range(2):
                        os_ = oset[0] % 2
                        oset[0] += 1
                        Oc = O[os_]
                        def emitS(kt, m=m, q0=q0):
                            a = nextA()
                            P.op('pe', lambda e, a=a, m=m, kt=kt, q0=q0: e.matmul(A[a][:, :], lhsT=kT[:, m, kt * 128:(kt + 1) * 128], rhs=qT[:, m, q0:q0 + 512],
                                                                                    start=True, stop=True), [('kT', m), ('qk', 0, m)], [('A', a)])
                            return a
                        a_next = emitS(0)
                        for kt in range(NT):
                            a = a_next
                            if kt + 1 < NT:
                                a_next = emitS(kt + 1)
                            P.op('act', lambda e, a=a: e.activation(out=PT[a][:, :], in_=A[a][:, :], func=AF.Exp, scale=SCALE), [('A', a)], [('PT', a)])

                            def fno(e, a=a, kt=kt, Oc=Oc):
                                e.matmul(Oc[0][:, :], lhsT=V_[:, kt, 0:128], rhs=PT[a][:, :], start=(kt == 0), stop=(kt == NT - 1))
                                e.matmul(Oc[1][:, :], lhsT=V_[:, kt, 128:256], rhs=PT[a][:, :], start=(kt == 0), stop=(kt == NT - 1))
                                return e.matmul(Oc[2][:, :], lhsT=onesb[:, :], rhs=PT[a][:, :], start=(kt == 0), stop=(kt == NT - 1))
                            P.op('pe', fno, ['V', ('PT', a), 'onesb'], [('O', os_)])
                            if kt == 3 and pending:
                                pending.pop(0)()
                        P.op('dve', lambda e, Oc=Oc: e.reciprocal(out=rs[:, :], in_=Oc[2][:, :]), [('O', os_)], ['rs'])
                        if m == 0:
                            for c in range(2):
                                P.op('dve', lambda e, c=c, Oc=Oc: e.tensor_tensor(out=oc[c][:, :], in0=Oc[c][:, :], in1=rs[:, :], op=ALU.mult),
                                     [('O', os_), 'rs'], [('oc', c)])
                        else:
                            P.op('dve', lambda e: e.tensor_scalar(out=rs[:, :], in0=rs[:, :], scalar1=neglam[:, 0:1], scalar2=None, op0=ALU.mult),
                                 ['rs', 'neglam'], ['rs'])
                            for c in range(2):
                                P.op('dve', lambda e, c=c, Oc=Oc: e.tensor_tensor(out=otmp[:, :], in0=Oc[c][:, :], in1=rs[:, :], op=ALU.mult),
                                     [('O', os_), 'rs'], ['otmp'])
                                P.op('pool', lambda e, c=c: e.tensor_tensor(out=oc[c][:, :], in0=oc[c][:, :], in1=otmp[:, :], op=ALU.add),
                                     [('oc', c), 'otmp'], [('oc', c)])
                    def tail(q0=q0, hd=hd):
                        for c in range(2):
                            P.op('act', lambda e, c=c: e.activation(out=sq[c][:, :], in_=oc[c][:, :], func=AF.Square), [('oc', c)], [('sq', c)])
                        a = nextA()
                        def fnn(e, a=a):
                            e.matmul(A[a][:, :], lhsT=onesf[:, :], rhs=sq[0][:, :], start=True, stop=False)
                            return e.matmul(A[a][:, :], lhsT=onesf[:, :], rhs=sq[1][:, :], start=False, stop=True)
                        P.op('pe', fnn, [('sq', 0), ('sq', 1), 'onesf'], [('A', a)])
                        P.op('dve', lambda e, a=a: e.tensor_scalar(out=rs[:, :], in0=A[a][:, :], scalar1=1.0 / 256, scalar2=EPS, op0=ALU.mult, op1=ALU.add),
                             [('A', a)], ['rs'])
                        P.op('act', lambda e: e.activation(out=rs[:, :], in_=rs[:, :], func=AF.Sqrt), ['rs'], ['rs'])
                        P.op('dve', lambda e: e.reciprocal(out=rs[:, :], in_=rs[:, :]), ['rs'], ['rs'])
                        for c in range(2):
                            P.op('dve', lambda e, c=c: e.scalar_tensor_tensor(out=ofst[c][:, :], in0=oc[c][:, :], scalar=subg[:, c:c + 1], in1=rs[:, :],
                                                                              op0=ALU.mult, op1=ALU.mult), [('oc', c), 'rs', 'consts'], [('ofst', c)])
                            ch = hd * 2 + c
                            P.dma('sp', [(mixT_d[(s * 16 + ch) * 128:(s * 16 + ch + 1) * 128, q0:q0 + 512], ofst[c][:, :])], [('ofst', c)], ['mixT_d'], f'st_of{c}')
                        nextA()
                    if ATT_STAGE >= 3:
                        pending.append(tail)
                while pending:
                    pending.pop(0)()
            P.barrier()
            P.flush()

    stages = []
    phase_setup()
    stages.append('setup')
    done = [False]

    def chk(name):
        if stop_after == name:
            done[0] = True
        return done[0]

    for l in range(start_layer, 2):
        if done[0]:
            break
        phase_ada(l)
        if chk(f'ada{l}'):
            break
        for s in range(2):
            with contextlib.ExitStack() as sth:
                hT = sb(f"hT_{l}_{s}", [128, 16, TOK], BF16, stack=sth)
                phase_norm1(l, s, hT, sth)
                if chk(f'norm1_{l}_{s}'):
                    break
                if l == 0:
                    phase_even(s, hT)
                else:
                    phase_attn(s, hT)
            if chk(f'mix_{l}_{s}'):
                break
            phase_outproj(l, s)
            if chk(f'outproj_{l}_{s}'):
                break
            phase_moe(l, s)
            if chk(f'moe_{l}_{s}'):
                break
    P.barrier()
    P.flush()
    es.close()
    return nc


def _fm(v, n=16):
    return np.ascontiguousarray(np.asarray(v, np.float32).reshape(n, 128).T)


def prep_inputs(inputs):
    g = {k: np.asarray(v) for k, v in inputs.items()}
    rep = {}
    rep['adaw'] = np.ascontiguousarray(g['ada_w'].reshape(2 * D, 6 * D))
    rep['adabT'] = np.ascontiguousarray(np.concatenate([_fm(g['ada_b'][l], 96) for l in range(2)], axis=1))
    rep['ngT'] = np.ascontiguousarray(np.concatenate([_fm(g['norm_mix_g'][0]), _fm(g['norm_ffn_g'][0]),
                                                       _fm(g['norm_mix_g'][1]), _fm(g['norm_ffn_g'][1])], axis=1))
    rep['fgrow'] = np.ascontiguousarray(g['final_g'].reshape(1, D))
    rep['wine'] = np.ascontiguousarray(g['w_in_e'][0])
    rep['cawT'] = np.ascontiguousarray(g['conv_a_w'][0].reshape(3, 8, 128).transpose(2, 1, 0).reshape(128, 24))
    rep['cbwT'] = np.ascontiguousarray(g['conv_b_w'][0].reshape(4, 8, 128).transpose(2, 1, 0).reshape(128, 32))
    rep['cbbT'] = _fm(g['conv_b_b'][0], 8)
    rep['lruw'] = np.ascontiguousarray(np.stack([g['lru_wa'][0], g['lru_wi'][0]], axis=0).reshape(4096, 128))
    rep['lrubT'] = np.ascontiguousarray(np.stack([g['lru_ba'][0], g['lru_bi'][0]], axis=0).reshape(2, 2, 8, 128).transpose(3, 0, 1, 2).reshape(128, 32))
    rep['lamT'] = np.ascontiguousarray(g['lru_lam'][0].reshape(2, 8, 128).transpose(2, 0, 1).reshape(128, 16))
    rep['woute'] = np.ascontiguousarray(g['w_out_e'][0])
    rep['wino'] = np.ascontiguousarray(g['w_in_o'][0])
    rep['lamv'] = np.ascontiguousarray(np.concatenate([g['lam_q1'][0], g['lam_k1'][0], g['lam_q2'][0], g['lam_k2'][0]]).reshape(1, 512))
    rep['sublnT'] = _fm(g['subln_g'][0], 2)
    rep['wouto'] = np.ascontiguousarray(g['w_out_o'][0])
    rep['rw'] = np.ascontiguousarray(g['router_w'])
    rep['rb'] = np.ascontiguousarray(g['router_b'].reshape(1, NE))
    rep['ewg'] = np.ascontiguousarray(g['exp_w_gate'].reshape(2 * NE * D, FF))
    rep['ewu'] = np.ascontiguousarray(g['exp_w_up'].reshape(2 * NE * D, FF))
    rep['ewd'] = np.ascontiguousarray(g['exp_w_down'].reshape(2 * NE * FF, D))
    rep['identd'] = np.eye(128, dtype=np.float32)
    t = np.arange(SEQ)
    row = (t // 64).astype(np.float32)
    col = (t % 64).astype(np.float32)
    inv = (10000.0 ** (-np.arange(0, 64, 2, dtype=np.float32) / 64)).astype(np.float32)
    ang_r = row[:, None] * inv
    ang_c = col[:, None] * inv
    ang = np.concatenate([ang_r, ang_r, ang_c, ang_c], axis=-1)
    rep['ropec'] = np.ascontiguousarray(np.cos(ang).astype(np.float32).T)
    rep['ropes'] = np.ascontiguousarray(np.sin(ang).astype(np.float32).T)
    rp = np.zeros((128, 128), np.float32)
    for m in range(128):
        if (m % 64) < 32:
            rp[m + 32, m] = -1.0
        else:
            rp[m - 32, m] = 1.0
    rep['rpermd'] = rp
    maps = []
    for c in range(8):
        mp = dict(rep)
        mp['xs'] = np.ascontiguousarray(g['x'][2 * c:2 * c + 2].reshape(2 * SEQ, D))
        mp['ctxs'] = np.ascontiguousarray(g['ctx'][2 * c:2 * c + 2].reshape(2 * CTX, D))
        c3 = np.stack([g['c'][2 * c], g['c'][2 * c + 1], g['c_ctx']], axis=0)
        mp['c3T'] = np.ascontiguousarray(c3.reshape(3, 16, 128).transpose(2, 1, 0).reshape(128, 48))
        maps.append(mp)
    return maps


def kernel(**inputs):
    maps = prep_inputs(inputs)
    nc = build()
    res = run_bass_kernel_spmd(nc, maps, core_ids=list(range(8)))
    out = np.stack([r["out"].reshape(2, SEQ, D) for r in res.results], axis=0).reshape(16, SEQ, D)
    return out.astype(np.float32)
```

```python
import math
import os
import contextlib
import numpy as np
import concourse.bass as bass
import concourse.mybir as mybir
from concourse.bass_utils import run_bass_kernel_spmd

F32 = mybir.dt.float32
BF16 = mybir.dt.bfloat16
ALU = mybir.AluOpType
AF = mybir.ActivationFunctionType
AX = mybir.AxisListType

D = 2048
SEQ = 2048
CTX = 256
TOK = SEQ + CTX
NT = TOK // 128
EPS = 1e-6
NE = 16
FF = 1024


def bc(ap, n):
    return bass.AP(ap.tensor, ap.offset, [list(x) for x in ap.ap] + [[0, n]])


def bc_mid(ap, n):
    a = [list(x) for x in ap.ap]
    return bass.AP(ap.tensor, ap.offset, [a[0], [0, n]] + a[1:])


def rev(ap):
    a = [list(x) for x in ap.ap]
    st, n = a[-1]
    a[-1] = [-st, n]
    return bass.AP(ap.tensor, ap.offset + st * (n - 1), a)


class Prog:
    def __init__(self, nc, es):
        self.nc = nc
        self.es = es
        self.E = {'pe': nc.tensor, 'act': nc.scalar, 'dve': nc.vector, 'pool': nc.gpsimd, 'sp': nc.sync}
        self.sems = {}
        self.cnt = {}
        self.waited = {e: {} for e in self.E}
        self.lastw = {}
        self.readers = {}
        self.q = {e: [] for e in self.E}

    def sem(self, name):
        if name not in self.sems:
            self.sems[name] = self.es.enter_context(self.nc.semaphore(name))
            self.cnt[name] = 0
        return self.sems[name]

    def _wait(self, eng, s, v):
        if self.waited[eng].get(s, 0) >= v:
            return
        self.waited[eng][s] = v
        self.q[eng].append(('wait', s, v))

    def _deps(self, eng, reads, writes):
        need = {}

        def add(tok):
            if tok is None:
                return
            s, v = tok
            if eng == 'pe' and s == 'c_pe':
                return
            if need.get(s, 0) < v:
                need[s] = v
        for k in reads:
            add(self.lastw.get(k))
        for k in writes:
            add(self.lastw.get(k))
            for s, v in self.readers.get(k, {}).items():
                add((s, v))
        for s, v in need.items():
            self._wait(eng, s, v)

    def _commit(self, tok, reads, writes):
        s, v = tok
        for k in writes:
            self.lastw[k] = tok
            self.readers[k] = {}
        for k in reads:
            r = self.readers.setdefault(k, {})
            if r.get(s, 0) < v:
                r[s] = v

    def op(self, eng, fn, reads=(), writes=()):
        self._deps(eng, reads, writes)
        s = 'c_' + eng
        self.sem(s)
        self.cnt[s] += 1
        self.q[eng].append(('op', fn, s, 1))
        self._commit((s, self.cnt[s]), reads, writes)

    def dma(self, eng, pairs, reads, writes, sem, **kw):
        self._deps(eng, reads, writes)
        self.sem(sem)
        self._wait(eng, sem, self.cnt[sem])
        for (o, i) in pairs:
            self.cnt[sem] += 16
            self.q[eng].append(('op', (lambda e, o=o, i=i: e.dma_start(out=o, in_=i, **kw)), sem, 16))
        self._commit((sem, self.cnt[sem]), reads, writes)

    def barrier(self):
        for eng in self.E:
            for s, v in self.cnt.items():
                if v > 0:
                    self._wait(eng, s, v)
        self.lastw.clear()
        self.readers.clear()

    def flush(self):
        sems = self.sems
        with self.nc.Block() as block:
            for eng, deco in (('pe', block.tensor), ('act', block.scalar), ('dve', block.vector),
                              ('pool', block.gpsimd), ('sp', block.sync)):
                items = self.q[eng]
                self.q[eng] = []
                if not items:
                    continue

                def body(e, items=items):
                    for it in items:
                        if it[0] == 'wait':
                            e.wait_ge(sems[it[1]], it[2])
                        else:
                            ins = it[1](e)
                            ins.then_inc(sems[it[2]], it[3])
                deco(body)


def build(stop_after=None, debug=False, start_layer=0):
    nc = bass.Bass("TRN2", target_bir_lowering=False)
    es = contextlib.ExitStack()
    P = Prog(nc, es)

    def din(name, shape, dt=F32):
        return nc.dram_tensor(name, list(shape), dt, kind="ExternalInput").ap()

    def dscr(name, shape, dt=F32):
        return nc.dram_tensor(name, list(shape), dt, kind=("ExternalOutput" if debug else "Internal")).ap()

    xs = din("xs", [2 * SEQ, D])
    ctxs = din("ctxs", [2 * CTX, D])
    c3T = din("c3T", [128, 48])
    adaw = din("adaw", [2 * D, 6 * D])
    adabT = din("adabT", [128, 192])
    ngT = din("ngT", [128, 64])
    fgrow = din("fgrow", [1, D])
    wine = din("wine", [D, 5120])
    cawT = din("cawT", [128, 24])
    cbwT = din("cbwT", [128, 32])
    cbbT = din("cbbT", [128, 8])
    lruw = din("lruw", [4096, 128])
    lrubT = din("lrubT", [128, 32])
    lamT = din("lamT", [128, 16])
    woute = din("woute", [D, D])
    wino = din("wino", [D, 6144])
    lamv = din("lamv", [1, 512])
    sublnT = din("sublnT", [128, 2])
    wouto = din("wouto", [D, D])
    rw = din("rw", [D, NE])
    rb = din("rb", [1, NE])
    ewg = din("ewg", [2 * NE * D, FF])
    ewu = din("ewu", [2 * NE * D, FF])
    ewd = din("ewd", [2 * NE * FF, D])
    identd = din("identd", [128, 128])
    ropec = din("ropec", [128, SEQ])
    ropes = din("ropes", [128, SEQ])
    rpermd = din("rpermd", [128, 128])

    outd = nc.dram_tensor("out", [2 * SEQ, D], F32, kind="ExternalOutput").ap()

    grow = dscr("grow", [2 * 2 * 3, D])
    mixT_d = dscr("mixT_d", [2 * 16 * 128, TOK], BF16)
    fT_d = dscr("fT_d", [2 * 128, 16 * TOK], BF16)
    x1_d = dscr("x1_d", [2 * TOK, D])
    x2_d = din("x2_d", [2 * TOK, D]) if start_layer == 1 else dscr("x2_d", [2 * TOK, D])
    x3_d = dscr("x3_d", [2 * SEQ, D])

    uid = [0]

    def sb(name, shape, dt=F32, stack=es):
        uid[0] += 1
        return stack.enter_context(nc.sbuf_tensor(f"{name}_{uid[0]}", list(shape), dt))

    def ps(name, shape, dt=F32, stack=es):
        uid[0] += 1
        return stack.enter_context(nc.psum_tensor(f"{name}_{uid[0]}", list(shape), dt))

    identf = sb("identf", [128, 128])
    identb = sb("identb", [128, 128], BF16)
    onesf = sb("onesf", [128, 128])
    onesb = sb("onesb", [128, 128], BF16)
    rpermb = sb("rpermb", [128, 128], BF16)
    rpermf = sb("rpermf", [128, 128])
    scT = sb("scT", [128, 48])
    scTb = sb("scTb", [128, 48], BF16)
    mhalf = sb("mhalf", [128, 1])
    adab = sb("adab", [128, 192])
    ng = sb("ng", [128, 64])
    modc = sb("modc", [128, 2, 4, 48])
    comb = sb("comb", [128, 2, NT, 16])
    caw = sb("caw", [128, 24])
    cbw = sb("cbw", [128, 32])
    cbb = sb("cbb", [128, 8])
    lrub = sb("lrub", [128, 32])
    cneg = sb("cneg", [128, 16])
    lamt = sb("lamt", [128, 16])
    rbb = sb("rbb", [128, 16])
    rwt = sb("rwt", [128, 16, 16])
    subg = sb("subg", [128, 2])
    neglam = sb("neglam", [128, 1])
    lamrow = sb("lamrow", [1, 512])
    lamtmp = sb("lamtmp", [1, 8])

    LAM_INIT = 0.8 - 0.6 * math.exp(-0.3 * 1)

    def phase_setup():
        with contextlib.ExitStack() as st:
            pl = ps("pl", [128, 512], stack=st)
            P.dma('sp', [(identf[:, :], identd), (scT[:, :], c3T), (adab[:, :], adabT), (ng[:, :], ngT),
                         (caw[:, :], cawT), (cbw[:, :], cbwT), (cbb[:, :], cbbT), (lrub[:, :], lrubT),
                         (lamt[:, :], lamT), (rbb[:, :], rb.partition_broadcast(128)),
                         (rwt[:, :, :], rw.rearrange("(kc p) e -> p kc e", p=128)),
                         (subg[:, :], sublnT), (lamrow[:, :], lamv), (rpermf[:, :], rpermd)],
                  [], ['consts'], 'ld_c')
            P.op('pool', lambda e: e.memset(onesf[:, :], 1.0), [], ['onesf'])
            P.op('pool', lambda e: e.memset(onesb[:, :], 1.0), [], ['onesb'])
            P.op('dve', lambda e: e.tensor_copy(out=identb[:, :], in_=identf[:, :]), ['consts'], ['identb'])
            P.op('dve', lambda e: e.tensor_copy(out=rpermb[:, :], in_=rpermf[:, :]), ['consts'], ['rpermb'])
            P.op('act', lambda e: e.activation(out=scT[:, :], in_=scT[:, :], func=AF.Silu), ['consts'], ['consts'])
            P.op('dve', lambda e: e.tensor_copy(out=scTb[:, :], in_=scT[:, :]), ['consts'], ['scTb'])
            P.op('pool', lambda e: e.memset(mhalf[:, :], -0.5), [], ['mhalf'])
            P.op('act', lambda e: e.activation(out=cneg[:, :], in_=lamt[:, :], func=AF.Exp, scale=-1.0), ['consts'], ['cneg'])
            P.op('act', lambda e: e.activation(out=cneg[:, :], in_=cneg[:, :], func=AF.Ln, bias=1.0), ['cneg'], ['cneg'])
            P.op('dve', lambda e: e.tensor_scalar(out=cneg[:, :], in0=cneg[:, :], scalar1=-8.0, scalar2=None, op0=ALU.mult),
                 ['cneg'], ['cneg'])
            P.op('dve', lambda e: e.tensor_scalar(out=subg[:, :], in0=subg[:, :], scalar1=(1.0 - LAM_INIT), scalar2=None, op0=ALU.mult),
                 ['consts'], ['consts'])
            P.op('dve', lambda e: e.tensor_tensor(out=lamrow[:, 0:128], in0=lamrow[:, 0:128], in1=lamrow[:, 128:256], op=ALU.mult),
                 ['consts'], ['lr1'])
            P.op('dve', lambda e: e.tensor_tensor(out=lamrow[:, 256:384], in0=lamrow[:, 256:384], in1=lamrow[:, 384:512], op=ALU.mult),
                 ['consts'], ['lr2'])
            P.op('dve', lambda e: e.tensor_reduce(out=lamtmp[:, 0:1], in_=lamrow[:, 0:128], axis=AX.X, op=ALU.add), ['lr1'], ['lt0'])
            P.op('dve', lambda e: e.tensor_reduce(out=lamtmp[:, 1:2], in_=lamrow[:, 256:384], axis=AX.X, op=ALU.add), ['lr2'], ['lt1'])
            P.op('act', lambda e: e.activation(out=lamtmp[:, 2:4], in_=lamtmp[:, 0:2], func=AF.Exp), ['lt0', 'lt1'], ['lt2'])
            P.op('dve', lambda e: e.scalar_tensor_tensor(out=lamtmp[:, 4:5], in0=lamtmp[:, 3:4], scalar=-LAM_INIT, in1=lamtmp[:, 2:3],
                                                         op0=ALU.add, op1=ALU.subtract), ['lt2'], ['lt4'])
            P.op('pe', lambda e: e.matmul(pl[:, 0:1], lhsT=onesf[0:1, :], rhs=lamtmp[0:1, 4:5], start=True, stop=True),
                 ['lt4', 'onesf'], ['pl'])
            P.op('act', lambda e: e.activation(out=neglam[:, :], in_=pl[:, 0:1], func=AF.Copy), ['pl'], ['neglam'])
            P.barrier()
            P.flush()

    def phase_ada(l):
        with contextlib.ExitStack() as st:
            wa = [sb(f"wa{i}", [128, 16, 384], BF16, stack=st) for i in range(2)]
            psA = ps("psA", [128, 512], stack=st)
            modT = sb("modT", [128, 96, 4], stack=st)
            gtmp = sb("gtmp", [128, 96], stack=st)
            psA3 = psA[:, 0:384].rearrange("p (j r) -> p j r", r=4)
            for piece in range(32):
                sl = piece % 2
                src = adaw[l * D:(l + 1) * D, piece * 384:(piece + 1) * 384].rearrange("(kc p) c -> p kc c", p=128)
                P.dma('pool', [(wa[sl][:, :, :], src)], [], [('wa', sl)], f'ld_wa{sl}')
                for cbk in range(3):
                    j = piece * 3 + cbk

                    def fn(e, j=j, cbk=cbk, sl=sl):
                        for kc in range(16):
                            ins = e.matmul(psA[:, j * 4:j * 4 + 3], lhsT=wa[sl][:, kc, cbk * 128:(cbk + 1) * 128],
                                           rhs=scTb[:, kc * 3:(kc + 1) * 3], start=(kc == 0), stop=(kc == 15))
                        return ins
                    P.op('pe', fn, [('wa', sl), 'scTb'], ['psA'])
            P.op('dve', lambda e: e.tensor_tensor(out=modT[:, :, 0:3], in0=psA3[:, :, 0:3], in1=bc(adab[:, l * 96:(l + 1) * 96], 3), op=ALU.add),
                 ['psA', 'consts'], ['modT'])
            for which, (sec_sc, sec_sh, gi) in enumerate([(1, 0, 0), (4, 3, 1)]):
                gsc = modc[:, l, 2 * which, :].rearrange("p (k r) -> p k r", r=3)
                sh = modc[:, l, 2 * which + 1, :].rearrange("p (k r) -> p k r", r=3)
                gcol = ng[:, (l * 2 + gi) * 16:(l * 2 + gi + 1) * 16]
                P.op('dve', lambda e, gsc=gsc, sec_sc=sec_sc: e.tensor_scalar(out=gsc, in0=modT[:, sec_sc * 16:(sec_sc + 1) * 16, 0:3],
                                                                              scalar1=1.0, scalar2=None, op0=ALU.add),
                     ['modT'], [('gsc', which)])
                P.op('dve', lambda e, gsc=gsc, gcol=gcol: e.tensor_tensor(out=gsc, in0=gsc, in1=bc(gcol, 3), op=ALU.mult),
                     [('gsc', which), 'consts'], [('gsc', which)])
                P.op('dve', lambda e, sh=sh, sec_sh=sec_sh: e.tensor_copy(out=sh, in_=modT[:, sec_sh * 16:(sec_sh + 1) * 16, 0:3]),
                     ['modT'], [('sh', which)])
            pairs = []
            for gi, sec in enumerate((2, 5)):
                for r in range(3):
                    row = (l * 2 + gi) * 3 + r
                    dst = grow[row:row + 1, :].rearrange("o (j p) -> p (o j)", p=128)
                    pairs.append((dst, modT[:, sec * 16:(sec + 1) * 16, r]))
            with nc.allow_non_contiguous_dma(reason="tiny modulation rows"):
                P.dma('sp', pairs, ['modT'], ['grow'], 'st_g')
                P.barrier()
                P.flush()

    def norm_tile(st_tiles, x_tile_key, xt, l, which, r, dst_fn, dst_key, tp, tp_key, npass=1, junk_key='junk', kp=''):
        junk, stat, xn = st_tiles
        gsc = modc[:, l, 2 * which, :]
        sh = modc[:, l, 2 * which + 1, :]
        P.op('act', lambda e: e.activation(out=junk[:, :], in_=xt[:, :], func=AF.Square, accum_out=stat[:, 0:1]),
             [x_tile_key], [(kp, junk_key), (kp, 'stat0')])
        P.op('dve', lambda e: e.tensor_scalar(out=stat[:, 1:2], in0=stat[:, 0:1], scalar1=1.0 / D, scalar2=EPS, op0=ALU.mult, op1=ALU.add),
             [(kp, 'stat0')], [(kp, 'stat1')])
        P.op('act', lambda e: e.activation(out=stat[:, 2:3], in_=stat[:, 1:2], func=AF.Sqrt), [(kp, 'stat1')], [(kp, 'stat2')])
        P.op('dve', lambda e: e.reciprocal(out=stat[:, 3:4], in_=stat[:, 2:3]), [(kp, 'stat2')], [(kp, 'stat3')])
        P.op('act', lambda e: e.activation(out=xn[:, :], in_=xt[:, :], func=AF.Copy, scale=stat[:, 3:4]),
             [x_tile_key, (kp, 'stat3')], [(kp, 'xn')])
        per = 16 // npass
        for ps_i in range(npass):
            def tfn(e, ps_i=ps_i):
                for k in range(per):
                    kc = ps_i * per + k
                    ins = e.transpose(tp[:, k * 128:(k + 1) * 128], xn[:, kc * 128:(kc + 1) * 128], identf[:, :])
                return ins
            P.op('pe', tfn, [(kp, 'xn'), 'consts'], [tp_key])

            def efn(e, ps_i=ps_i):
                for k in range(per):
                    kc = ps_i * per + k
                    ins = e.tensor_scalar(out=dst_fn(kc), in0=tp[:, k * 128:(k + 1) * 128],
                                          scalar1=gsc[:, kc * 3 + r:kc * 3 + r + 1], scalar2=sh[:, kc * 3 + r:kc * 3 + r + 1],
                                          op0=ALU.mult, op1=ALU.add)
                return ins
            P.op('dve', efn, [tp_key, ('gsc', which), ('sh', which)], [dst_key])

    def phase_norm1(l, s, hT, st):
        with contextlib.ExitStack() as st2:
            xt = [sb(f"n1xt{i}", [128, D], stack=st2) for i in range(2)]
            junk = [sb(f"n1junk{i}", [128, D], BF16, stack=st2) for i in range(2)]
            stat = [sb(f"n1stat{i}", [128, 4], stack=st2) for i in range(2)]
            xn = [sb(f"n1xn{i}", [128, D], stack=st2) for i in range(2)]
            tp = [ps(f"n1tp{i}", [128, D], stack=st2) for i in range(2)]
            for tt in range(NT):
                sl = tt % 2
                if l == 0:
                    src = ctxs[s * CTX + tt * 128: s * CTX + (tt + 1) * 128, :] if tt < 2 else \
                        xs[s * SEQ + (tt - 2) * 128: s * SEQ + (tt - 1) * 128, :]
                else:
                    src = x2_d[s * TOK + tt * 128: s * TOK + (tt + 1) * 128, :]
                r = 2 if tt < 2 else s
                P.dma('sp', [(xt[sl][:, :], src)], [], [('xt', sl)], f'ld_xt{sl}')
                norm_tile((junk[sl], stat[sl], xn[sl]), ('xt', sl), xt[sl], l, 0, r,
                          lambda kc, tt=tt: hT[:, kc, tt * 128:(tt + 1) * 128], ('hT', tt), tp[sl], ('tp', sl), kp=f'n{sl}')
            P.barrier()
            P.flush()

    BLKS = [(0, 256), (256, 768), (768, 1280), (1280, 1792), (1792, 2304)]

    def blk_tiles(t0, t1):
        return [('hT', t) for t in range(t0 // 128, t1 // 128)]

    def phase_even(s, hT):
        with contextlib.ExitStack() as st:
            w = sb("ew0", [128, 16, 5, 128], BF16, stack=st)
            lw = sb("elw", [128, 32, 128], BF16, stack=st)
            L = [sb(f"eL{i}", [128, TOK], (F32 if i == 4 else BF16), stack=st) for i in range(5)]
            B = [None if i == 3 else sb(f"eB{i}", [128, TOK], F32, stack=st) for i in range(7)]
            ubf = sb("eubf", [128, TOK], BF16, stack=st)
            outb = sb("eob0", [128, TOK], BF16, stack=st)
            pp = [ps(f"epp{i}", [128, 512], stack=st) for i in range(8)]
            P.dma('pool', [(lw[:, :, :], lruw.rearrange("(g i) j -> i g j", i=128))], [], ['lw'], 'ld_lw')
            bank = [0]

            def nextbank():
                b = bank[0]
                bank[0] = (b + 1) % 8
                return b
            SEGS = [(0, CTX), (CTX, TOK)]

            def load_w(j):
                pairs = []
                for sec in range(5):
                    col0 = sec * 1024 + j * 128
                    pairs.append((w[:, :, sec, :], wine[:, col0:col0 + 128].rearrange("(kc p) c -> p kc c", p=128)))
                P.dma('pool', pairs, [], ['w'], 'ld_ew0')

            def proj_groups(secs):
                out = []
                for sec in secs:
                    for (t0, t1) in BLKS:
                        def g(sec=sec, t0=t0, t1=t1):
                            b = nextbank()
                            n = t1 - t0

                            def fn(e):
                                for kc in range(16):
                                    ins = e.matmul(pp[b][:, 0:n], lhsT=w[:, kc, sec, :], rhs=hT[:, kc, t0:t1],
                                                   start=(kc == 0), stop=(kc == 15))
                                return ins
                            P.op('pe', fn, ['w'] + blk_tiles(t0, t1), [('pp', b)])
                            P.op('act', lambda e: e.activation(out=L[sec][:, t0:t1], in_=pp[b][:, 0:n], func=AF.Copy),
                                 [('pp', b)], [('L', sec)])
                        out.append(g)
                return out

            def early_chain(j):
                P.op('dve', lambda e: e.tensor_tensor(out=B[6][:, :], in0=L[3][:, :], in1=L[3][:, :], op=ALU.mult), [('L', 3)], [('B', 6)])
                P.op('dve', lambda e: e.tensor_scalar(out=B[6][:, :], in0=B[6][:, :], scalar1=0.044715, scalar2=1.0, op0=ALU.mult, op1=ALU.add),
                     [('B', 6)], [('B', 6)])
                P.op('dve', lambda e: e.tensor_tensor(out=B[6][:, :], in0=B[6][:, :], in1=L[3][:, :], op=ALU.mult), [('B', 6), ('L', 3)], [('B', 6)])
                P.op('act', lambda e: e.activation(out=B[6][:, :], in_=B[6][:, :], func=AF.Sigmoid, scale=1.5957691216057308),
                     [('B', 6)], [('B', 6)])
                P.op('dve', lambda e: e.tensor_tensor(out=L[3][:, :], in0=L[3][:, :], in1=B[6][:, :], op=ALU.mult), [('B', 6), ('L', 3)], [('L', 3)])
                P.op('dve', lambda e: e.tensor_tensor(out=B[1][:, :], in0=L[1][:, :], in1=L[2][:, :], op=ALU.mult), [('L', 1), ('L', 2)], [('B', 1)])
                P.op('dve', lambda e: e.tensor_scalar(out=B[2][:, :], in0=B[1][:, :], scalar1=caw[:, j * 3 + 1:j * 3 + 2], scalar2=None, op0=ALU.mult),
                     [('B', 1), 'consts'], [('B', 2)])
                for (a0, a1) in SEGS:
                    P.op('dve', lambda e, a0=a0, a1=a1: e.scalar_tensor_tensor(
                        out=B[2][:, a0 + 1:a1], in0=B[1][:, a0:a1 - 1], scalar=caw[:, j * 3:j * 3 + 1], in1=B[2][:, a0 + 1:a1],
                        op0=ALU.mult, op1=ALU.add), [('B', 1), ('B', 2)], [('B', 2)])
                    P.op('dve', lambda e, a0=a0, a1=a1: e.scalar_tensor_tensor(
                        out=B[2][:, a0:a1 - 1], in0=B[1][:, a0 + 1:a1], scalar=caw[:, j * 3 + 2:j * 3 + 3], in1=B[2][:, a0:a1 - 1],
                        op0=ALU.mult, op1=ALU.add), [('B', 1), ('B', 2)], [('B', 2)])
                P.op('pool', lambda e: e.tensor_tensor(out=outb[:, :], in0=L[0][:, :], in1=B[2][:, :], op=ALU.mult),
                     [('L', 0), ('B', 2)], ['ob'])
                P.dma('sp', [(mixT_d[(s * 16 + j) * 128:(s * 16 + j + 1) * 128, :], outb[:, :])], ['ob'], ['mixT_d'], 'st_ob0')
                P.op('dve', lambda e: e.tensor_scalar(out=B[5][:, :], in0=L[4][:, :], scalar1=cbw[:, j * 4 + 2:j * 4 + 3], scalar2=cbb[:, j:j + 1],
                                                      op0=ALU.mult, op1=ALU.add), [('L', 4), 'consts'], [('B', 5)])
                for (a0, a1) in SEGS:
                    for (kk, sh_) in ((0, 2), (1, 1)):
                        P.op('dve', lambda e, a0=a0, a1=a1, kk=kk, sh_=sh_: e.scalar_tensor_tensor(
                            out=B[5][:, a0 + sh_:a1], in0=L[4][:, a0:a1 - sh_], scalar=cbw[:, j * 4 + kk:j * 4 + kk + 1], in1=B[5][:, a0 + sh_:a1],
                            op0=ALU.mult, op1=ALU.add), [('L', 4), ('B', 5)], [('B', 5)])
                    P.op('dve', lambda e, a0=a0, a1=a1: e.scalar_tensor_tensor(
                        out=B[5][:, a0:a1 - 1], in0=L[4][:, a0 + 1:a1], scalar=cbw[:, j * 4 + 3:j * 4 + 4], in1=B[5][:, a0:a1 - 1],
                        op0=ALU.mult, op1=ALU.add), [('L', 4), ('B', 5)], [('B', 5)])
                P.op('pool', lambda e: e.tensor_copy(out=ubf[:, :], in_=B[5][:, :]), [('B', 5)], ['ubf'])

            def late_ops(j):
                ops = []
                for d in range(2):
                    for gate, dst in ((0, 0), (1, 1)):
                        g = (gate * 2 + d) * 8 + j
                        for (t0, t1) in BLKS:
                            def gg(g=g, t0=t0, t1=t1, dst=dst):
                                b = nextbank()
                                n = t1 - t0
                                P.op('pe', lambda e: e.matmul(pp[b][:, 0:n], lhsT=lw[:, g, :], rhs=ubf[:, t0:t1], start=True, stop=True),
                                     ['lw', 'ubf'], [('pp', b)])
                                P.op('act', lambda e: e.activation(out=B[dst][:, t0:t1], in_=pp[b][:, 0:n], func=AF.Sigmoid, bias=lrub[:, g:g + 1]),
                                     [('pp', b), 'consts'], [('B', dst)])
                            ops.append(gg)
                    cn = cneg[:, d * 8 + j:d * 8 + j + 1]
                    ops.append(lambda cn=cn: P.op('act', lambda e: e.activation(out=B[0][:, :], in_=B[0][:, :], func=AF.Exp, scale=cn), [('B', 0), 'cneg'], [('B', 0)]))
                    ops.append(lambda: P.op('act', lambda e: e.activation(out=B[2][:, :], in_=B[0][:, :], func=AF.Square), [('B', 0)], [('B', 2)]))
                    ops.append(lambda: P.op('act', lambda e: e.activation(out=B[2][:, :], in_=B[2][:, :], func=AF.Sqrt, scale=-1.0, bias=1.0), [('B', 2)], [('B', 2)]))
                    ops.append(lambda: P.op('dve', lambda e: e.tensor_tensor(out=B[1][:, :], in0=B[1][:, :], in1=B[5][:, :], op=ALU.mult), [('B', 1), ('B', 5)], [('B', 1)]))
                    ops.append(lambda: P.op('dve', lambda e: e.tensor_tensor(out=B[1][:, :], in0=B[1][:, :], in1=B[2][:, :], op=ALU.mult), [('B', 1), ('B', 2)], [('B', 1)]))
                    if d == 0:
                        ops.append(lambda: P.op('dve', lambda e: e.tensor_tensor_scan(out=B[4][:, :], data0=B[0][:, :], data1=B[1][:, :], initial=0.0,
                                                                                       op0=ALU.mult, op1=ALU.add), [('B', 0), ('B', 1)], [('B', 4)]))
                    else:
                        ops.append(lambda: P.op('dve', lambda e: e.tensor_tensor_scan(out=rev(B[6][:, 0:CTX]), data0=rev(B[0][:, 0:CTX]), data1=rev(B[1][:, 0:CTX]),
                                                                                       initial=0.0, op0=ALU.mult, op1=ALU.add), [('B', 0), ('B', 1)], [('B', 6)]))
                        ops.append(lambda: P.op('dve', lambda e: e.tensor_tensor_scan(out=rev(B[6][:, CTX:TOK]), data0=rev(B[0][:, CTX:TOK]), data1=rev(B[1][:, CTX:TOK]),
                                                                                       initial=B[6][:, 0:1], op0=ALU.mult, op1=ALU.add), [('B', 0), ('B', 1), ('B', 6)], [('B', 6)]))
                ops.append(lambda: P.op('pool', lambda e: e.tensor_tensor(out=B[4][:, :], in0=B[4][:, :], in1=B[6][:, :], op=ALU.add), [('B', 4), ('B', 6)], [('B', 4)]))
                ops.append(lambda: P.op('pool', lambda e: e.tensor_tensor(out=outb[:, :], in0=L[3][:, :], in1=B[4][:, :], op=ALU.mult),
                                        [('L', 3), ('B', 4)], ['ob']))
                ops.append(lambda: P.dma('sp', [(mixT_d[(s * 16 + 8 + j) * 128:(s * 16 + 9 + j) * 128, :], outb[:, :])], ['ob'], ['mixT_d'], 'st_ob0'))
                return ops

            load_w(0)
            for g in proj_groups((0, 1, 2, 4, 3)):
                g()
            for j in range(8):
                early_chain(j)
                late = late_ops(j)
                if j + 1 < 8:
                    load_w(j + 1)
                    pg = proj_groups((0, 1, 2, 4))
                    pg3 = proj_groups((3,))
                else:
                    pg, pg3 = [], []
                for op_ in late:
                    op_()
                    if pg:
                        pg.pop(0)()
                for g in pg:
                    g()
                for g in pg3:
                    g()
            P.barrier()
            P.flush()

    def phase_outproj(l, s):
        ntile = NT if l == 0 else 16
        with contextlib.ExitStack() as st:
            wo = sb("owo", [128, 16, D], BF16, stack=st)
            aTt = [sb(f"oaT{i}", [128, 16, 128], BF16, stack=st) for i in range(2)]
            g1b = [sb(f"og1b{i}", [128, D], stack=st) for i in range(2)]
            xt = [sb(f"oxt{i}", [128, D], stack=st) for i in range(2)]
            xn = [sb(f"oxn{i}", [128, D], stack=st) for i in range(2)]
            stat = [sb(f"ostat{i}", [128, 4], stack=st) for i in range(2)]
            f32t = [sb(f"of32t{i}", [128, 16, 128], stack=st) for i in range(2)]
            fTt = [sb(f"ofTt{i}", [128, 16, 128], BF16, stack=st) for i in range(2)]
            scores = sb("oscores", [128, NT, 4, 4], stack=st)
            yp = ps("oyp", [128, D], stack=st)
            tp = ps("otp", [128, 1024], stack=st)
            lg = ps("olg", [128, 512], stack=st)
            wsrc = (woute if l == 0 else wouto).rearrange("(kc p) c -> p kc c", p=128)
            P.dma('pool', [(wo[:, kc, :], wsrc[:, kc, :]) for kc in range(16)], [], ['wo'], 'ld_wo')
            rows = [(l * 2 + 0) * 3 + 2, (l * 2 + 0) * 3 + s]
            P.dma('sp', [(g1b[i][:, :], grow[rows[i]:rows[i] + 1, :].partition_broadcast(128)) for i in range(2)], [], ['g1b'], 'ld_g1b')
            fT3 = fT_d[s * 128:(s + 1) * 128, :].rearrange("p (kc t) -> p kc t", kc=16)
            asrc = mixT_d[s * 2048:(s + 1) * 2048, :].rearrange("(c p) t -> p c t", p=128)
            for tt in range(ntile):
                sl = tt % 2
                kp = f'o{sl}'
                if l == 0:
                    isctx = tt < 2
                    src = ctxs[s * CTX + tt * 128: s * CTX + (tt + 1) * 128, :] if isctx else \
                        xs[s * SEQ + (tt - 2) * 128: s * SEQ + (tt - 1) * 128, :]
                    dst = x1_d[s * TOK + tt * 128: s * TOK + (tt + 1) * 128, :]
                else:
                    isctx = False
                    src = x2_d[s * TOK + CTX + tt * 128: s * TOK + CTX + (tt + 1) * 128, :]
                    dst = x3_d[s * SEQ + tt * 128: s * SEQ + (tt + 1) * 128, :]
                ftcol = tt * 128
                r = 2 if isctx else s
                gb_ = g1b[0] if isctx else g1b[1]
                a_, xt_, xn_, f32_, fT_, stat_ = aTt[sl], xt[sl], xn[sl], f32t[sl], fTt[sl], stat[sl]
                junk = fT_[:, :, :].rearrange("p a b -> p (a b)")
                P.dma('sp', [(a_[:, :, :], asrc[:, :, tt * 128:(tt + 1) * 128])], [], [('aT', sl)], f'ld_aT{sl}')
                P.dma('sp', [(xt_[:, :], src)], [], [('xt', sl)], f'ld_oxt{sl}')

                def yfn(e, a_=a_):
                    for cb in range(4):
                        for kc in range(16):
                            ins = e.matmul(yp[:, cb * 512:(cb + 1) * 512], lhsT=a_[:, kc, :],
                                           rhs=wo[:, kc, cb * 512:(cb + 1) * 512], start=(kc == 0), stop=(kc == 15))
                    return ins
                P.op('pe', yfn, [('aT', sl), 'wo'], ['yp'])
                P.op('dve', lambda e, gb_=gb_, xn_=xn_: e.tensor_tensor(out=xn_[:, :], in0=yp[:, :], in1=gb_[:, :], op=ALU.mult), ['yp', 'g1b'], [(kp, 'xn')])
                P.op('dve', lambda e, xn_=xn_, xt_=xt_: e.tensor_tensor(out=xt_[:, :], in0=xn_[:, :], in1=xt_[:, :], op=ALU.add), [(kp, 'xn'), ('xt', sl)], [('xt', sl)])
                P.dma('sp', [(dst, xt_[:, :])], [('xt', sl)], [('x1_d', sl)], f'st_x1{sl}')
                norm_tile((junk, stat_, xn_), ('xt', sl), xt_, l, 1, r, lambda kc, f32_=f32_: f32_[:, kc, :], ('f32t', sl), tp, 'tp', npass=2,
                          junk_key='fTt', kp=kp)
                P.op('pool', lambda e, fT_=fT_, f32_=f32_: e.tensor_copy(out=fT_[:, :, :], in_=f32_[:, :, :]), [('f32t', sl)], [(kp, 'fTt')])
                P.dma('sp', [(fT3[:, :, ftcol:ftcol + 128], fT_[:, :, :])], [(kp, 'fTt')], [('fT_d', sl)], f'st_fT{sl}')

                def lfn(e, f32_=f32_):
                    for kc in range(16):
                        ins = e.matmul(lg[:, 0:16], lhsT=f32_[:, kc, :], rhs=rwt[:, kc, :], start=(kc == 0), stop=(kc == 15))
                    return ins
                P.op('pe', lfn, [('f32t', sl), 'consts'], ['lg'])
                P.op('act', lambda e, tt=tt: e.activation(out=scores[:, tt, :, :].rearrange("p a b -> p (a b)"), in_=lg[:, 0:16], func=AF.Sigmoid),
                     ['lg'], ['scores'])
            routing(st, scores, ntile, s)
            P.barrier()
            P.flush()

    def routing(st, scores, T, s):
        sel = sb("r_sel", [128, NT, 4, 4], stack=st)
        sel2 = sb("r_sel2", [128, NT, 4, 4], stack=st)
        m1k = sb("r_m1k", [128, NT, 4, 4], stack=st)
        m2k = sb("r_m2k", [128, NT, 4, 4], stack=st)
        t1 = sb("r_t1", [128, NT, 4], stack=st)
        gs = sb("r_gs", [128, NT, 4], stack=st)
        gmask = sb("r_gmask", [128, NT, 4], stack=st)
        red = sb("r_red", [128, NT], stack=st)
        k = ['rt']

        def V(fn):
            P.op('dve', fn, ['scores', 'consts'] + k, k)
        sc_ = scores[:, 0:T]
        sl_, s2_, a1_, a2_ = sel[:, 0:T], sel2[:, 0:T], m1k[:, 0:T], m2k[:, 0:T]
        t1_, gs_, gm_, rd_ = t1[:, 0:T], gs[:, 0:T], gmask[:, 0:T], red[:, 0:T]

        def f16(a):
            return a.rearrange("p t a b -> p t (a b)")
        V(lambda e: e.tensor_tensor(out=f16(sl_), in0=f16(sc_), in1=bc_mid(rbb[:, :], T), op=ALU.add))
        pairs = [(0, 1), (0, 2), (0, 3), (1, 2), (1, 3), (2, 3)]
        for i, (a, b) in enumerate(pairs):
            dst = gs_ if i == 0 else t1_
            V(lambda e, a=a, b=b, dst=dst: e.tensor_tensor(out=dst, in0=sl_[:, :, :, a], in1=sl_[:, :, :, b], op=ALU.add))
            if i > 0:
                V(lambda e: e.tensor_tensor(out=gs_, in0=gs_, in1=t1_, op=ALU.max))
        V(lambda e: e.tensor_reduce(out=rd_, in_=gs_, axis=AX.X, op=ALU.max))
        V(lambda e: e.tensor_tensor(out=gm_, in0=gs_, in1=bc(rd_, 4), op=ALU.is_equal))
        V(lambda e: e.tensor_scalar(out=f16(s2_), in0=f16(sl_), scalar1=2.0, scalar2=None, op0=ALU.add))
        V(lambda e: e.tensor_tensor(out=s2_, in0=s2_, in1=bc(gm_, 4), op=ALU.mult))
        V(lambda e: e.tensor_reduce(out=rd_, in_=f16(s2_), axis=AX.X, op=ALU.max))
        V(lambda e: e.tensor_tensor(out=f16(a1_), in0=f16(s2_), in1=bc(rd_, 16), op=ALU.is_equal))
        V(lambda e: e.scalar_tensor_tensor(out=f16(s2_), in0=f16(a1_), scalar=-4.0, in1=f16(s2_), op0=ALU.mult, op1=ALU.add))
        V(lambda e: e.tensor_reduce(out=rd_, in_=f16(s2_), axis=AX.X, op=ALU.max))
        V(lambda e: e.tensor_tensor(out=f16(a2_), in0=f16(s2_), in1=bc(rd_, 16), op=ALU.is_equal))
        V(lambda e: e.tensor_tensor(out=f16(a1_), in0=f16(a1_), in1=f16(a2_), op=ALU.add))
        V(lambda e: e.tensor_tensor(out=f16(a1_), in0=f16(a1_), in1=f16(sc_), op=ALU.mult))
        V(lambda e: e.tensor_reduce(out=rd_, in_=f16(a1_), axis=AX.X, op=ALU.add))
        V(lambda e: e.reciprocal(out=rd_, in_=rd_))
        P.op('dve', lambda e: e.tensor_tensor(out=comb[:, s, 0:T, :], in0=f16(a1_), in1=bc(rd_, 16), op=ALU.mult), k, [('comb', s)])

    def phase_moe(l, s):
        if l == 0:
            TB, NBK, NSUB, SUBW, ntok = 1152, 2, 3, 384, TOK
        else:
            TB, NBK, NSUB, SUBW, ntok = 1024, 2, 2, 512, SEQ
        ntl = TB // 128
        with contextlib.ExitStack() as st:
            acc = sb("macc", [128, ntl, D], stack=st)
            fT = sb("mfT", [128, 16, TB], BF16, stack=st)
            WGU = [(sb(f"mwg{i}", [128, 16, 256], BF16, stack=st), sb(f"mwu{i}", [128, 16, 256], BF16, stack=st)) for i in range(2)]
            WD = [sb(f"mwd{i}", [128, 2, D], BF16, stack=st) for i in range(3)]
            hid = [sb(f"mhid{i}", [128, 2, TB], BF16, stack=st) for i in range(2)]
            sg = [sb(f"msg{i}", [128, 512], stack=st) for i in range(2)]
            g2b = [sb(f"mg2b{i}", [128, D], stack=st) for i in range(2)]
            stat = sb("mstat", [128, 8], stack=st)
            if l == 1:
                junk = sb("mjunk", [128, D], BF16, stack=st)
                fgb = sb("mfgb", [128, D], stack=st)
            gps = [ps(f"mgps{i}", [128, 512], stack=st) for i in range(2)]
            ups = [ps(f"mups{i}", [128, 512], stack=st) for i in range(2)]
            ops_ = [ps(f"mops{i}", [128, 1024], stack=st) for i in range(2)]
            rows = [(l * 2 + 1) * 3 + 2, (l * 2 + 1) * 3 + s]
            P.dma('sp', [(g2b[i][:, :], grow[rows[i]:rows[i] + 1, :].partition_broadcast(128)) for i in range(2)], [], ['g2b'], 'ld_g2b')
            if l == 1:
                P.dma('sp', [(fgb[:, :], fgrow.partition_broadcast(128))], [], ['fgb'], 'ld_fgb')
            fT3 = fT_d[s * 128:(s + 1) * 128, :].rearrange("p (kc t) -> p kc t", kc=16)
            ucount = [0]
            gi = [0]
            for bk in range(NBK):
                tok0 = bk * TB
                P.dma('sp', [(fT[:, :, 0:TB], fT3[:, :, tok0:tok0 + TB])], [], ['fT'], 'ld_mfT')
                prev_wd = []
                for ex in range(NE):
                    for q in range(4):
                        u = ucount[0]
                        ucount[0] += 1
                        sl = u % 2
                        sl3 = u % 3
                        wg, wu = WGU[sl]
                        wd = WD[sl3]
                        rg = (l * NE + ex) * D
                        rd = (l * NE + ex) * FF + q * 256
                        P.dma('pool', [(wg[:, :, :], ewg[rg:rg + D, q * 256:(q + 1) * 256].rearrange("(kc p) c -> p kc c", p=128)),
                                       (wu[:, :, :], ewu[rg:rg + D, q * 256:(q + 1) * 256].rearrange("(kc p) c -> p kc c", p=128))],
                              [], [('Wg', sl), ('Wu', sl)], f'ld_mw{sl}')
                        P.dma('pool', [(wd[:, :, :], ewd[rd:rd + 256, :].rearrange("(fc p) c -> p fc c", p=128))],
                              [], [('Wd', sl3)], f'ld_md{sl3}')
                        hs = hid[sl]

                        def gu_item(fc, nb, mid_cb, sl=sl, wg=wg, wu=wu, hs=hs):
                            i = gi[0] % 2
                            gi[0] += 1
                            c0 = nb * SUBW

                            def gfn(e):
                                for kc in range(16):
                                    ins = e.matmul(gps[i][:, 0:SUBW], lhsT=wg[:, kc, fc * 128:(fc + 1) * 128], rhs=fT[:, kc, c0:c0 + SUBW],
                                                   start=(kc == 0), stop=(kc == 15))
                                return ins

                            def ufn(e):
                                for kc in range(16):
                                    ins = e.matmul(ups[i][:, 0:SUBW], lhsT=wu[:, kc, fc * 128:(fc + 1) * 128], rhs=fT[:, kc, c0:c0 + SUBW],
                                                   start=(kc == 0), stop=(kc == 15))
                                return ins
                            P.op('pe', gfn, [('Wg', sl), 'fT'], [('gps', i)])
                            P.op('act', lambda e: e.activation(out=sg[i][:, 0:SUBW], in_=gps[i][:, 0:SUBW], func=AF.Silu),
                                 [('gps', i)], [('sg', i)])
                            mid_cb()
                            P.op('pe', ufn, [('Wu', sl), 'fT'], [('ups', i)])
                            P.op('dve', lambda e: e.tensor_tensor(out=hs[:, fc, c0:c0 + SUBW], in0=ups[i][:, 0:SUBW],
                                                                  in1=sg[i][:, 0:SUBW], op=ALU.mult),
                                 [('ups', i), ('sg', i)], [('hid', sl)])

                        def wd_item(tt, half, sl=sl, sl3=sl3, hs=hs, wd=wd, ex=ex, first=(ex == 0 and q == 0)):
                            gt = bk * ntl + tt

                            def ofn(e):
                                for fc in range(2):
                                    for c2 in range(2):
                                        cb = half * 2 + c2
                                        ins = e.matmul(ops_[half][:, c2 * 512:(c2 + 1) * 512], lhsT=hs[:, fc, tt * 128:(tt + 1) * 128],
                                                       rhs=wd[:, fc, cb * 512:(cb + 1) * 512], start=(fc == 0), stop=(fc == 1))
                                return ins
                            P.op('pe', ofn, [('hid', sl), ('Wd', sl3)], [('ops', half)])
                            if first:
                                P.op('dve', lambda e: e.tensor_scalar(
                                    out=acc[:, tt, half * 1024:(half + 1) * 1024], in0=ops_[half][:, :], scalar1=comb[:, s, gt, ex:ex + 1],
                                    scalar2=None, op0=ALU.mult),
                                    [('ops', half), ('comb', s)], [('acc', tt, half)])
                            else:
                                P.op('dve', lambda e: e.scalar_tensor_tensor(
                                    out=acc[:, tt, half * 1024:(half + 1) * 1024], in0=ops_[half][:, :], scalar=comb[:, s, gt, ex:ex + 1],
                                    in1=acc[:, tt, half * 1024:(half + 1) * 1024], op0=ALU.mult, op1=ALU.add),
                                    [('ops', half), ('comb', s), ('acc', tt, half)], [('acc', tt, half)])
                        gu_list = [(fc, nb) for fc in range(2) for nb in range(NSUB)]
                        nslots = 2 * len(gu_list)
                        nwd = len(prev_wd)
                        slot_i = [0]

                        def drain_slot():
                            k = slot_i[0]
                            slot_i[0] += 1
                            cnt = ((k + 1) * nwd) // nslots - (k * nwd) // nslots
                            for _ in range(cnt):
                                if prev_wd:
                                    prev_wd.pop(0)()
                        for (fc, nb) in gu_list:
                            gu_item(fc, nb, drain_slot)
                            drain_slot()
                        while prev_wd:
                            prev_wd.pop(0)()
                        prev_wd = [(lambda tt=tt, half=half, f=wd_item: f(tt, half)) for tt in range(ntl) for half in range(2)]
                while prev_wd:
                    prev_wd.pop(0)()
                sl_last = (ucount[0] - 1) % 2
                xbuf = [WGU[sl_last][i][:, :, :].bitcast(F32).rearrange("p a b -> p (a b)") for i in range(2)]
                for tt in range(ntl):
                    gt = bk * ntl + tt
                    xi = tt % 2
                    xb_ = xbuf[xi]
                    gk = ('Wg', sl_last) if xi == 0 else ('Wu', sl_last)
                    if l == 0:
                        isctx = gt < 2
                        src = x1_d[s * TOK + gt * 128: s * TOK + (gt + 1) * 128, :]
                        dst = x2_d[s * TOK + gt * 128: s * TOK + (gt + 1) * 128, :]
                    else:
                        isctx = False
                        src = x3_d[s * SEQ + gt * 128: s * SEQ + (gt + 1) * 128, :]
                        dst = outd[s * SEQ + gt * 128: s * SEQ + (gt + 1) * 128, :]
                    gb_ = g2b[0] if isctx else g2b[1]
                    P.dma('sp', [(xb_, src)], [], [('x1t', xi), gk], f'ld_mx{xi}')
                    P.op('dve', lambda e, tt=tt, gb_=gb_: e.tensor_tensor(out=acc[:, tt, :], in0=acc[:, tt, :], in1=gb_[:, :], op=ALU.mult),
                         [('acc', tt, 0), ('acc', tt, 1), 'g2b'], [('acc', tt, 0), ('acc', tt, 1)])
                    P.op(('pool' if tt % 2 == 0 else 'dve'), lambda e, tt=tt, xb_=xb_: e.tensor_tensor(out=xb_, in0=acc[:, tt, :], in1=xb_, op=ALU.add),
                         [('acc', tt, 0), ('acc', tt, 1), ('x1t', xi)], [('x1t', xi)])
                    if l == 1:
                        P.op('act', lambda e, xb_=xb_, xi=xi: e.activation(out=junk[:, :], in_=xb_, func=AF.Square, accum_out=stat[:, xi * 4:xi * 4 + 1]),
                             [('x1t', xi)], ['junk', ('st0', xi)])
                        P.op('act', lambda e, xi=xi: e.activation(out=stat[:, xi * 4 + 1:xi * 4 + 2], in_=stat[:, xi * 4:xi * 4 + 1], func=AF.Ln, scale=1.0 / D, bias=EPS),
                             [('st0', xi)], [('st1', xi)])
                        P.op('act', lambda e, xi=xi: e.activation(out=stat[:, xi * 4 + 2:xi * 4 + 3], in_=stat[:, xi * 4 + 1:xi * 4 + 2], func=AF.Exp, scale=-0.5),
                             [('st1', xi)], [('st2', xi)])
                        P.op('act', lambda e, xb_=xb_, xi=xi: e.activation(out=xb_, in_=xb_, func=AF.Copy, scale=stat[:, xi * 4 + 2:xi * 4 + 3]),
                             [('x1t', xi), ('st2', xi)], [('x1t', xi)])
                        P.op('pool', lambda e, xb_=xb_: e.tensor_tensor(out=xb_, in0=xb_, in1=fgb[:, :], op=ALU.mult), [('x1t', xi), 'fgb'], [('x1t', xi)])
                    P.dma('sp', [(dst, xb_)], [('x1t', xi), gk], [('mout', xi)], f'st_mo{xi}')
            P.barrier()
            P.flush()

    def phase_attn(s, hT):
        SCALE = 128 ** -0.5
        with contextlib.ExitStack() as st:
            w = [sb(f"aw{i}", [128, 16, 768], BF16, stack=st) for i in range(2)]
            qT = sb("aqT", [128, 2, SEQ], BF16, stack=st)
            kT = sb("akT", [128, 2, TOK], BF16, stack=st)
            V_ = sb("aV", [128, NT, 256], BF16, stack=st)
            cosT = sb("acos", [128, SEQ], stack=st)
            sinT = sb("asin", [128, SEQ], stack=st)
            qb16 = sb("aqb16", [128, 512], BF16, stack=st)
            t1 = sb("at1", [128, 512], stack=st)
            t2 = sb("at2", [128, 512], stack=st)
            PT = [sb(f"aPT{i}", [128, 512], BF16, stack=st) for i in range(2)]
            rs = sb("ars", [128, 512], stack=st)
            oc = [sb(f"aoc{i}", [128, 512], stack=st) for i in range(2)]
            otmp = sb("aotmp", [128, 512], stack=st)
            sq = [sb(f"asq{i}", [128, 512], stack=st) for i in range(2)]
            ofst = [sb(f"aofst{i}", [128, 512], BF16, stack=st) for i in range(2)]
            A = [ps(f"aA{i}", [128, 512], stack=st) for i in range(2)]
            O = [[ps(f"aO{i}_{c}", [128, 512], stack=st) for c in range(3)] for i in range(2)]
            P.dma('sp', [(cosT[:, :], ropec), (sinT[:, :], ropes)], [], ['rope'], 'ld_rope')
            ai = [0]

            def nextA():
                a = ai[0] % 2
                ai[0] += 1
                return a
            oset = [0]
            LATB = [(0, 512), (512, 1024), (1024, 1536), (1536, 2048)]
            ATT_STAGE = int(os.environ.get('ATT_STAGE', '9'))
            pending = []
            for hd in range(int(os.environ.get('ATT_HEADS', '8'))):
                sl = hd % 2
                P.dma('pool', [(w[sl][:, :, sec * 256:(sec + 1) * 256],
                                wino[:, sec * 2048 + hd * 256: sec * 2048 + (hd + 1) * 256].rearrange("(kc p) c -> p kc c", p=128))
                               for sec in range(3)], [], [('w', sl)], f'ld_aw{sl}')
                for sec, dstT in ((0, qT), (1, kT)):
                    for m in range(2):
                        wc0 = sec * 256 + m * 128
                        if sec == 1:
                            a = nextA()

                            def fnc(e, a=a, wc0=wc0, sl=sl):
                                for kc in range(16):
                                    ins = e.matmul(A[a][:, 0:256], lhsT=w[sl][:, kc, wc0:wc0 + 128], rhs=hT[:, kc, 0:256], start=(kc == 0), stop=(kc == 15))
                                return ins
                            P.op('pe', fnc, [('w', sl), ('hT', 0), ('hT', 1)], [('A', a)])
                            P.op('act', lambda e, a=a, m=m: e.activation(out=kT[:, m, 0:256], in_=A[a][:, 0:256], func=AF.Copy), [('A', a)], [('kT', m)])
                        for (l0, l1) in LATB:
                            a = nextA()
                            a2 = nextA()
                            toff = 0 if sec == 0 else CTX

                            def fnp(e, a=a, wc0=wc0, sl=sl, l0=l0, l1=l1):
                                for kc in range(16):
                                    ins = e.matmul(A[a][:, :], lhsT=w[sl][:, kc, wc0:wc0 + 128], rhs=hT[:, kc, CTX + l0:CTX + l1], start=(kc == 0), stop=(kc == 15))
                                return ins
                            P.op('pe', fnp, [('w', sl)] + blk_tiles(CTX + l0, CTX + l1), [('A', a)])
                            P.op('act', lambda e, a=a: e.activation(out=qb16[:, :], in_=A[a][:, :], func=AF.Copy), [('A', a)], ['qb16'])
                            P.op('dve', lambda e, a=a, l0=l0, l1=l1: e.tensor_tensor(out=t1[:, :], in0=A[a][:, :], in1=cosT[:, l0:l1], op=ALU.mult),
                                 [('A', a), 'rope', 'qb16'], ['t1'])
                            P.op('pe', lambda e, a2=a2: e.matmul(A[a2][:, :], lhsT=rpermb[:, :], rhs=qb16[:, :], start=True, stop=True),
                                 ['qb16', 'rpermb'], [('A', a2)])
                            P.op('dve', lambda e, a2=a2, l0=l0, l1=l1: e.tensor_tensor(out=t2[:, :], in0=A[a2][:, :], in1=sinT[:, l0:l1], op=ALU.mult),
                                 [('A', a2), 'rope'], ['t2'])
                            P.op('pool', lambda e, dstT=dstT, m=m, toff=toff, l0=l0, l1=l1: e.tensor_tensor(
                                out=dstT[:, m, toff + l0:toff + l1], in0=t1[:, :], in1=t2[:, :], op=ALU.add),
                                ['t1', 't2'], [('qk', sec, m)] if sec == 0 else [('kT', m)])
                for tt in range(NT):
                    a = nextA()

                    def fnv(e, a=a, tt=tt, sl=sl):
                        for kc in range(16):
                            ins = e.matmul(A[a][:, 0:256], lhsT=hT[:, kc, tt * 128:(tt + 1) * 128], rhs=w[sl][:, kc, 512:768], start=(kc == 0), stop=(kc == 15))
                        return ins
                    P.op('pe', fnv, [('w', sl), ('hT', tt)], [('A', a)])
                    P.op('act', lambda e, a=a, tt=tt: e.activation(out=V_[:, tt, :], in_=A[a][:, 0:256], func=AF.Copy), [('A', a)], ['V'])
                for qb in range(4 if ATT_STAGE >= 2 else 0):
                    q0 = qb * 512
                    for m in range(2):
                        os_ = oset[0] % 2
                        oset[0] += 1
                        Oc = O[os_]
                        def emitS(kt, m=m, q0=q0):
                            a = nextA()
                            P.op('pe', lambda e, a=a, m=m, kt=kt, q0=q0: e.matmul(A[a][:, :], lhsT=kT[:, m, kt * 128:(kt + 1) * 128], rhs=qT[:, m, q0:q0 + 512],
                                                                                    start=True, stop=True), [('kT', m), ('qk', 0, m)], [('A', a)])
                            return a
                        a_next = emitS(0)
                        for kt in range(NT):
                            a = a_next
                            if kt + 1 < NT:
                                a_next = emitS(kt + 1)
                            P.op('act', lambda e, a=a: e.activation(out=PT[a][:, :], in_=A[a][:, :], func=AF.Exp, scale=SCALE), [('A', a)], [('PT', a)])

                            def fno(e, a=a, kt=kt, Oc=Oc):
                                e.matmul(Oc[0][:, :], lhsT=V_[:, kt, 0:128], rhs=PT[a][:, :], start=(kt == 0), stop=(kt == NT - 1))
                                e.matmul(Oc[1][:, :], lhsT=V_[:, kt, 128:256], rhs=PT[a][:, :], start=(kt == 0), stop=(kt == NT - 1))
                                return e.matmul(Oc[2][:, :], lhsT=onesb[:, :], rhs=PT[a][:, :], start=(kt == 0), stop=(kt == NT - 1))
                            P.op('pe', fno, ['V', ('PT', a), 'onesb'], [('O', os_)])
                            if kt == 3 and pending:
                                pending.pop(0)()
                        P.op('dve', lambda e, Oc=Oc: e.reciprocal(out=rs[:, :], in_=Oc[2][:, :]), [('O', os_)], ['rs'])
                        if m == 0:
                            for c in range(2):
                                P.op('dve', lambda e, c=c, Oc=Oc: e.tensor_tensor(out=oc[c][:, :], in0=Oc[c][:, :], in1=rs[:, :], op=ALU.mult),
                                     [('O', os_), 'rs'], [('oc', c)])
                        else:
                            P.op('dve', lambda e: e.tensor_scalar(out=rs[:, :], in0=rs[:, :], scalar1=neglam[:, 0:1], scalar2=None, op0=ALU.mult),
                                 ['rs', 'neglam'], ['rs'])
                            for c in range(2):
                                P.op('dve', lambda e, c=c, Oc=Oc: e.tensor_tensor(out=otmp[:, :], in0=Oc[c][:, :], in1=rs[:, :], op=ALU.mult),
                                     [('O', os_), 'rs'], ['otmp'])
                                P.op('pool', lambda e, c=c: e.tensor_tensor(out=oc[c][:, :], in0=oc[c][:, :], in1=otmp[:, :], op=ALU.add),
                                     [('oc', c), 'otmp'], [('oc', c)])
                    def tail(q0=q0, hd=hd):
                        for c in range(2):
                            P.op('act', lambda e, c=c: e.activation(out=sq[c][:, :], in_=oc[c][:, :], func=AF.Square), [('oc', c)], [('sq', c)])
                        a = nextA()
                        def fnn(e, a=a):
                            e.matmul(A[a][:, :], lhsT=onesf[:, :], rhs=sq[0][:, :], start=True, stop=False)
                            return e.matmul(A[a][:, :], lhsT=onesf[:, :], rhs=sq[1][:, :], start=False, stop=True)
                        P.op('pe', fnn, [('sq', 0), ('sq', 1), 'onesf'], [('A', a)])
                        P.op('dve', lambda e, a=a: e.tensor_scalar(out=rs[:, :], in0=A[a][:, :], scalar1=1.0 / 256, scalar2=EPS, op0=ALU.mult, op1=ALU.add),
                             [('A', a)], ['rs'])
                        P.op('act', lambda e: e.activation(out=rs[:, :], in_=rs[:, :], func=AF.Sqrt), ['rs'], ['rs'])
                        P.op('dve', lambda e: e.reciprocal(out=rs[:, :], in_=rs[:, :]), ['rs'], ['rs'])
                        for c in range(2):
                            P.op('dve', lambda e, c=c: e.scalar_tensor_tensor(out=ofst[c][:, :], in0=oc[c][:, :], scalar=subg[:, c:c + 1], in1=rs[:, :],
                                                                              op0=ALU.mult, op1=ALU.mult), [('oc', c), 'rs', 'consts'], [('ofst', c)])
                            ch = hd * 2 + c
                            P.dma('sp', [(mixT_d[(s * 16 + ch) * 128:(s * 16 + ch + 1) * 128, q0:q0 + 512], ofst[c][:, :])], [('ofst', c)], ['mixT_d'], f'st_of{c}')
                        nextA()
                    if ATT_STAGE >= 3:
                        pending.append(tail)
                while pending:
                    pending.pop(0)()
            P.barrier()
            P.flush()

    stages = []
    phase_setup()
    stages.append('setup')
    done = [False]

    def chk(name):
        if stop_after == name:
            done[0] = True
        return done[0]

    for l in range(start_layer, 2):
        if done[0]:
            break
        phase_ada(l)
        if chk(f'ada{l}'):
            break
        for s in range(2):
            with contextlib.ExitStack() as sth:
                hT = sb(f"hT_{l}_{s}", [128, 16, TOK], BF16, stack=sth)
                phase_norm1(l, s, hT, sth)
                if chk(f'norm1_{l}_{s}'):
                    break
                if l == 0:
                    phase_even(s, hT)
                else:
                    phase_attn(s, hT)
            if chk(f'mix_{l}_{s}'):
                break
            phase_outproj(l, s)
            if chk(f'outproj_{l}_{s}'):
                break
            phase_moe(l, s)
            if chk(f'moe_{l}_{s}'):
                break
    P.barrier()
    P.flush()
    es.close()
    return nc


def _fm(v, n=16):
    return np.ascontiguousarray(np.asarray(v, np.float32).reshape(n, 128).T)


def prep_inputs(inputs):
    g = {k: np.asarray(v) for k, v in inputs.items()}
    rep = {}
    rep['adaw'] = np.ascontiguousarray(g['ada_w'].reshape(2 * D, 6 * D))
    rep['adabT'] = np.ascontiguousarray(np.concatenate([_fm(g['ada_b'][l], 96) for l in range(2)], axis=1))
    rep['ngT'] = np.ascontiguousarray(np.concatenate([_fm(g['norm_mix_g'][0]), _fm(g['norm_ffn_g'][0]),
                                                       _fm(g['norm_mix_g'][1]), _fm(g['norm_ffn_g'][1])], axis=1))
    rep['fgrow'] = np.ascontiguousarray(g['final_g'].reshape(1, D))
    rep['wine'] = np.ascontiguousarray(g['w_in_e'][0])
    rep['cawT'] = np.ascontiguousarray(g['conv_a_w'][0].reshape(3, 8, 128).transpose(2, 1, 0).reshape(128, 24))
    rep['cbwT'] = np.ascontiguousarray(g['conv_b_w'][0].reshape(4, 8, 128).transpose(2, 1, 0).reshape(128, 32))
    rep['cbbT'] = _fm(g['conv_b_b'][0], 8)
    rep['lruw'] = np.ascontiguousarray(np.stack([g['lru_wa'][0], g['lru_wi'][0]], axis=0).reshape(4096, 128))
    rep['lrubT'] = np.ascontiguousarray(np.stack([g['lru_ba'][0], g['lru_bi'][0]], axis=0).reshape(2, 2, 8, 128).transpose(3, 0, 1, 2).reshape(128, 32))
    rep['lamT'] = np.ascontiguousarray(g['lru_lam'][0].reshape(2, 8, 128).transpose(2, 0, 1).reshape(128, 16))
    rep['woute'] = np.ascontiguousarray(g['w_out_e'][0])
    rep['wino'] = np.ascontiguousarray(g['w_in_o'][0])
    rep['lamv'] = np.ascontiguousarray(np.concatenate([g['lam_q1'][0], g['lam_k1'][0], g['lam_q2'][0], g['lam_k2'][0]]).reshape(1, 512))
    rep['sublnT'] = _fm(g['subln_g'][0], 2)
    rep['wouto'] = np.ascontiguousarray(g['w_out_o'][0])
    rep['rw'] = np.ascontiguousarray(g['router_w'])
    rep['rb'] = np.ascontiguousarray(g['router_b'].reshape(1, NE))
    rep['ewg'] = np.ascontiguousarray(g['exp_w_gate'].reshape(2 * NE * D, FF))
    rep['ewu'] = np.ascontiguousarray(g['exp_w_up'].reshape(2 * NE * D, FF))
    rep['ewd'] = np.ascontiguousarray(g['exp_w_down'].reshape(2 * NE * FF, D))
    rep['identd'] = np.eye(128, dtype=np.float32)
    t = np.arange(SEQ)
    row = (t // 64).astype(np.float32)
    col = (t % 64).astype(np.float32)
    inv = (10000.0 ** (-np.arange(0, 64, 2, dtype=np.float32) / 64)).astype(np.float32)
    ang_r = row[:, None] * inv
    ang_c = col[:, None] * inv
    ang = np.concatenate([ang_r, ang_r, ang_c, ang_c], axis=-1)
    rep['ropec'] = np.ascontiguousarray(np.cos(ang).astype(np.float32).T)
    rep['ropes'] = np.ascontiguousarray(np.sin(ang).astype(np.float32).T)
    rp = np.zeros((128, 128), np.float32)
    for m in range(128):
        if (m % 64) < 32:
            rp[m + 32, m] = -1.0
        else:
            rp[m - 32, m] = 1.0
    rep['rpermd'] = rp
    maps = []
    for c in range(8):
        mp = dict(rep)
        mp['xs'] = np.ascontiguousarray(g['x'][2 * c:2 * c + 2].reshape(2 * SEQ, D))
        mp['ctxs'] = np.ascontiguousarray(g['ctx'][2 * c:2 * c + 2].reshape(2 * CTX, D))
        c3 = np.stack([g['c'][2 * c], g['c'][2 * c + 1], g['c_ctx']], axis=0)
        mp['c3T'] = np.ascontiguousarray(c3.reshape(3, 16, 128).transpose(2, 1, 0).reshape(128, 48))
        maps.append(mp)
    return maps


def kernel(**inputs):
    maps = prep_inputs(inputs)
    nc = build()
    res = run_bass_kernel_spmd(nc, maps, core_ids=list(range(8)))
    out = np.stack([r["out"].reshape(2, SEQ, D) for r in res.results], axis=0).reshape(16, SEQ, D)
    return out.astype(np.float32)
```
